# Optimizing a Trainium2 kernel written in Bass

```python
import math
import jax
import jax.numpy as jnp
from jax import lax
import numpy as np

D_MODEL = 4096
BATCH = 4
SEQ = 4096
DEPTH = 4

CTX_LEN = 256
GRID_W = 64
BLOCK = 128
WINDOW = 128
ROPE_BASE = 10000.0
NORM_EPS = 1e-6
N_MOD = 6

HEAD_DIM = 128
SWA_HEADS = 12
SWA_KV_HEADS = 4
MLA_HEADS = 10
MLA_Q_RANK = 768
MLA_KV_RANK = 512
MLA_NOPE = 128
MLA_ROPE = 64
MLA_V = 128
DIF_HEADS = 5
DIF_QK = 128
DIF_V = 256

MIX_WIDTH = SWA_HEADS * HEAD_DIM + MLA_HEADS * MLA_V + DIF_HEADS * DIF_V
IN_SIZES = (SWA_HEADS * HEAD_DIM, SWA_KV_HEADS * HEAD_DIM, SWA_KV_HEADS * HEAD_DIM,
            MLA_Q_RANK, MLA_KV_RANK, MLA_ROPE,
            DIF_HEADS * 2 * DIF_QK, DIF_HEADS * 2 * DIF_QK, DIF_HEADS * DIF_V)
IN_WIDTH = sum(IN_SIZES)

D_FF = 4096
N_EXPERTS = 8
TOP_K = 2
D_FF_EXPERT = 1024
N_DENSE = (DEPTH + 1) // 2
N_MOE = DEPTH // 2

kernel_name = 'hybrid_group_dit_trunk'


def _rms_norm(x, g):
    xf = x.astype(jnp.float32)
    y = xf * lax.rsqrt(jnp.mean(xf * xf, axis=-1, keepdims=True) + NORM_EPS)
    return (y * g.astype(jnp.float32)).astype(x.dtype)


def _modulate(x, shift, scale):
    return x * (1 + scale) + shift


def _axial_rope(t_row, t_col, dim):
    quarter = dim // 4
    inv = ROPE_BASE ** (-jnp.arange(quarter, dtype=jnp.float32) / quarter)
    ang = jnp.concatenate([t_row[:, None].astype(jnp.float32) * inv,
                           t_col[:, None].astype(jnp.float32) * inv], axis=-1)
    return jnp.cos(ang), jnp.sin(ang)


def _rope(x, cos, sin):
    half = x.shape[-1] // 2
    idx = (None, slice(None)) + (None,) * (x.ndim - 3) + (slice(None),)
    cs, sn = cos[idx], sin[idx]
    x1 = x[..., :half].astype(jnp.float32)
    x2 = x[..., half:].astype(jnp.float32)
    return jnp.concatenate([x1 * cs - x2 * sn, x1 * sn + x2 * cs], axis=-1).astype(x.dtype)


def _split_cols(z):
    out, start = [], 0
    for w in IN_SIZES:
        out.append(z[..., start:start + w])
        start += w
    return out


def _swa_project(q_cols, k_cols, v_cols, q_g, k_g, rope):
    b, n = q_cols.shape[0], q_cols.shape[1]
    q = _rms_norm(q_cols.reshape(b, n, SWA_HEADS, HEAD_DIM), q_g)
    k = _rms_norm(k_cols.reshape(b, n, SWA_KV_HEADS, HEAD_DIM), k_g)
    v = v_cols.reshape(b, n, SWA_KV_HEADS, HEAD_DIM)
    if rope is not None:
        q = _rope(q, *rope)
        k = _rope(k, *rope)
    return q, k, v


def _mla_project(cq, ckv, kr, cq_g, ckv_g, w_uq, w_ukv, q_g, k_g, rope):
    b, n = cq.shape[0], cq.shape[1]
    q = (_rms_norm(cq, cq_g) @ w_uq).reshape(b, n, MLA_HEADS, MLA_NOPE + MLA_ROPE)
    kv = (_rms_norm(ckv, ckv_g) @ w_ukv).reshape(b, n, MLA_HEADS, MLA_NOPE + MLA_V)
    k_nope, v = kv[..., :MLA_NOPE], kv[..., MLA_NOPE:]
    k = jnp.concatenate([k_nope, jnp.broadcast_to(kr[:, :, None, :], (b, n, MLA_HEADS, MLA_ROPE))], axis=-1)
    q = _rms_norm(q, q_g)
    k = _rms_norm(k, k_g)
    if rope is not None:
        q = jnp.concatenate([q[..., :MLA_NOPE], _rope(q[..., MLA_NOPE:], *rope)], axis=-1)
        k = jnp.concatenate([k[..., :MLA_NOPE], _rope(k[..., MLA_NOPE:], *rope)], axis=-1)
    return q, k, v


def _dif_project(q_cols, k_cols, v_cols, q_g, k_g, rope):
    b, n = q_cols.shape[0], q_cols.shape[1]
    q = _rms_norm(q_cols.reshape(b, n, DIF_HEADS, 2, DIF_QK), q_g)
    k = _rms_norm(k_cols.reshape(b, n, DIF_HEADS, 2, DIF_QK), k_g)
    v = v_cols.reshape(b, n, DIF_HEADS, DIF_V)
    if rope is not None:
        q = _rope(q, *rope)
        k = _rope(k, *rope)
    return q, k, v


def _window_gqa(q, k, v, kc, vc, sink):
    b, s = q.shape[0], q.shape[1]
    nb = s // BLOCK
    rep = SWA_HEADS // SWA_KV_HEADS
    scale = HEAD_DIM ** -0.5
    qb = q.reshape(b, nb, BLOCK, SWA_KV_HEADS, rep, HEAD_DIM)

    def bands(t):
        tp = jnp.pad(t, ((0, 0), (BLOCK, BLOCK), (0, 0), (0, 0)))
        tp = tp.reshape(b, nb + 2, BLOCK, SWA_KV_HEADS, HEAD_DIM)
        return jnp.concatenate([tp[:, :-2], tp[:, 1:-1], tp[:, 2:]], axis=2)

    kw, vw = bands(k), bands(v)
    qpos = jnp.arange(s).reshape(nb, BLOCK)
    kpos = jnp.arange(nb)[:, None] * BLOCK + jnp.arange(3 * BLOCK)[None, :] - BLOCK
    valid = ((jnp.abs(qpos[:, :, None] - kpos[:, None, :]) <= WINDOW)
             & (kpos >= 0)[:, None, :] & (kpos < s)[:, None, :])
    s_win = jnp.einsum('bnqgrd,bnkgd->bngrqk', qb, kw).astype(jnp.float32) * scale
    s_win = jnp.where(valid[None, :, None, None], s_win, -jnp.inf)
    s_ctx = jnp.einsum('bnqgrd,bcgd->bngrqc', qb, kc).astype(jnp.float32) * scale
    s_sink = jnp.broadcast_to(sink.astype(jnp.float32).reshape(1, 1, SWA_KV_HEADS, rep, 1, 1),
                              s_win.shape[:-1] + (1,))
    p = jax.nn.softmax(jnp.concatenate([s_win, s_ctx, s_sink], axis=-1), axis=-1).astype(v.dtype)
    nw, nc = 3 * BLOCK, kc.shape[1]
    out = (jnp.einsum('bngrqk,bnkgd->bnqgrd', p[..., :nw], vw)
           + jnp.einsum('bngrqc,bcgd->bnqgrd', p[..., nw:nw + nc], vc))
    return out.reshape(b, s, SWA_HEADS * HEAD_DIM)


def _ctx_gqa(qc, kc, vc, sink):
    b, n = qc.shape[0], qc.shape[1]
    rep = SWA_HEADS // SWA_KV_HEADS
    qg = qc.reshape(b, n, SWA_KV_HEADS, rep, HEAD_DIM)
    sc = jnp.einsum('bqgrd,bkgd->bgrqk', qg, kc).astype(jnp.float32) * (HEAD_DIM ** -0.5)
    s_sink = jnp.broadcast_to(sink.astype(jnp.float32).reshape(1, SWA_KV_HEADS, rep, 1, 1),
                              sc.shape[:-1] + (1,))
    p = jax.nn.softmax(jnp.concatenate([sc, s_sink], axis=-1), axis=-1)[..., :n].astype(vc.dtype)
    return jnp.einsum('bgrqk,bkgd->bqgrd', p, vc).reshape(b, n, SWA_HEADS * HEAD_DIM)


def _attend(q, k, v):
    s = jnp.einsum('bqhd,bkhd->bhqk', q, k).astype(jnp.float32) * (q.shape[-1] ** -0.5)
    p = jax.nn.softmax(s, axis=-1).astype(v.dtype)
    return jnp.einsum('bhqk,bkhe->bqhe', p, v)


def _diff_attend(q, k, v, lam):
    s = jnp.einsum('bqhmd,bkhmd->bhmqk', q, k).astype(jnp.float32) * (DIF_QK ** -0.5)
    p = jax.nn.softmax(s, axis=-1)
    a = (p[:, :, 0] - lam * p[:, :, 1]).astype(v.dtype)
    return jnp.einsum('bhqk,bkhe->bqhe', a, v)


def _sweep_query_blocks(fn, q):
    b, s = q.shape[0], q.shape[1]
    nb = s // BLOCK
    qb = jnp.moveaxis(q.reshape((b, nb, BLOCK) + q.shape[2:]), 1, 0)
    out = lax.map(fn, qb)
    return jnp.moveaxis(out, 0, 1).reshape((b, s) + out.shape[3:])


def _merge(y_swa, y_mla, y_dif, subln_g, lam_init):
    b, n = y_swa.shape[0], y_swa.shape[1]
    y_dif = _rms_norm(y_dif, subln_g) * (1 - lam_init)
    return jnp.concatenate([y_swa.reshape(b, n, -1), y_mla.reshape(b, n, -1),
                            y_dif.reshape(b, n, -1)], axis=-1)


def _swiglu(x, w1, w3, w2):
    return (jax.nn.silu(x @ w1) * (x @ w3)) @ w2


def _moe(x, router, w1, w3, w2):
    logits = (x @ router).astype(jnp.float32)
    top_v, top_i = lax.top_k(logits, TOP_K)
    gates = jax.nn.softmax(top_v, axis=-1)
    combine = jnp.sum(jax.nn.one_hot(top_i, N_EXPERTS, dtype=jnp.float32) * gates[..., None],
                      axis=-2).astype(x.dtype)
    y = jnp.zeros_like(x)
    for e in range(N_EXPERTS):
        y = y + combine[..., e:e + 1] * _swiglu(x, w1[e], w3[e], w2[e])
    return y


def _channel_mixer(l, x, ffn_w1, ffn_w3, ffn_w2, moe_router, moe_w1, moe_w3, moe_w2):
    i = l // 2
    if l % 2 == 0:
        return _swiglu(x, ffn_w1[i], ffn_w3[i], ffn_w2[i])
    return _moe(x, moe_router[i], moe_w1[i], moe_w3[i], moe_w2[i])


def setup_inputs(seed: int = 0) -> dict:
    key = jax.random.key(seed)
    k = jax.random.split(key, 30)
    f32 = jnp.float32

    def nrm(kk, shape, scale):
        return jax.random.normal(kk, shape, f32) * scale

    def gain(kk, shape):
        return 1.0 + 0.02 * jax.random.normal(kk, shape, f32)

    L, D = DEPTH, D_MODEL
    return {
        'x': nrm(k[0], (BATCH, SEQ, D), 1.0),
        'c': nrm(k[1], (BATCH, D), 1.0),
        'ctx': nrm(k[2], (BATCH, CTX_LEN, D), 1.0),
        'c_ctx': nrm(k[3], (D,), 1.0),
        'ada_w': nrm(k[4], (L, D, N_MOD * D), 0.5 * D ** -0.5),
        'ada_b': nrm(k[5], (L, N_MOD * D), 0.02),
        'norm1_g': gain(k[6], (L, D)),
        'norm2_g': gain(k[7], (L, D)),
        'w_in': nrm(k[8], (L, D, IN_WIDTH), D ** -0.5),
        'w_out': nrm(k[9], (L, MIX_WIDTH, D), MIX_WIDTH ** -0.5),
        'swa_sink': nrm(k[10], (L, SWA_HEADS), 0.5),
        'swa_q_norm': gain(k[11], (L, HEAD_DIM)),
        'swa_k_norm': gain(k[12], (L, HEAD_DIM)),
        'mla_cq_norm': gain(k[13], (L, MLA_Q_RANK)),
        'mla_ckv_norm': gain(k[14], (L, MLA_KV_RANK)),
        'mla_w_uq': nrm(k[15], (L, MLA_Q_RANK, MLA_HEADS * (MLA_NOPE + MLA_ROPE)), MLA_Q_RANK ** -0.5),
        'mla_w_ukv': nrm(k[16], (L, MLA_KV_RANK, MLA_HEADS * (MLA_NOPE + MLA_V)), MLA_KV_RANK ** -0.5),
        'mla_q_norm': gain(k[17], (L, MLA_NOPE + MLA_ROPE)),
        'mla_k_norm': gain(k[18], (L, MLA_NOPE + MLA_ROPE)),
        'dif_lambda': nrm(k[19], (L, 4, DIF_QK), 0.1),
        'dif_q_norm': gain(k[20], (L, 2, DIF_QK)),
        'dif_k_norm': gain(k[21], (L, 2, DIF_QK)),
        'dif_subln': gain(k[22], (L, DIF_V)),
        'ffn_w1': nrm(k[23], (N_DENSE, D, D_FF), D ** -0.5),
        'ffn_w3': nrm(k[24], (N_DENSE, D, D_FF), D ** -0.5),
        'ffn_w2': nrm(k[25], (N_DENSE, D_FF, D), D_FF ** -0.5),
        'moe_router': nrm(k[26], (N_MOE, D, N_EXPERTS), D ** -0.5),
        'moe_w1': nrm(k[27], (N_MOE, N_EXPERTS, D, D_FF_EXPERT), D ** -0.5),
        'moe_w3': nrm(k[28], (N_MOE, N_EXPERTS, D, D_FF_EXPERT), D ** -0.5),
        'moe_w2': nrm(k[29], (N_MOE, N_EXPERTS, D_FF_EXPERT, D), D_FF_EXPERT ** -0.5),
    }


def reference(x, c, ctx, c_ctx, ada_w, ada_b, norm1_g, norm2_g, w_in, w_out,
              swa_sink, swa_q_norm, swa_k_norm,
              mla_cq_norm, mla_ckv_norm, mla_w_uq, mla_w_ukv, mla_q_norm, mla_k_norm,
              dif_lambda, dif_q_norm, dif_k_norm, dif_subln,
              ffn_w1, ffn_w3, ffn_w2, moe_router, moe_w1, moe_w3, moe_w2):
    b, s, d = x.shape
    rows = s // GRID_W
    t_row = jnp.broadcast_to(jnp.arange(rows)[:, None], (rows, GRID_W)).reshape(-1)
    t_col = jnp.broadcast_to(jnp.arange(GRID_W)[None, :], (rows, GRID_W)).reshape(-1)
    rope_head = _axial_rope(t_row, t_col, HEAD_DIM)
    rope_mla = _axial_rope(t_row, t_col, MLA_ROPE)
    silu_c = jax.nn.silu(c)
    silu_cc = jax.nn.silu(c_ctx)
    h, hc = x, ctx
    for l in range(DEPTH):
        last = l == DEPTH - 1
        mod = (silu_c @ ada_w[l] + ada_b[l]).reshape(b, 1, N_MOD, d)
        modc = (silu_cc @ ada_w[l] + ada_b[l]).reshape(N_MOD, d)
        sh1, sc1, g1, sh2, sc2, g2 = [mod[:, :, i] for i in range(N_MOD)]
        csh1, csc1, cg1, csh2, csc2, cg2 = [modc[i] for i in range(N_MOD)]

        lat = _split_cols(_modulate(_rms_norm(h, norm1_g[l]), sh1, sc1) @ w_in[l])
        cx = _split_cols(_modulate(_rms_norm(hc, norm1_g[l]), csh1, csc1) @ w_in[l])

        sq, sk, sv = _swa_project(lat[0], lat[1], lat[2], swa_q_norm[l], swa_k_norm[l], rope_head)
        sqc, skc, svc = _swa_project(cx[0], cx[1], cx[2], swa_q_norm[l], swa_k_norm[l], None)
        mq, mk, mv = _mla_project(lat[3], lat[4], lat[5], mla_cq_norm[l], mla_ckv_norm[l],
                                  mla_w_uq[l], mla_w_ukv[l], mla_q_norm[l], mla_k_norm[l], rope_mla)
        mqc, mkc, mvc = _mla_project(cx[3], cx[4], cx[5], mla_cq_norm[l], mla_ckv_norm[l],
                                     mla_w_uq[l], mla_w_ukv[l], mla_q_norm[l], mla_k_norm[l], None)
        dq, dk, dv = _dif_project(lat[6], lat[7], lat[8], dif_q_norm[l], dif_k_norm[l], rope_head)
        dqc, dkc, dvc = _dif_project(cx[6], cx[7], cx[8], dif_q_norm[l], dif_k_norm[l], None)

        lam_init = 0.8 - 0.6 * math.exp(-0.3 * l)
        lq1, lk1, lq2, lk2 = [dif_lambda[l, i].astype(jnp.float32) for i in range(4)]
        lam = jnp.exp(jnp.sum(lq1 * lk1)) - jnp.exp(jnp.sum(lq2 * lk2)) + lam_init

        mk_all = jnp.concatenate([mk, mkc], axis=1)
        mv_all = jnp.concatenate([mv, mvc], axis=1)
        dk_all = jnp.concatenate([dk, dkc], axis=1)
        dv_all = jnp.concatenate([dv, dvc], axis=1)
        y_swa = _window_gqa(sq, sk, sv, skc, svc, swa_sink[l])
        y_mla = _sweep_query_blocks(lambda qb: _attend(qb, mk_all, mv_all), mq)
        y_dif = _sweep_query_blocks(lambda qb: _diff_attend(qb, dk_all, dv_all, lam), dq)
        y = _merge(y_swa, y_mla, y_dif, dif_subln[l], lam_init) @ w_out[l]
        h_mixed = h + g1 * y

        if not last:
            yc = _merge(_ctx_gqa(sqc, skc, svc, swa_sink[l]), _attend(mqc, mkc, mvc),
                        _diff_attend(dqc, dkc, dvc, lam), dif_subln[l], lam_init) @ w_out[l]
            hc = hc + cg1 * yc
        h = h_mixed

        hn = _modulate(_rms_norm(h, norm2_g[l]), sh2, sc2)
        h = h + g2 * _channel_mixer(l, hn, ffn_w1, ffn_w3, ffn_w2, moe_router, moe_w1, moe_w3, moe_w2)
        if not last:
            hcn = _modulate(_rms_norm(hc, norm2_g[l]), csh2, csc2)
            hc = hc + cg2 * _channel_mixer(l, hcn, ffn_w1, ffn_w3, ffn_w2, moe_router, moe_w1, moe_w3, moe_w2)
    return h
```

```python
import math
from contextlib import ExitStack

import numpy as np
import concourse.bass as bass
import concourse.mybir as mybir
from concourse.bass_utils import run_bass_kernel_spmd

F32 = mybir.dt.float32
BF16 = mybir.dt.bfloat16
AF = mybir.ActivationFunctionType
ALU = mybir.AluOpType
AX = mybir.AxisListType

D = 4096
KC = D // 128
SEQ = 4096
CTX = 256
NT = SEQ + CTX
DEPTH = 4
NCORES = 4
EPS = 1e-6
GRID_W = 64
IN_W = 7744
NEXP = 8
DFE = 1024

C_SQ, C_SK, C_SV, C_CQ, C_CKV, C_KR, C_DQ, C_DK, C_DV = 0, 1536, 2048, 2560, 3328, 3840, 3904, 5184, 6464

V_N1, V_N2, V_ADAB, V_CQ, V_CKV, V_SQN, V_SKN, V_MQN, V_MKN, V_DQN, V_DKN, V_SUB, V_LAM, V_SINK = (
    0, 32, 64, 256, 262, 266, 267, 268, 270, 272, 274, 276, 278, 282)
NV = 294

GROUPS = [(0, 1024), (1024, 1024), (2048, 1024), (3072, 1280)]
SUBT = [(i * 512, 512) for i in range(8)] + [(4096, 256)]

ENGS = ("sync", "scalar", "vector", "gpsimd", "tensor")
NDS = 40


class Tok:
    __slots__ = ("sem", "val", "dma")

    def __init__(self, sem, val, dma=False):
        self.sem = sem
        self.val = val
        self.dma = dma


class Prog:
    def __init__(self, nc):
        self.nc = nc
        self.esem = {}
        self.ecnt = {}
        for e in ("scalar", "vector", "gpsimd", "tensor"):
            self.esem[e] = nc.alloc_semaphore(name="sem_" + e)
            self.ecnt[e] = 0
        self.dsem = [nc.alloc_semaphore(name="dsem%d" % i) for i in range(NDS)]
        self.dcnt = [0] * NDS
        self.dfree = list(range(NDS))
        self.nph = 0


class Phase:
    def __init__(self, P, name):
        self.P = P
        self.nc = P.nc
        P.nph += 1
        self.name = "%s%d" % (name, P.nph)
        self.tasks = {e: [] for e in ENGS}
        self.stack = ExitStack()
        self.my_ds = []
        self.nt = 0

    def sb(self, name, shape, dt):
        self.nt += 1
        return self.stack.enter_context(self.nc.sbuf_tensor("%s_%s%d" % (self.name, name, self.nt), list(shape), dt))

    def dsem(self):
        i = self.P.dfree.pop()
        self.my_ds.append(i)
        return i

    def op(self, eng, fn, deps=(), signal=True):
        P = self.P
        tok = None
        if signal:
            P.ecnt[eng] += 1
            tok = Tok(P.esem[eng], P.ecnt[eng])
        self.tasks[eng].append((fn, [d for d in deps if d is not None], tok))
        return tok

    def dma(self, out, in_, si, deps=(), q="sync"):
        P = self.P
        P.dcnt[si] += 16
        tok = Tok(P.dsem[si], P.dcnt[si], True)
        self.tasks[q].append((lambda e: e.dma_start(out=out, in_=in_), [d for d in deps if d is not None], tok))
        return tok

    def run(self):
        P = self.P
        finals = [Tok(P.dsem[i], P.dcnt[i], True) for i in self.my_ds]
        self.tasks["sync"].append((None, finals, None))
        with self.nc.Block() as blk:
            for eng in ENGS:
                tasks = self.tasks[eng]
                if not tasks:
                    continue

                def body(e, tasks=tasks):
                    waited = {}
                    for fn, deps, tok in tasks:
                        for d in deps:
                            k = id(d.sem)
                            if waited.get(k, -1) >= d.val:
                                continue
                            e.wait_ge(d.sem, d.val)
                            waited[k] = d.val
                        if fn is None:
                            continue
                        ins = fn(e)
                        if tok is not None:
                            ins.then_inc(tok.sem, 16 if tok.dma else 1)

                getattr(blk, eng)(body)
        self.stack.close()
        P.dfree.extend(self.my_ds)


class Ring:
    def __init__(self, ph, name, n, shape=None, dt=None, dma=False, bufs=None):
        self.n = n
        self.bufs = bufs if bufs is not None else [ph.sb("%s%d" % (name, i), shape, dt) for i in range(n)]
        self.free = [[] for _ in range(n)]
        self.ds = [ph.dsem() for _ in range(n)] if dma else None
        self.i = 0

    def next(self):
        s = self.i % self.n
        self.i += 1
        deps = self.free[s]
        self.free[s] = []
        return s, self.bufs[s], deps

    def release(self, s, tok):
        if tok is not None:
            self.free[s].append(tok)


def nsplits(t0, n):
    out = []
    while n > 0:
        m = min(512, n)
        out.append((t0, m))
        t0 += m
        n -= m
    return out


class EpiCopy:
    def __init__(self, dst, dt=BF16):
        self.dst = dst
        self.dt = dt

    def setup(self, ph, psr):
        self.psr = psr
        self.st = Ring(ph, "epst", 4, [128, 512], self.dt, dma=True)
        self.k = 0

    def __call__(self, ph, t0, n, tiles, petok, uinfo):
        for j, (s, ps, w) in enumerate(tiles):
            ss, stg, deps = self.st.next()
            eng = "scalar" if self.k % 2 == 0 else "vector"
            self.k += 1
            if eng == "scalar":
                tk = ph.op(eng, lambda e, stg=stg, ps=ps, w=w, n=n: e.activation(out=stg[:w, :n], in_=ps[:w, :n], func=AF.Copy),
                           deps=[petok] + deps)
            else:
                tk = ph.op(eng, lambda e, stg=stg, ps=ps, w=w, n=n: e.tensor_copy(out=stg[:w, :n], in_=ps[:w, :n]),
                           deps=[petok] + deps)
            self.psr.release(s, tk)
            dtok = ph.dma(self.dst(t0, n, j, uinfo), stg[:w, :n], self.st.ds[ss], deps=[tk])
            self.st.release(ss, dtok)


class EpiSwiGLU:
    def __init__(self, dst, cb=None):
        self.dst = dst
        self.cb = cb

    def setup(self, ph, psr):
        self.psr = psr
        self.sg = Ring(ph, "sg", 3, [128, 512], F32)
        self.st = Ring(ph, "epst", 3, [128, 512], BF16, dma=True)
        if self.cb is not None:
            self.cbr = Ring(ph, "cbr", 3, [128, 512], F32, dma=True)
            self.t2 = Ring(ph, "t2", 2, [128, 512], F32)

    def __call__(self, ph, t0, n, tiles, petok, uinfo):
        (sa, pa, w), (sb_, pb, _) = tiles
        s1, sg, d1 = self.sg.next()
        t1 = ph.op("scalar", lambda e: e.activation(out=sg[:w, :n], in_=pa[:w, :n], func=AF.Silu), deps=[petok] + d1)
        self.psr.release(sa, t1)
        ss, stg, d2 = self.st.next()
        if self.cb is None:
            t2 = ph.op("vector", lambda e: e.tensor_tensor(out=stg[:w, :n], in0=pb[:w, :n], in1=sg[:w, :n], op=ALU.mult),
                       deps=[petok, t1] + d2)
            self.psr.release(sb_, t2)
            self.sg.release(s1, t2)
        else:
            sc, cbt, d3 = self.cbr.next()
            tc = ph.dma(cbt[:w, :n], self.cb(t0, n, uinfo), self.cbr.ds[sc], deps=d3)
            sx, tx, d4 = self.t2.next()
            ta = ph.op("vector", lambda e: e.tensor_tensor(out=tx[:w, :n], in0=pb[:w, :n], in1=sg[:w, :n], op=ALU.mult),
                       deps=[petok, t1] + d4)
            self.psr.release(sb_, ta)
            self.sg.release(s1, ta)
            t2 = ph.op("gpsimd", lambda e: e.tensor_tensor(out=stg[:w, :n], in0=tx[:w, :n], in1=cbt[:w, :n], op=ALU.mult),
                       deps=[ta, tc] + d2)
            self.t2.release(sx, t2)
            self.cbr.release(sc, t2)
        dtok = ph.dma(self.dst(t0, n, 0, uinfo), stg[:w, :n], self.st.ds[ss], deps=[t2])
        self.st.release(ss, dtok)


class EpiResid:
    def __init__(self, hT, gcol):
        self.hT = hT
        self.gcol = gcol

    def setup(self, ph, psr):
        self.psr = psr
        self.hin = Ring(ph, "hin", 3, [128, 512], F32, dma=True)
        self.hout = Ring(ph, "hout", 3, [128, 512], F32, dma=True)

    def __call__(self, ph, t0, n, tiles, petok, uinfo):
        (s, ps, w), = tiles
        f0 = uinfo["f0"]
        si, hin, d1 = self.hin.next()
        tl = ph.dma(hin[:w, :n], self.hT[f0:f0 + w, t0:t0 + n], self.hin.ds[si], deps=d1)
        so, hout, d2 = self.hout.next()
        g = self.gcol(f0 // 128, t0 >= SEQ)
        tk = ph.op("vector", lambda e: e.scalar_tensor_tensor(out=hout[:w, :n], in0=ps[:w, :n], scalar=g, in1=hin[:w, :n],
                                                                 op0=ALU.mult, op1=ALU.add), deps=[petok, tl] + d2)
        self.psr.release(s, tk)
        self.hin.release(si, tk)
        dtok = ph.dma(self.hT[f0:f0 + w, t0:t0 + n], hout[:w, :n], self.hout.ds[so], deps=[tk])
        self.hout.release(so, dtok)


class EpiMod:
    def __init__(self, modsb, bcol):
        self.modsb = modsb
        self.bcol = bcol

    def setup(self, ph, psr):
        self.psr = psr

    def __call__(self, ph, t0, n, tiles, petok, uinfo):
        (s, ps, w), = tiles
        fc = uinfo["f0"] // 128
        tk = ph.op("vector", lambda e: e.tensor_scalar(out=self.modsb[:, fc, 0:2], in0=ps[:, 0:2], scalar1=self.bcol(fc),
                                                        scalar2=None, op0=ALU.add), deps=[petok])
        self.psr.release(s, tk)


def linear(P, name, K, groups, blocks, xT=None, x_sb=None, cast_engs=("gpsimd", "vector", "gpsimd", "scalar"), stgcfg=(3, 1024)):
    ph = Phase(P, name)
    KCn = K // 128
    gmax = max(g[1] for g in groups)
    psr = Ring(ph, "ps", 8, bufs=P.psb)
    epis = []
    for b in blocks:
        for u in b["units"]:
            if u["epi"] not in epis:
                epis.append(u["epi"])
    for ep in epis:
        ep.setup(ph, psr)
    if x_sb is None:
        X = ph.sb("X", [128, KCn, gmax], BF16)
        xds = ph.dsem()
    else:
        X = x_sb
    Wr = Ring(ph, "Wb", 2, [128, KCn, 512], BF16)
    Sr = Ring(ph, "Ws", stgcfg[0], [128, stgcfg[1]], F32, dma=True)

    steps = [(g, b) for g in range(len(groups)) for b in range(len(blocks))]
    xfree = []
    xtok = {}
    loaded = {}
    ncast = [0]

    def load_w(step):
        g, bi = steps[step]
        blk = blocks[bi]
        ws, Wb, wdeps = Wr.next()
        off = 0
        lasts = {}
        for (W, c0, w) in blk["segs"]:
            Wv = W.rearrange("(kc p) f -> p kc f", p=128)
            kcs = max(1, stgcfg[1] // w)
            k0 = 0
            while k0 < KCn:
                kn = min(kcs, KCn - k0)
                ss, stg, sdeps = Sr.next()
                sv = stg[:, 0:kn * w].rearrange("p (k w) -> p k w", w=w)
                td = ph.dma(sv, Wv[:, k0:k0 + kn, c0:c0 + w], Sr.ds[ss], deps=sdeps)
                ceng = cast_engs[ncast[0] % len(cast_engs)]
                ncast[0] += 1
                if ceng == "scalar":
                    tc = ph.op(ceng, lambda e, Wb=Wb, k0=k0, kn=kn, off=off, w=w, sv=sv: e.activation(
                        out=Wb[:, k0:k0 + kn, off:off + w], in_=sv, func=AF.Copy), deps=[td] + wdeps)
                else:
                    tc = ph.op(ceng, lambda e, Wb=Wb, k0=k0, kn=kn, off=off, w=w, sv=sv: e.tensor_copy(
                        out=Wb[:, k0:k0 + kn, off:off + w], in_=sv), deps=[td] + wdeps)
                Sr.release(ss, tc)
                lasts[ceng] = tc
                k0 += kn
            off += w
        loaded[step] = (ws, Wb, list(lasts.values()))

    load_w(0)
    for step, (g, bi) in enumerate(steps):
        g0, gn = groups[g]
        if bi == 0 and x_sb is None:
            xt = None
            for q in range(0, KCn, 8):
                qn = min(8, KCn - q)
                xt = ph.dma(X[:, q:q + qn, 0:gn], xT.rearrange("(kc p) t -> p kc t", p=128)[:, q:q + qn, g0:g0 + gn], xds,
                            deps=xfree if q == 0 else [])
            xfree = []
            xtok[g] = xt
        if step + 1 < len(steps):
            load_w(step + 1)
        ws, Wb, wtoks = loaded.pop(step)
        first_deps = wtoks + ([xtok[g]] if x_sb is None else [])
        lastpe = None
        for u in blocks[bi]["units"]:
            if u["mode"] == "F":
                for (t0, n) in nsplits(g0, gn):
                    tiles = []
                    pet = None
                    for (off, w) in u["chunks"]:
                        s, ps, pdeps = psr.next()
                        for kc in range(KCn):
                            lastk = kc == KCn - 1
                            pet = ph.op("tensor", lambda e, ps=ps, Wb=Wb, kc=kc, off=off, w=w, t0=t0, n=n, g0=g0, lastk=lastk:
                                        e.matmul(ps[:w, :n], lhsT=Wb[:, kc, off:off + w], rhs=X[:, kc, t0 - g0:t0 - g0 + n],
                                                 start=(kc == 0), stop=lastk),
                                        deps=(pdeps + first_deps) if kc == 0 else (), signal=lastk)
                        tiles.append((s, ps, w))
                    lastpe = pet
                    u["epi"](ph, t0, n, tiles, pet, u)
            else:
                (off, w), = u["chunks"]
                for tt in range(g0, g0 + gn, 128):
                    s, ps, pdeps = psr.next()
                    pet = None
                    for kc in range(KCn):
                        lastk = kc == KCn - 1
                        pet = ph.op("tensor", lambda e, ps=ps, Wb=Wb, kc=kc, off=off, w=w, tt=tt, g0=g0, lastk=lastk:
                                    e.matmul(ps[:, :w], lhsT=X[:, kc, tt - g0:tt - g0 + 128], rhs=Wb[:, kc, off:off + w],
                                             start=(kc == 0), stop=lastk),
                                    deps=(pdeps + first_deps) if kc == 0 else (), signal=lastk)
                    lastpe = pet
                    u["epi"](ph, tt, w, [(s, ps, 128)], pet, u)
        Wr.release(ws, lastpe)
        if bi == len(blocks) - 1:
            xfree = [lastpe]
    ph.run()


def fblocks(W, c0, c1, epi, base_f0=None, extra=None):
    out = []
    c = c0
    while c < c1:
        bw = min(512, c1 - c)
        units = []
        o = 0
        while o < bw:
            w = min(128, bw - o)
            u = dict(mode="F", chunks=[(o, w)], epi=epi, f0=(c + o) if base_f0 is None else base_f0 + (c + o - c0))
            if extra:
                u.update(extra)
            units.append(u)
            o += w
        out.append(dict(segs=[(W, c, bw)], units=units))
        c += bw
    return out


def tblocks(W, c0, c1, epi, dcol0=0):
    out = []
    c = c0
    while c < c1:
        bw = min(512, c1 - c)
        out.append(dict(segs=[(W, c, bw)], units=[dict(mode="T", chunks=[(0, bw)], epi=epi, f0=dcol0 + c - c0)]))
        c += bw
    return out


class Ctx:
    pass


def init_phase(P, C):
    ph = Phase(P, "init")
    ds = ph.dsem()
    for q in range(0, D, 512):
        ph.dma(C.hT[q:q + 512, :], C.xT[q:q + 512, :], ds)
    cv = ph.sb("cv", [128, KC, 2], F32)
    d2 = ph.dsem()
    t = ph.dma(cv[:], C.cvec, d2)
    ph.op("scalar", lambda e: e.activation(out=C.scsb[:], in_=cv[:], func=AF.Silu), deps=[t])
    d3 = ph.dsem()
    sts = []
    t = None
    for (dst, src) in ((C.ones_bf, C.c_ones), (C.r128, C.c_r128), (C.r64, C.c_r64)):
        st = ph.sb("cst", list(src.shape), F32)
        t = ph.dma(st[:], src, d3)
        sts.append((dst, st))
    for (dst, st) in sts:
        ph.op("vector", lambda e, dst=dst, st=st: e.tensor_copy(out=dst[:], in_=st[:]), deps=[t])
    d4 = ph.dsem()
    ph.dma(C.ident[:], C.c_ident, d4)
    ph.dma(C.sel[:], C.c_sel, d4)
    ph.dma(C.masks[:], C.c_masks, d4)
    ph.run()


def layer_vec_phase(P, C, l):
    ph = Phase(P, "vec")
    ds = ph.dsem()
    t = ph.dma(C.vsb[:], C.vecs[l], ds)
    lam_init = 0.8 - 0.6 * math.exp(-0.3 * l)
    pr = ph.sb("pr", [128, 2], F32)
    t1 = ph.op("vector", lambda e: e.tensor_tensor(out=pr[:, 0:1], in0=C.vsb[:, V_LAM:V_LAM + 1], in1=C.vsb[:, V_LAM + 1:V_LAM + 2],
                                                    op=ALU.mult), deps=[t])
    t2 = ph.op("vector", lambda e: e.tensor_tensor(out=pr[:, 1:2], in0=C.vsb[:, V_LAM + 2:V_LAM + 3], in1=C.vsb[:, V_LAM + 3:V_LAM + 4],
                                                    op=ALU.mult), deps=[t, t1])
    ps = P.psb[0]
    t3 = ph.op("tensor", lambda e: e.matmul(ps[:, 0:2], lhsT=C.ones32[:], rhs=pr[:, 0:2], start=True, stop=True), deps=[t2])
    ex = ph.sb("ex", [128, 2], F32)
    t4 = ph.op("scalar", lambda e: e.activation(out=ex[:], in_=ps[:, 0:2], func=AF.Exp), deps=[t3])
    t5 = ph.op("vector", lambda e: e.tensor_tensor(out=C.lam[:, 0:1], in0=ex[:, 0:1], in1=ex[:, 1:2], op=ALU.subtract), deps=[t4])
    t6 = ph.op("vector", lambda e: e.tensor_scalar(out=C.lam[:, 0:1], in0=C.lam[:, 0:1], scalar1=lam_init, scalar2=None, op0=ALU.add),
               deps=[t5])
    t7 = ph.op("vector", lambda e: e.tensor_scalar(out=C.lam[:, 1:2], in0=C.lam[:, 0:1], scalar1=-1.0, scalar2=None, op0=ALU.mult),
               deps=[t6])
    ph.op("scalar", lambda e: e.activation(out=C.esink[:], in_=C.vsb[:, V_SINK:V_SINK + 12], func=AF.Exp), deps=[t, t4])
    ph.op("vector", lambda e: e.tensor_scalar(out=C.subg[:], in0=C.vsb[:, V_SUB:V_SUB + 2], scalar1=1.0 - lam_init, scalar2=None,
                                              op0=ALU.mult), deps=[t, t7])
    ph.run()


def mod_phase(P, C, l):
    epi = EpiMod(C.modsb, lambda fc: C.vsb[:, V_ADAB + fc:V_ADAB + fc + 1])
    blocks = fblocks(C.ada_w[l], 0, 6 * D, epi)
    linear(P, "mod", D, [(0, 2)], blocks, x_sb=C.scsb, cast_engs=("gpsimd", "vector", "scalar"), stgcfg=(8, 2048))


def norm_phase(P, C, l, which):
    ph = Phase(P, "norm")
    gcol = V_N1 if which == 1 else V_N2
    sh_i, sc_i = (0, 1) if which == 1 else (3, 4)
    AB = ph.sb("AB", [128, 4, KC], F32)
    abt = None
    for ci in (0, 1):
        t = ph.op("vector", lambda e, ci=ci: e.tensor_scalar(out=AB[:, 2 * ci, :], in0=C.modsb[:, sc_i * KC:(sc_i + 1) * KC, ci],
                                                               scalar1=1.0, scalar2=None, op0=ALU.add), deps=[abt])
        t = ph.op("vector", lambda e, ci=ci: e.tensor_tensor(out=AB[:, 2 * ci, :], in0=AB[:, 2 * ci, :], in1=C.vsb[:, gcol:gcol + KC],
                                                               op=ALU.mult), deps=[t])
        abt = ph.op("vector", lambda e, ci=ci: e.tensor_copy(out=AB[:, 2 * ci + 1, :], in_=C.modsb[:, sh_i * KC:(sh_i + 1) * KC, ci]),
                    deps=[t])
    hr = Ring(ph, "hb", 2, [128, KC, 512], F32)
    hsem = [[ph.dsem() for _ in range(4)] for _ in range(2)]
    sqr = Ring(ph, "sq", 3, [128, 8, 512], BF16)
    rsr = Ring(ph, "rs", 3, [128, 512], F32)
    tmr = Ring(ph, "tm", 4, [128, 512], F32)
    osr = Ring(ph, "os", 3, [128, 8, 512], BF16, dma=True)
    psr = Ring(ph, "ps", 8, bufs=P.psb)
    hv = C.hT.rearrange("(kc p) t -> p kc t", p=128)
    xv = C.xnT.rearrange("(kc p) t -> p kc t", p=128)

    def stage_a(t0, n):
        hs, hb, hdeps = hr.next()
        s, ps, pdeps = psr.next()
        ltoks = []
        pet = None
        for q in range(0, KC, 8):
            lt = ph.dma(hb[:, q:q + 8, :n], hv[:, q:q + 8, t0:t0 + n], hsem[hs][q // 8], deps=hdeps)
            ltoks.append(lt)
        for q in range(0, KC, 8):
            ss, sq, sdeps = sqr.next()
            ta = ph.op("scalar", lambda e, sq=sq, hb=hb, q=q, n=n: e.activation(out=sq[:, :, :n], in_=hb[:, q:q + 8, :n], func=AF.Square),
                       deps=[ltoks[q // 8]] + sdeps)
            for j in range(8):
                lastk = (q + j == KC - 1)
                pet = ph.op("tensor", lambda e, ps=ps, sq=sq, j=j, n=n, q=q, lastk=lastk: e.matmul(
                    ps[:, :n], lhsT=C.ones_bf[:], rhs=sq[:, j, :n], start=(q + j == 0), stop=lastk),
                    deps=([ta] + (pdeps if q == 0 else [])) if j == 0 else (), signal=(j == 7))
            sqr.release(ss, pet)
        rs_, rs, rdeps = rsr.next()
        t1 = ph.op("scalar", lambda e, rs=rs, ps=ps, n=n: e.activation(out=rs[:, :n], in_=ps[:, :n], func=AF.Sqrt, bias=C.epsc[:, 0:1],
                                                                      scale=1.0 / D), deps=[pet] + rdeps)
        psr.release(s, t1)
        t2 = ph.op("vector", lambda e, rs=rs, n=n: e.reciprocal(out=rs[:, :n], in_=rs[:, :n]), deps=[t1])
        return (t0, n, hs, hb, ltoks, rs_, rs, t2)

    def stage_b(info):
        t0, n, hs, hb, ltoks, rs_, rs, t2 = info
        ci = 1 if t0 >= SEQ else 0
        lastact = None
        for q in range(0, KC, 8):
            os_, ost, odeps = osr.next()
            for j in range(8):
                kc = q + j
                ts_, tm, tdeps = tmr.next()
                tv = ph.op("vector", lambda e, tm=tm, hb=hb, kc=kc, rs=rs, n=n: e.tensor_tensor(out=tm[:, :n], in0=hb[:, kc, :n],
                                                                                              in1=rs[:, :n], op=ALU.mult),
                           deps=[t2, ltoks[q // 8]] + tdeps)
                lastact = ph.op("scalar", lambda e, ost=ost, j=j, tm=tm, kc=kc, n=n, ci=ci: e.activation(
                    out=ost[:, j, :n], in_=tm[:, :n], func=AF.Identity, bias=AB[:, 2 * ci + 1, kc:kc + 1],
                    scale=AB[:, 2 * ci, kc:kc + 1]), deps=[tv, abt] + (odeps if j == 0 else []))
                tmr.release(ts_, lastact)
            dt_ = ph.dma(xv[:, q:q + 8, t0:t0 + n], ost[:, :, :n], osr.ds[os_], deps=[lastact])
            osr.release(os_, dt_)
        hr.release(hs, lastact)
        rsr.release(rs_, lastact)

    infos = [stage_a(*SUBT[0])]
    for i in range(len(SUBT)):
        if i + 1 < len(SUBT):
            infos.append(stage_a(*SUBT[i + 1]))
        stage_b(infos[i])
    ph.run()


def rmsrope_phase(P, C, items):
    ph = Phase(P, "rr")
    tabH = Ring(ph, "tabH", 2, [128, 2, 512], F32, dma=True)
    tabM = Ring(ph, "tabM", 2, [64, 2, 512], F32, dma=True)
    zr = Ring(ph, "z", 16, [128, 512], BF16, dma=True)
    sqr = Ring(ph, "sq", 8, [128, 512], BF16)
    rsr = Ring(ph, "rs", 6, [128, 512], F32)
    znr = Ring(ph, "zn", 6, [128, 512], BF16)
    t1r = Ring(ph, "t1", 6, [128, 512], F32)
    t2r = Ring(ph, "t2", 6, [128, 512], F32)
    osr = Ring(ph, "os", 16, [128, 512], BF16, dma=True)
    psr = Ring(ph, "ps", 8, bufs=P.psb)
    work = [(si, it) for si in range(len(SUBT)) for it in items]
    tabs = {}
    st = {}

    def stage_a(u):
        si, it = work[u]
        t0, n = SUBT[si]
        if si not in tabs:
            hs, tH, hd = tabH.next()
            tokH = ph.dma(tH[:, :, :n], C.ropeH.rearrange("c p t -> p c t")[:, :, t0:t0 + n], tabH.ds[hs], deps=hd)
            ms, tM, md = tabM.next()
            tokM = ph.dma(tM[:, :, :n], C.ropeM.rearrange("c p t -> p c t")[:, :, t0:t0 + n], tabM.ds[ms], deps=md)
            tabs[si] = (hs, tH, tokH, ms, tM, tokM)
        nch = len(it["src"])
        s, ps, pdeps = psr.next()
        zs = []
        pet = None
        for j, (src, w) in enumerate(it["src"]):
            zs_, z, zd = zr.next()
            lt = ph.dma(z[:w, :n], src[:, t0:t0 + n], zr.ds[zs_], deps=zd)
            ss, sq, sd = sqr.next()
            ta = ph.op("scalar", lambda e, sq=sq, z=z, w=w, n=n: e.activation(out=sq[:w, :n], in_=z[:w, :n], func=AF.Square),
                       deps=[lt] + sd)
            pet = ph.op("tensor", lambda e, ps=ps, sq=sq, w=w, n=n, j=j, nch=nch: e.matmul(
                ps[:, :n], lhsT=C.ones_bf[:w, :], rhs=sq[:w, :n], start=(j == 0), stop=(j == nch - 1)),
                deps=[ta] + (pdeps if j == 0 else []))
            sqr.release(ss, pet)
            zs.append((zs_, z, lt, w))
        st[u] = dict(s=s, ps=ps, pet=pet, zs=zs)

    def stage_b(u):
        si, it = work[u]
        t0, n = SUBT[si]
        d = st[u]
        ps, pet = d["ps"], d["pet"]
        rs_, rs, rd = rsr.next()
        tq = ph.op("scalar", lambda e, rs=rs, ps=ps, n=n, dim=it["dim"]: e.activation(
            out=rs[:, :n], in_=ps[:, :n], func=AF.Ln, bias=C.epsc[:, 0:1], scale=1.0 / dim), deps=[pet] + rd)
        psr.release(d["s"], tq)
        tr = ph.op("scalar", lambda e, rs=rs, n=n: e.activation(out=rs[:, :n], in_=rs[:, :n], func=AF.Exp, scale=-0.5), deps=[tq])
        d["rs"] = (rs_, rs)
        d["cwork"] = []
        lastu = tr
        for j, (zs_, z, lt, w) in enumerate(d["zs"]):
            g = it["g"][j]
            rope = it["rope"][j]
            os_, ost, od = osr.next()
            if rope is None:
                tn = ph.op("vector", lambda e, ost=ost, z=z, g=g, rs=rs, w=w, n=n: e.scalar_tensor_tensor(
                    out=ost[:w, :n], in0=z[:w, :n], scalar=g, in1=rs[:w, :n], op0=ALU.mult, op1=ALU.mult), deps=[tr, lt] + od)
                zr.release(zs_, tn)
                dtk = ph.dma(it["dst"][j][:, t0:t0 + n], ost[:w, :n], osr.ds[os_], deps=[tn])
                osr.release(os_, dtk)
                lastu = tn
            else:
                ns_, zn, nd = znr.next()
                tn = ph.op("vector", lambda e, zn=zn, z=z, g=g, rs=rs, w=w, n=n: e.scalar_tensor_tensor(
                    out=zn[:w, :n], in0=z[:w, :n], scalar=g, in1=rs[:w, :n], op0=ALU.mult, op1=ALU.mult), deps=[tr, lt] + nd)
                zr.release(zs_, tn)
                s2, ps2, pd2 = psr.next()
                Rm = C.r128 if rope == "H" else C.r64
                tp = ph.op("tensor", lambda e, ps2=ps2, Rm=Rm, zn=zn, w=w, n=n: e.matmul(
                    ps2[:w, :n], lhsT=Rm[:w, :w], rhs=zn[:w, :n], start=True, stop=True), deps=[tn] + pd2)
                d["cwork"].append((j, w, rope, os_, ost, od, ns_, zn, tn, s2, ps2, tp))
                lastu = tn
        d["lastu"] = lastu

    def stage_c(u):
        si, it = work[u]
        t0, n = SUBT[si]
        d = st.pop(u)
        hs, tH, tokH, ms, tM, tokM = tabs[si]
        lastu = d["lastu"]
        for (j, w, rope, os_, ost, od, ns_, zn, tn, s2, ps2, tp) in d["cwork"]:
            tab, ttok = (tH, tokH) if rope == "H" else (tM, tokM)
            a_, t1, ad = t1r.next()
            ta1 = ph.op("gpsimd", lambda e, t1=t1, zn=zn, tab=tab, w=w, n=n: e.tensor_tensor(
                out=t1[:w, :n], in0=zn[:w, :n], in1=tab[:w, 0, :n], op=ALU.mult), deps=[tn, ttok] + ad)
            b_, t2, bd = t2r.next()
            ta2 = ph.op("vector", lambda e, t2=t2, ps2=ps2, tab=tab, w=w, n=n: e.tensor_tensor(
                out=t2[:w, :n], in0=ps2[:w, :n], in1=tab[:w, 1, :n], op=ALU.mult), deps=[tp, ttok] + bd)
            psr.release(s2, ta2)
            fin = ph.op("gpsimd", lambda e, ost=ost, t1=t1, t2=t2, w=w, n=n: e.tensor_tensor(
                out=ost[:w, :n], in0=t1[:w, :n], in1=t2[:w, :n], op=ALU.add), deps=[ta1, ta2] + od)
            znr.release(ns_, fin)
            t1r.release(a_, fin)
            t2r.release(b_, fin)
            dtk = ph.dma(it["dst"][j][:, t0:t0 + n], ost[:w, :n], osr.ds[os_], deps=[fin])
            osr.release(os_, dtk)
            lastu = fin
        rs_, rs = d["rs"]
        rsr.release(rs_, lastu)
        if u + 1 == len(work) or work[u + 1][0] != si:
            tabH.release(hs, lastu)
            tabM.release(ms, lastu)

    nw = len(work)
    for tau in range(nw + 2):
        if tau < nw:
            stage_a(tau)
        if 0 <= tau - 1 < nw:
            stage_b(tau - 1)
        if 0 <= tau - 2 < nw:
            stage_c(tau - 2)
    ph.run()


def attn_phase(P, C, vheads):
    ph = Phase(P, "attn")
    NKT = NT // 128
    k128 = Ring(ph, "k128", 2, [128, NT], BF16, dma=True)
    k64 = Ring(ph, "k64", 2, [64, NT], BF16, dma=True)
    vr = Ring(ph, "v", 2, [128, NKT, 256], BF16, dma=True)
    q128 = Ring(ph, "q128", 2, [128, 512], BF16, dma=True)
    q64 = Ring(ph, "q64", 2, [64, 512], BF16, dma=True)
    pr = Ring(ph, "p", 4, [128, 512], BF16)
    rvr = Ring(ph, "rv", 2, [128, 512], F32)
    accR = Ring(ph, "acc", 4, bufs=P.psb[0:4])
    sR = Ring(ph, "s", 4, bufs=P.psb[4:8])
    saccR = Ring(ph, "sacc", 2, [128, 512], F32)
    ostb = Ring(ph, "ob", 3, [128, 512], BF16, dma=True)
    ostf = Ring(ph, "of", 3, [128, 512], F32, dma=True)
    for vh in vheads:
        ew = vh["ew"]
        nec = ew // 128
        kparts = []
        for (src, w) in vh["k"]:
            ring = k128 if w == 128 else k64
            ks, kb, kd = ring.next()
            tk = None
            for q in range(0, NT, 1088):
                tk = ph.dma(kb[:w, q:q + 1088], src[:, q:q + 1088], ring.ds[ks], deps=kd if q == 0 else [])
            kparts.append((ring, ks, kb, tk, w))
        vs, vb, vd = vr.next()
        tv = None
        Vv = vh["V"].rearrange("(kt p) e -> p kt e", p=128)
        for q in range(0, NKT, 17):
            tv = ph.dma(vb[:, q:q + 17, :ew], Vv[:, q:q + 17, :], vr.ds[vs], deps=vd if q == 0 else [])
        lastpe = None
        for (t0, n) in SUBT:
            qparts = []
            for (src, w) in vh["q"]:
                ring = q128 if w == 128 else q64
                qs, qb, qd = ring.next()
                tq = ph.dma(qb[:w, :n], src[:, t0:t0 + n], ring.ds[qs], deps=qd)
                qparts.append((ring, qs, qb, tq, w))
            kts = list(range(NKT)) if t0 < SEQ else [NKT - 2, NKT - 1]
            accs = [accR.next() for _ in range(nec + 1)]
            sa_, sacc, sad = saccR.next()
            tacc = [None]

            def emit_s(kt):
                s, ps, pd = sR.next()
                pet = None
                npart = len(kparts)
                for j in range(npart):
                    _, _, kb, tk, w = kparts[j]
                    _, _, qb, tq, _ = qparts[j]
                    pet = ph.op("tensor", lambda e, ps=ps, kb=kb, qb=qb, w=w, kt=kt, n=n, j=j, npart=npart: e.matmul(
                        ps[:, :n], lhsT=kb[:w, kt * 128:(kt + 1) * 128], rhs=qb[:w, :n], start=(j == 0), stop=(j == npart - 1)),
                        deps=([tk, tq] + (pd if j == 0 else [])), signal=(j == npart - 1))
                p_, pb, ppd = pr.next()
                ta = ph.op("scalar", lambda e, pb=pb, ps=ps, n=n, sc=vh["scale"]: e.activation(out=pb[:, :n], in_=ps[:, :n], func=AF.Exp,
                                                                                              scale=sc), deps=[pet] + ppd)
                sR.release(s, ta)
                return (p_, pb, ta)

            def emit_pv(kt, first, last, pinfo):
                p_, pb, ta = pinfo
                pet = None
                for ec in range(nec):
                    a_, acc, ad = accs[ec]
                    lhs = vb[:, kt, ec * 128:(ec + 1) * 128]
                    pet = ph.op("tensor", lambda e, acc=acc, lhs=lhs, pb=pb, n=n, first=first, last=last: e.matmul(
                        acc[:, :n], lhsT=lhs, rhs=pb[:, :n], start=first, stop=last),
                        deps=[ta, tv] + (ad if first else []), signal=(ec == nec - 1))
                if first:
                    tacc[0] = ph.op("vector", lambda e, sacc=sacc, pb=pb, n=n: e.tensor_copy(out=sacc[:, :n], in_=pb[:, :n]),
                                    deps=[ta] + sad)
                else:
                    tacc[0] = ph.op("vector", lambda e, sacc=sacc, pb=pb, n=n: e.tensor_tensor(out=sacc[:, :n], in0=sacc[:, :n],
                                                                                             in1=pb[:, :n], op=ALU.add),
                                    deps=[ta, tacc[0]])
                pr.release(p_, pet)
                pr.release(p_, tacc[0])
                return pet

            pend = emit_s(kts[0])
            for i, kt in enumerate(kts):
                nxt = emit_s(kts[i + 1]) if i + 1 < len(kts) else None
                lastpe = emit_pv(kt, i == 0, i == len(kts) - 1, pend)
                pend = nxt
            for (ring, qs, qb, tq, w) in qparts:
                ring.release(qs, lastpe)
            r_, rv, rd = rvr.next()
            a_, accs_, asd = accs[nec]
            tsum = ph.op("tensor", lambda e, accs_=accs_, sacc=sacc, n=n: e.matmul(accs_[:, :n], lhsT=C.ones32[:], rhs=sacc[:, :n],
                                                                                start=True, stop=True), deps=[tacc[0]] + asd)
            saccR.release(sa_, tsum)
            t1 = ph.op("vector", lambda e, rv=rv, accs_=accs_, n=n: e.reciprocal(out=rv[:, :n], in_=accs_[:, :n]), deps=[tsum] + rd)
            accR.release(a_, t1)
            lt = None
            for ec in range(nec):
                a_, acc, _ = accs[ec]
                oring = ostb if vh["dt"] == BF16 else ostf
                o_, ob, od = oring.next()
                lt = ph.op("vector", lambda e, ob=ob, acc=acc, rv=rv, n=n: e.tensor_tensor(out=ob[:, :n], in0=acc[:, :n], in1=rv[:, :n],
                                                                                         op=ALU.mult), deps=[t1, lastpe] + od)
                accR.release(a_, lt)
                dk = ph.dma(vh["dst"](ec)[:, t0:t0 + n], ob[:, :n], oring.ds[o_], deps=[lt])
                oring.release(o_, dk)
            rvr.release(r_, lt)
        for (ring, ks, kb, tk, w) in kparts:
            ring.release(ks, lastpe)
        vr.release(vs, lastpe)
    ph.run()


def swa_phase(P, C):
    ph = Phase(P, "swa")
    NKT = NT // 128
    NB = SEQ // 128
    scale = 128 ** -0.5
    kr = Ring(ph, "k", 2, [128, NT], BF16, dma=True)
    vr = Ring(ph, "v", 2, [128, NKT, 128], BF16, dma=True)
    qr = Ring(ph, "q", 2, [128, 3, NT], BF16, dma=True)
    pr = Ring(ph, "p", 6, [128, 384], BF16)
    dnr = Ring(ph, "dn", 3, [128, 128], F32)
    osr = Ring(ph, "os", 4, [128, 128], BF16, dma=True)
    accR = Ring(ph, "acc", 4, bufs=P.psb[0:4])
    sR = Ring(ph, "s", 4, bufs=P.psb[4:8])
    for g in range(4):
        ks, kb, kd = kr.next()
        tk = None
        for q in range(0, NT, 1088):
            tk = ph.dma(kb[:, q:q + 1088], C.skT[g * 128:(g + 1) * 128, q:q + 1088], kr.ds[ks], deps=kd if q == 0 else [])
        vs, vb, vd = vr.next()
        tv = None
        Vv = C.svV[:, g * 128:(g + 1) * 128].rearrange("(kt p) e -> p kt e", p=128)
        for q in range(0, NKT, 17):
            tv = ph.dma(vb[:, q:q + 17, :], Vv[:, q:q + 17, :], vr.ds[vs], deps=vd if q == 0 else [])
        qs, qb, qd = qr.next()
        tq = None
        for r in range(3):
            h = 3 * g + r
            tq = ph.dma(qb[:, r, :], C.sqT[h * 128:(h + 1) * 128, :], qr.ds[qs], deps=qd if r == 0 else [])
        lastpe = None
        for nb in range(NKT):
            if nb < NB:
                kts = []
                if nb > 0:
                    kts.append((nb - 1, 0))
                kts.append((nb, None))
                if nb < NB - 1:
                    kts.append((nb + 1, 1))
                kts += [(NKT - 2, None), (NKT - 1, None)]
            else:
                kts = [(NKT - 2, None), (NKT - 1, None)]
            a0, acc_o, ad0 = accR.next()
            a1, acc_s, ad1 = accR.next()
            pinfos = []
            for (kt, mk) in kts:
                s, ps, pd = sR.next()
                pet = ph.op("tensor", lambda e, ps=ps, kb=kb, qb=qb, kt=kt, nb=nb: e.matmul(
                    ps[:, 0:384].rearrange("p (r q) -> p r q", r=3), lhsT=kb[:, kt * 128:(kt + 1) * 128],
                    rhs=qb[:, :, nb * 128:(nb + 1) * 128], start=True, stop=True), deps=[tk, tq] + pd)
                p_, pb, ppd = pr.next()
                ta = ph.op("scalar", lambda e, pb=pb, ps=ps: e.activation(out=pb[:, :], in_=ps[:, 0:384], func=AF.Exp, scale=scale),
                           deps=[pet] + ppd)
                sR.release(s, ta)
                if mk is not None:
                    ta = ph.op("gpsimd", lambda e, pb=pb, mk=mk: e.tensor_tensor(out=pb[:, :], in0=pb[:, :], in1=C.masks[:, mk, :],
                                                                                 op=ALU.mult), deps=[ta])
                pinfos.append((p_, pb, ta, kt))
            for i, (p_, pb, ta, kt) in enumerate(pinfos):
                first = i == 0
                last = i == len(pinfos) - 1
                ph.op("tensor", lambda e, acc_o=acc_o, vb=vb, kt=kt, pb=pb, first=first, last=last: e.matmul(
                    acc_o[:, 0:384], lhsT=vb[:, kt, :], rhs=pb[:, :], start=first, stop=last),
                    deps=[ta, tv] + (ad0 if first else []), signal=False)
                lastpe = ph.op("tensor", lambda e, acc_s=acc_s, pb=pb, first=first, last=last: e.matmul(
                    acc_s[:, 0:384], lhsT=C.ones_bf[:], rhs=pb[:, :], start=first, stop=last),
                    deps=(ad1 if first else []))
                pr.release(p_, lastpe)
            lt = None
            for r in range(3):
                h = 3 * g + r
                d_, dn, dd = dnr.next()
                t1 = ph.op("vector", lambda e, dn=dn, acc_s=acc_s, r=r, h=h: e.tensor_scalar(
                    out=dn[:, :], in0=acc_s[:, r * 128:(r + 1) * 128], scalar1=C.esink[:, h:h + 1], scalar2=None, op0=ALU.add),
                    deps=[lastpe] + dd)
                t2 = ph.op("vector", lambda e, dn=dn: e.reciprocal(out=dn[:, :], in_=dn[:, :]), deps=[t1])
                o_, ob, od = osr.next()
                lt = ph.op("vector", lambda e, ob=ob, acc_o=acc_o, dn=dn, r=r: e.tensor_tensor(
                    out=ob[:, :], in0=acc_o[:, r * 128:(r + 1) * 128], in1=dn[:, :], op=ALU.mult), deps=[t2] + od)
                dnr.release(d_, lt)
                dk = ph.dma(C.yT[h * 128:(h + 1) * 128, nb * 128:(nb + 1) * 128], ob[:, :], osr.ds[o_], deps=[lt])
                osr.release(o_, dk)
            accR.release(a0, lt)
            accR.release(a1, lt)
        kr.release(ks, lastpe)
        vr.release(vs, lastpe)
        qr.release(qs, lastpe)
    ph.run()


def dif_merge_phase(P, C):
    ph = Phase(P, "dmrg")
    yr = Ring(ph, "y", 8, [128, 512], F32, dma=True)
    ydr = Ring(ph, "yd", 4, [128, 512], F32)
    sqr = Ring(ph, "sq", 4, [128, 512], BF16)
    rsr = Ring(ph, "rs", 2, [128, 512], F32)
    osr = Ring(ph, "os", 4, [128, 512], BF16, dma=True)
    psr = Ring(ph, "ps", 8, bufs=P.psb)
    for h in range(5):
        for (t0, n) in SUBT:
            s, ps, pd = psr.next()
            yds = []
            pet = None
            for c in range(2):
                a_, ya, ad = yr.next()
                la = ph.dma(ya[:, :n], C.dyT[h, 0, c * 128:(c + 1) * 128, t0:t0 + n], yr.ds[a_], deps=ad)
                b_, yb, bd = yr.next()
                lb = ph.dma(yb[:, :n], C.dyT[h, 1, c * 128:(c + 1) * 128, t0:t0 + n], yr.ds[b_], deps=bd)
                d_, yd, dd = ydr.next()
                t1 = ph.op("vector", lambda e, yd=yd, yb=yb, ya=ya, n=n: e.scalar_tensor_tensor(
                    out=yd[:, :n], in0=yb[:, :n], scalar=C.lam[:, 1:2], in1=ya[:, :n], op0=ALU.mult, op1=ALU.add), deps=[la, lb] + dd)
                yr.release(a_, t1)
                yr.release(b_, t1)
                q_, sq, qd = sqr.next()
                t2 = ph.op("scalar", lambda e, sq=sq, yd=yd, n=n: e.activation(out=sq[:, :n], in_=yd[:, :n], func=AF.Square),
                           deps=[t1] + qd)
                pet = ph.op("tensor", lambda e, ps=ps, sq=sq, n=n, c=c: e.matmul(ps[:, :n], lhsT=C.ones_bf[:], rhs=sq[:, :n],
                                                                               start=(c == 0), stop=(c == 1)),
                            deps=[t2] + (pd if c == 0 else []))
                sqr.release(q_, pet)
                yds.append((d_, yd, t1))
            r_, rs, rd = rsr.next()
            tq = ph.op("scalar", lambda e, rs=rs, ps=ps, n=n: e.activation(out=rs[:, :n], in_=ps[:, :n], func=AF.Sqrt,
                                                                          bias=C.epsc[:, 0:1], scale=1.0 / 256), deps=[pet] + rd)
            psr.release(s, tq)
            tr = ph.op("vector", lambda e, rs=rs, n=n: e.reciprocal(out=rs[:, :n], in_=rs[:, :n]), deps=[tq])
            lt = None
            for c, (d_, yd, t1) in enumerate(yds):
                o_, ob, od = osr.next()
                lt = ph.op("vector", lambda e, ob=ob, yd=yd, rs=rs, n=n, c=c: e.scalar_tensor_tensor(
                    out=ob[:, :n], in0=yd[:, :n], scalar=C.subg[:, c:c + 1], in1=rs[:, :n], op0=ALU.mult, op1=ALU.mult),
                    deps=[tr, t1] + od)
                ydr.release(d_, lt)
                r0 = 2816 + h * 256 + c * 128
                dk = ph.dma(C.yT[r0:r0 + 128, t0:t0 + n], ob[:, :n], osr.ds[o_], deps=[lt])
                osr.release(o_, dk)
            rsr.release(r_, lt)
    ph.run()


def moe_gate_phase(P, C):
    ph = Phase(P, "gate")
    NKT = NT // 128
    lg = ph.sb("lg", [128, NKT, 8], F32)
    ds = ph.dsem()
    tl = ph.dma(lg[:], C.lgT.rearrange("(kt p) e -> p kt e", p=128), ds)
    combT = ph.sb("combT", [8, NT], F32)
    wk = Ring(ph, "wk", 3, [128, 48], F32)
    psr = Ring(ph, "ps", 8, bufs=P.psb)
    last = None
    for kt in range(NKT):
        w_, w, wd = wk.next()
        L = lg[:, kt, :]
        m1, eq1, l2, m2, eq2, dd, g1, cmb = (w[:, 0:1], w[:, 8:16], w[:, 16:24], w[:, 1:2], w[:, 24:32], w[:, 2:3], w[:, 3:4], w[:, 32:40])
        g2 = w[:, 4:5]
        t = ph.op("vector", lambda e, m1=m1, L=L: e.tensor_reduce(out=m1, in_=L, axis=AX.X, op=ALU.max), deps=[tl] + wd)
        t = ph.op("vector", lambda e, eq1=eq1, L=L, m1=m1: e.tensor_scalar(out=eq1, in0=L, scalar1=m1, scalar2=None, op0=ALU.is_equal), deps=[t])
        t = ph.op("vector", lambda e, l2=l2, eq1=eq1, L=L: e.scalar_tensor_tensor(out=l2, in0=eq1, scalar=-1e30, in1=L, op0=ALU.mult,
                                                                                 op1=ALU.add), deps=[t])
        t = ph.op("vector", lambda e, m2=m2, l2=l2: e.tensor_reduce(out=m2, in_=l2, axis=AX.X, op=ALU.max), deps=[t])
        t = ph.op("vector", lambda e, eq2=eq2, l2=l2, m2=m2: e.tensor_scalar(out=eq2, in0=l2, scalar1=m2, scalar2=None, op0=ALU.is_equal),
                  deps=[t])
        t = ph.op("vector", lambda e, dd=dd, m2=m2, m1=m1: e.tensor_tensor(out=dd, in0=m2, in1=m1, op=ALU.subtract), deps=[t])
        t = ph.op("scalar", lambda e, dd=dd: e.activation(out=dd, in_=dd, func=AF.Exp), deps=[t])
        t = ph.op("vector", lambda e, g1=g1, dd=dd: e.tensor_scalar(out=g1, in0=dd, scalar1=1.0, scalar2=None, op0=ALU.add), deps=[t])
        t = ph.op("vector", lambda e, g1=g1: e.reciprocal(out=g1, in_=g1), deps=[t])
        t = ph.op("vector", lambda e, g2=g2, dd=dd, g1=g1: e.tensor_tensor(out=g2, in0=dd, in1=g1, op=ALU.mult), deps=[t])
        t = ph.op("vector", lambda e, cmb=cmb, eq1=eq1, g1=g1: e.tensor_scalar(out=cmb, in0=eq1, scalar1=g1, scalar2=None, op0=ALU.mult),
                  deps=[t])
        t = ph.op("vector", lambda e, cmb=cmb, eq2=eq2, g2=g2: e.scalar_tensor_tensor(out=cmb, in0=eq2, scalar=g2, in1=cmb, op0=ALU.mult,
                                                                                      op1=ALU.add), deps=[t])
        s, ps, pd = psr.next()
        tp = ph.op("tensor", lambda e, ps=ps, cmb=cmb: e.transpose(out=ps[0:8, 0:128], in_=cmb, identity=C.ident[:]), deps=[t] + pd)
        last = ph.op("vector", lambda e, ps=ps, kt=kt: e.tensor_copy(out=combT[0:8, kt * 128:(kt + 1) * 128], in_=ps[0:8, 0:128]),
                     deps=[tp])
        psr.release(s, last)
        wk.release(w_, tp)
    osr = Ring(ph, "os", 3, [128, 512], F32, dma=True)
    for ex in range(NEXP):
        for (t0, n) in SUBT:
            s, ps, pd = psr.next()
            tp = ph.op("tensor", lambda e, ps=ps, ex=ex, t0=t0, n=n: e.matmul(ps[:, :n], lhsT=C.sel[0:8, ex, :], rhs=combT[0:8, t0:t0 + n],
                                                                             start=True, stop=True), deps=[last] + pd)
            o_, ob, od = osr.next()
            tc = ph.op("scalar", lambda e, ob=ob, ps=ps, n=n: e.activation(out=ob[:, :n], in_=ps[:, :n], func=AF.Copy), deps=[tp] + od)
            psr.release(s, tc)
            dk = ph.dma(C.cb[ex, :, t0:t0 + n], ob[:, :n], osr.ds[o_], deps=[tc])
            osr.release(o_, dk)
    ph.run()


def final_phase(P, C):
    ph = Phase(P, "fin")
    ds = ph.dsem()
    for q in range(0, D, 512):
        ph.dma(C.outT[q:q + 512, :], C.hT[q:q + 512, 0:SEQ], ds)
    ph.run()


GROUPS = [(0, 1024), (1024, 1024), (2048, 1024), (3072, 1280)]


def build(n_layers=DEPTH, dbg=(), stop=None, Lw=DEPTH):
    nc = bass.Bass("TRN2", target_bir_lowering=False)
    C = Ctx()
    L = Lw
    L2 = max(1, Lw // 2)

    def din(name, shape, dt=F32):
        return nc.dram_tensor(name, list(shape), dt, kind="ExternalInput").ap()

    def dscr(name, shape, dt):
        kind = "ExternalOutput" if name in dbg else "Internal"
        return nc.dram_tensor(name, list(shape), dt, kind=kind).ap()

    C.xT = din("xT", [D, NT])
    C.cvec = din("cvec", [128, KC, 2])
    C.ada_w = din("ada_w", [L, D, 6 * D])
    C.w_in = din("w_in", [L, D, IN_W])
    C.w_out = din("w_out", [L, D, D])
    C.w_uq = din("mla_w_uq", [L, 768, 1920])
    C.w_ukv = din("mla_w_ukv", [L, 512, 2560])
    C.ffn_w1 = din("ffn_w1", [L2, D, D])
    C.ffn_w3 = din("ffn_w3", [L2, D, D])
    C.ffn_w2 = din("ffn_w2", [L2, D, D])
    C.router = din("moe_router", [L2, D, NEXP])
    C.moe_w1 = din("moe_w1", [L2, NEXP, D, DFE])
    C.moe_w3 = din("moe_w3", [L2, NEXP, D, DFE])
    C.moe_w2 = din("moe_w2", [L2, NEXP, DFE, D])
    C.vecs = din("vecs", [L, 128, NV])
    C.ropeH = din("ropeH", [2, 128, NT])
    C.ropeM = din("ropeM", [2, 64, NT])
    C.c_ones = din("c_ones", [128, 128])
    C.c_r128 = din("c_r128", [128, 128])
    C.c_r64 = din("c_r64", [64, 64])
    C.c_ident = din("c_ident", [128, 128])
    C.c_sel = din("c_sel", [8, NEXP, 128])
    C.c_masks = din("c_masks", [128, 2, 384])
    C.outT = nc.dram_tensor("outT", [D, SEQ], F32, kind="ExternalOutput").ap()

    C.hT = dscr("hT", [D, NT], F32)
    C.xnT = dscr("xnT", [D, NT], BF16)
    C.zT = dscr("zT", [IN_W, NT], BF16)
    C.sqT = dscr("sqT", [1536, NT], BF16)
    C.skT = dscr("skT", [512, NT], BF16)
    C.svV = dscr("svV", [NT, 512], BF16)
    C.dvV = dscr("dvV", [NT, 1280], BF16)
    C.cqnT = dscr("cqnT", [768, NT], BF16)
    C.ckvnT = dscr("ckvnT", [512, NT], BF16)
    C.mqraw = dscr("mqraw", [1920, NT], BF16)
    C.mkraw = dscr("mkraw", [1280, NT], BF16)
    C.mvV = dscr("mvV", [NT, 1280], BF16)
    C.mqT = dscr("mqT", [1920, NT], BF16)
    C.mkT = dscr("mkT", [1920, NT], BF16)
    C.dqT = dscr("dqT", [1280, NT], BF16)
    C.dkT = dscr("dkT", [1280, NT], BF16)
    C.dyT = dscr("dyT", [5, 2, 256, NT], F32)
    C.yT = dscr("yT", [D, NT], BF16)
    C.hidT = dscr("hidT", [2 * D, NT], BF16)
    C.lgT = dscr("lgT", [NT, NEXP], F32)
    C.cb = dscr("cb", [NEXP, 128, NT], F32)

    P = Prog(nc)
    with ExitStack() as st:
        def sbp(name, shape, dt):
            return st.enter_context(nc.sbuf_tensor(name, list(shape), dt))

        P.psb = [st.enter_context(nc.psum_tensor("psb%d" % i, [128, 512], F32)) for i in range(8)]
        C.scsb = sbp("scsb", [128, KC, 2], BF16)
        C.modsb = sbp("modsb", [128, 6 * KC, 2], F32)
        C.vsb = sbp("vsb", [128, NV], F32)
        C.ones_bf = sbp("ones_bf", [128, 128], BF16)
        C.ones32 = sbp("ones32", [128, 128], F32)
        C.r128 = sbp("r128", [128, 128], BF16)
        C.r64 = sbp("r64", [64, 64], BF16)
        C.ident = sbp("ident", [128, 128], F32)
        C.sel = sbp("sel", [8, NEXP, 128], F32)
        C.masks = sbp("masks", [128, 2, 384], F32)
        C.lam = sbp("lam", [128, 2], F32)
        C.esink = sbp("esink", [128, 12], F32)
        C.subg = sbp("subg", [128, 2], F32)
        C.epsc = sbp("epsc", [128, 1], F32)

        count = [0]

        def go():
            count[0] += 1
            return stop is None or count[0] <= stop

        ph = Phase(P, "c0")
        ph.op("vector", lambda e: e.memset(C.epsc[:], EPS))
        ph.op("vector", lambda e: e.memset(C.ones32[:], 1.0))
        ph.run()
        init_phase(P, C)

        def zdst(t0, n, j, u):
            w = u["chunks"][j][1]
            return C.zT[u["f0"]:u["f0"] + w, t0:t0 + n]

        for l in range(n_layers):
            if not go(): break
            layer_vec_phase(P, C, l)
            if not go(): break
            mod_phase(P, C, l)
            if not go(): break
            norm_phase(P, C, l, 1)
            if not go(): break
            W = C.w_in[l]
            epiZ = EpiCopy(zdst)
            epiSV = EpiCopy(lambda t0, n, j, u: C.svV[t0:t0 + 128, u["f0"]:u["f0"] + n])
            epiDV = EpiCopy(lambda t0, n, j, u: C.dvV[t0:t0 + 128, u["f0"]:u["f0"] + n])
            blocks = (fblocks(W, 0, C_SV, epiZ) + tblocks(W, C_SV, C_CQ, epiSV) + fblocks(W, C_CQ, C_DQ, epiZ)
                      + fblocks(W, C_DQ, C_DV, epiZ) + tblocks(W, C_DV, IN_W, epiDV))
            linear(P, "inp", D, GROUPS, blocks, xT=C.xnT)
            if not go(): break
            items = []
            for h in range(12):
                items.append(dict(src=[(C.zT[h * 128:(h + 1) * 128, :], 128)], g=[C.vsb[:, V_SQN:V_SQN + 1]], dim=128, rope=["H"],
                                  dst=[C.sqT[h * 128:(h + 1) * 128, :]]))
            for g in range(4):
                r0 = C_SK + g * 128
                items.append(dict(src=[(C.zT[r0:r0 + 128, :], 128)], g=[C.vsb[:, V_SKN:V_SKN + 1]], dim=128, rope=["H"],
                                  dst=[C.skT[g * 128:(g + 1) * 128, :]]))
            for c in range(10):
                m = c % 2
                items.append(dict(src=[(C.zT[C_DQ + c * 128:C_DQ + (c + 1) * 128, :], 128)], g=[C.vsb[:, V_DQN + m:V_DQN + m + 1]],
                                  dim=128, rope=["H"], dst=[C.dqT[c * 128:(c + 1) * 128, :]]))
                items.append(dict(src=[(C.zT[C_DK + c * 128:C_DK + (c + 1) * 128, :], 128)], g=[C.vsb[:, V_DKN + m:V_DKN + m + 1]],
                                  dim=128, rope=["H"], dst=[C.dkT[c * 128:(c + 1) * 128, :]]))
            items.append(dict(src=[(C.zT[C_CQ + j * 128:C_CQ + (j + 1) * 128, :], 128) for j in range(6)],
                              g=[C.vsb[:, V_CQ + j:V_CQ + j + 1] for j in range(6)], dim=768, rope=[None] * 6,
                              dst=[C.cqnT[j * 128:(j + 1) * 128, :] for j in range(6)]))
            items.append(dict(src=[(C.zT[C_CKV + j * 128:C_CKV + (j + 1) * 128, :], 128) for j in range(4)],
                              g=[C.vsb[:, V_CKV + j:V_CKV + j + 1] for j in range(4)], dim=512, rope=[None] * 4,
                              dst=[C.ckvnT[j * 128:(j + 1) * 128, :] for j in range(4)]))
            rmsrope_phase(P, C, items)
            if not go(): break
            epq = EpiCopy(lambda t0, n, j, u: C.mqraw[u["f0"]:u["f0"] + u["chunks"][j][1], t0:t0 + n])
            linear(P, "upq", 768, GROUPS, fblocks(C.w_uq[l], 0, 1920, epq), xT=C.cqnT)
            if not go(): break
            epk = EpiCopy(lambda t0, n, j, u: C.mkraw[u["f0"]:u["f0"] + 128, t0:t0 + n])
            epv = EpiCopy(lambda t0, n, j, u: C.mvV[t0:t0 + 128, u["f0"]:u["f0"] + n])
            blocks = []
            for hp in range(5):
                units = []
                for r in range(2):
                    h = hp * 2 + r
                    units.append(dict(mode="F", chunks=[(r * 256, 128)], epi=epk, f0=h * 128))
                    units.append(dict(mode="T", chunks=[(r * 256 + 128, 128)], epi=epv, f0=h * 128))
                blocks.append(dict(segs=[(C.w_ukv[l], hp * 512, 512)], units=units))
            linear(P, "upkv", 512, GROUPS, blocks, xT=C.ckvnT)
            if not go(): break
            items = []
            for h in range(10):
                items.append(dict(src=[(C.mqraw[h * 192:h * 192 + 128, :], 128), (C.mqraw[h * 192 + 128:(h + 1) * 192, :], 64)],
                                  g=[C.vsb[:, V_MQN:V_MQN + 1], C.vsb[0:64, V_MQN + 1:V_MQN + 2]], dim=192, rope=[None, "M"],
                                  dst=[C.mqT[h * 192:h * 192 + 128, :], C.mqT[h * 192 + 128:(h + 1) * 192, :]]))
                items.append(dict(src=[(C.mkraw[h * 128:(h + 1) * 128, :], 128), (C.zT[C_KR:C_KR + 64, :], 64)],
                                  g=[C.vsb[:, V_MKN:V_MKN + 1], C.vsb[0:64, V_MKN + 1:V_MKN + 2]], dim=192, rope=[None, "M"],
                                  dst=[C.mkT[h * 192:h * 192 + 128, :], C.mkT[h * 192 + 128:(h + 1) * 192, :]]))
            rmsrope_phase(P, C, items)
            if not go(): break
            vheads = []
            for h in range(10):
                vheads.append(dict(q=[(C.mqT[h * 192:h * 192 + 128, :], 128), (C.mqT[h * 192 + 128:(h + 1) * 192, :], 64)],
                                   k=[(C.mkT[h * 192:h * 192 + 128, :], 128), (C.mkT[h * 192 + 128:(h + 1) * 192, :], 64)],
                                   V=C.mvV[:, h * 128:(h + 1) * 128], ew=128, scale=192 ** -0.5, dt=BF16,
                                   dst=(lambda ec, h=h: C.yT[1536 + h * 128:1536 + (h + 1) * 128, :])))
            for h in range(5):
                for m in range(2):
                    c = h * 2 + m
                    vheads.append(dict(q=[(C.dqT[c * 128:(c + 1) * 128, :], 128)], k=[(C.dkT[c * 128:(c + 1) * 128, :], 128)],
                                       V=C.dvV[:, h * 256:(h + 1) * 256], ew=256, scale=128 ** -0.5, dt=F32,
                                       dst=(lambda ec, h=h, m=m: C.dyT[h, m, ec * 128:(ec + 1) * 128, :])))
            attn_phase(P, C, vheads)
            if not go(): break
            swa_phase(P, C)
            if not go(): break
            dif_merge_phase(P, C)
            if not go(): break
            eo = EpiResid(C.hT, lambda fc, isc: C.modsb[:, 2 * KC + fc, (1 if isc else 0):(2 if isc else 1)])
            linear(P, "outp", D, GROUPS, fblocks(C.w_out[l], 0, D, eo), xT=C.yT)
            if not go(): break
            norm_phase(P, C, l, 2)
            if not go(): break
            e2 = EpiResid(C.hT, lambda fc, isc: C.modsb[:, 5 * KC + fc, (1 if isc else 0):(2 if isc else 1)])
            i = l // 2
            if l % 2 == 0:
                es = EpiSwiGLU(lambda t0, n, j, u: C.hidT[u["f0"]:u["f0"] + 128, t0:t0 + n])
                blocks = []
                for f in range(0, D, 256):
                    units = [dict(mode="F", chunks=[(o, 128), (256 + o, 128)], epi=es, f0=f + o) for o in (0, 128)]
                    blocks.append(dict(segs=[(C.ffn_w1[i], f, 256), (C.ffn_w3[i], f, 256)], units=units))
                linear(P, "ffu", D, GROUPS, blocks, xT=C.xnT)
                if not go(): break
                linear(P, "ffd", D, GROUPS, fblocks(C.ffn_w2[i], 0, D, e2), xT=C.hidT[0:D, :])
            else:
                er = EpiCopy(lambda t0, n, j, u: C.lgT[t0:t0 + 128, 0:n], dt=F32)
                linear(P, "rtr", D, GROUPS, tblocks(C.router[i], 0, NEXP, er), xT=C.xnT)
                if not go(): break
                moe_gate_phase(P, C)
                if not go(): break
                es = EpiSwiGLU(lambda t0, n, j, u: C.hidT[u["f0"]:u["f0"] + 128, t0:t0 + n],
                               cb=lambda t0, n, u: C.cb[u["ex"], :, t0:t0 + n])
                blocks = []
                for ex in range(NEXP):
                    for f in range(0, DFE, 256):
                        units = [dict(mode="F", chunks=[(o, 128), (256 + o, 128)], epi=es, f0=ex * DFE + f + o, ex=ex) for o in (0, 128)]
                        blocks.append(dict(segs=[(C.moe_w1[i, ex], f, 256), (C.moe_w3[i, ex], f, 256)], units=units))
                linear(P, "mou", D, GROUPS, blocks, xT=C.xnT)
                if not go(): break
                W2 = C.moe_w2[i].rearrange("e k d -> (e k) d")
                linear(P, "mod1", D, GROUPS, fblocks(W2[0:D, :], 0, D, e2), xT=C.hidT[0:D, :])
                e3 = EpiResid(C.hT, lambda fc, isc: C.modsb[:, 5 * KC + fc, (1 if isc else 0):(2 if isc else 1)])
                linear(P, "mod2", D, GROUPS, fblocks(W2[D:2 * D, :], 0, D, e3), xT=C.hidT[D:2 * D, :])
        final_phase(P, C)
    return nc


def _rope_tables(dim):
    rows = SEQ // GRID_W
    t = np.arange(SEQ)
    t_row = (t // GRID_W).astype(np.float32)
    t_col = (t % GRID_W).astype(np.float32)
    quarter = dim // 4
    inv = (np.float32(10000.0) ** (-np.arange(quarter, dtype=np.float32) / np.float32(quarter))).astype(np.float32)
    ang = np.concatenate([t_row[:, None] * inv, t_col[:, None] * inv], axis=-1).astype(np.float32)
    cos = np.cos(ang).astype(np.float32)
    sin = np.sin(ang).astype(np.float32)
    tab = np.zeros((2, dim, NT), np.float32)
    tab[0, :, SEQ:] = 1.0
    half = dim // 2
    tab[0, :half, :SEQ] = cos.T
    tab[0, half:, :SEQ] = cos.T
    tab[1, :half, :SEQ] = sin.T
    tab[1, half:, :SEQ] = sin.T
    return tab


def _rot_lhsT(dim):
    half = dim // 2
    m = np.zeros((dim, dim), np.float32)
    for i in range(half):
        m[i + half, i] = -1.0
        m[i, i + half] = 1.0
    return m


def _host_inputs(inp, L=DEPTH, ncores=NCORES):
    f = lambda a: np.ascontiguousarray(np.asarray(a, dtype=np.float32))
    vecs = np.zeros((L, 128, NV), np.float32)
    for l in range(L):
        vecs[l, :, V_N1:V_N1 + KC] = f(inp["norm1_g"])[l].reshape(KC, 128).T
        vecs[l, :, V_N2:V_N2 + KC] = f(inp["norm2_g"])[l].reshape(KC, 128).T
        vecs[l, :, V_ADAB:V_ADAB + 6 * KC] = f(inp["ada_b"])[l].reshape(6 * KC, 128).T
        vecs[l, :, V_CQ:V_CQ + 6] = f(inp["mla_cq_norm"])[l].reshape(6, 128).T
        vecs[l, :, V_CKV:V_CKV + 4] = f(inp["mla_ckv_norm"])[l].reshape(4, 128).T
        vecs[l, :, V_SQN] = f(inp["swa_q_norm"])[l]
        vecs[l, :, V_SKN] = f(inp["swa_k_norm"])[l]
        vecs[l, :, V_MQN] = f(inp["mla_q_norm"])[l][:128]
        vecs[l, :64, V_MQN + 1] = f(inp["mla_q_norm"])[l][128:]
        vecs[l, :, V_MKN] = f(inp["mla_k_norm"])[l][:128]
        vecs[l, :64, V_MKN + 1] = f(inp["mla_k_norm"])[l][128:]
        vecs[l, :, V_DQN:V_DQN + 2] = f(inp["dif_q_norm"])[l].T
        vecs[l, :, V_DKN:V_DKN + 2] = f(inp["dif_k_norm"])[l].T
        vecs[l, :, V_SUB:V_SUB + 2] = f(inp["dif_subln"])[l].reshape(2, 128).T
        vecs[l, :, V_LAM:V_LAM + 4] = f(inp["dif_lambda"])[l].T
        vecs[l, :, V_SINK:V_SINK + 12] = f(inp["swa_sink"])[l][None, :]
    masks = np.zeros((128, 2, 384), np.float32)
    k = np.arange(128)[:, None]
    q = np.arange(128)[None, :]
    masks[:, 0, :] = np.tile((q <= k).astype(np.float32), (1, 3))
    masks[:, 1, :] = np.tile((k <= q).astype(np.float32), (1, 3))
    sel = np.zeros((8, NEXP, 128), np.float32)
    for e in range(NEXP):
        sel[e, e, :] = 1.0
    shared = dict(
        ada_w=f(inp["ada_w"]), w_in=f(inp["w_in"]), w_out=f(inp["w_out"]), mla_w_uq=f(inp["mla_w_uq"]),
        mla_w_ukv=f(inp["mla_w_ukv"]), ffn_w1=f(inp["ffn_w1"]), ffn_w3=f(inp["ffn_w3"]), ffn_w2=f(inp["ffn_w2"]),
        moe_router=f(inp["moe_router"]), moe_w1=f(inp["moe_w1"]), moe_w3=f(inp["moe_w3"]), moe_w2=f(inp["moe_w2"]),
        vecs=vecs, ropeH=_rope_tables(128), ropeM=_rope_tables(64), c_ones=np.ones((128, 128), np.float32),
        c_r128=_rot_lhsT(128), c_r64=_rot_lhsT(64), c_ident=np.eye(128, dtype=np.float32), c_sel=sel, c_masks=masks)
    x = f(inp["x"])
    ctx = f(inp["ctx"])
    c = f(inp["c"])
    cc = f(inp["c_ctx"])
    maps = []
    for b in range(ncores):
        xT = np.ascontiguousarray(np.concatenate([x[b].T, ctx[b].T], axis=1))
        cvec = np.ascontiguousarray(np.stack([c[b].reshape(KC, 128).T, cc.reshape(KC, 128).T], axis=-1))
        m = dict(shared)
        m["xT"] = xT
        m["cvec"] = cvec
        maps.append(m)
    return maps


def kernel(**inputs):
    maps = _host_inputs(inputs)
    nc = build()
    res = run_bass_kernel_spmd(nc, maps, core_ids=list(range(NCORES)))
    out = np.stack([np.ascontiguousarray(res.results[b]["outT"].T) for b in range(NCORES)], axis=0)
    return out.astype(np.float32)
```

```python
import math
from contextlib import ExitStack

import numpy as np
import concourse.bass as bass
import concourse.mybir as mybir
from concourse.bass_utils import run_bass_kernel_spmd

F32 = mybir.dt.float32
BF16 = mybir.dt.bfloat16
AF = mybir.ActivationFunctionType
ALU = mybir.AluOpType
AX = mybir.AxisListType

D = 4096
KC = D // 128
SEQ = 4096
CTX = 256
NT = SEQ + CTX
DEPTH = 4
NCORES = 4
EPS = 1e-6
GRID_W = 64
IN_W = 7744
NEXP = 8
DFE = 1024

C_SQ, C_SK, C_SV, C_CQ, C_CKV, C_KR, C_DQ, C_DK, C_DV = 0, 1536, 2048, 2560, 3328, 3840, 3904, 5184, 6464

V_N1, V_N2, V_ADAB, V_CQ, V_CKV, V_SQN, V_SKN, V_MQN, V_MKN, V_DQN, V_DKN, V_SUB, V_LAM, V_SINK = (
    0, 32, 64, 256, 262, 266, 267, 268, 270, 272, 274, 276, 278, 282)
NV = 294

GROUPS = [(0, 1024), (1024, 1024), (2048, 1024), (3072, 1280)]
SUBT = [(i * 512, 512) for i in range(8)] + [(4096, 256)]

ENGS = ("sync", "scalar", "vector", "gpsimd", "tensor")
ATT_LOOK, ATT_DEFER, ATT_POOL = 2, 2, 1
NDS = 40


class Tok:
    __slots__ = ("sem", "val", "dma")

    def __init__(self, sem, val, dma=False):
        self.sem = sem
        self.val = val
        self.dma = dma


class Prog:
    def __init__(self, nc):
        self.nc = nc
        self.esem = {}
        self.ecnt = {}
        for e in ("scalar", "vector", "gpsimd", "tensor"):
            self.esem[e] = nc.alloc_semaphore(name="sem_" + e)
            self.ecnt[e] = 0
        self.dsem = [nc.alloc_semaphore(name="dsem%d" % i) for i in range(NDS)]
        self.dcnt = [0] * NDS
        self.dfree = list(range(NDS))
        self.nph = 0


class Phase:
    def __init__(self, P, name):
        self.P = P
        self.nc = P.nc
        P.nph += 1
        self.name = "%s%d" % (name, P.nph)
        self.tasks = {e: [] for e in ENGS}
        self.stack = ExitStack()
        self.my_ds = []
        self.nt = 0

    def sb(self, name, shape, dt):
        self.nt += 1
        return self.stack.enter_context(self.nc.sbuf_tensor("%s_%s%d" % (self.name, name, self.nt), list(shape), dt))

    def dsem(self):
        i = self.P.dfree.pop()
        self.my_ds.append(i)
        return i

    def op(self, eng, fn, deps=(), signal=True):
        P = self.P
        tok = None
        if signal:
            P.ecnt[eng] += 1
            tok = Tok(P.esem[eng], P.ecnt[eng])
        self.tasks[eng].append((fn, [d for d in deps if d is not None], tok))
        return tok

    def dma(self, out, in_, si, deps=(), q="sync"):
        P = self.P
        P.dcnt[si] += 16
        tok = Tok(P.dsem[si], P.dcnt[si], True)
        self.tasks[q].append((lambda e: e.dma_start(out=out, in_=in_), [d for d in deps if d is not None], tok))
        return tok

    def run(self):
        P = self.P
        finals = [Tok(P.dsem[i], P.dcnt[i], True) for i in self.my_ds]
        self.tasks["sync"].append((None, finals, None))
        with self.nc.Block() as blk:
            for eng in ENGS:
                tasks = self.tasks[eng]
                if not tasks:
                    continue

                def body(e, tasks=tasks):
                    waited = {}
                    for fn, deps, tok in tasks:
                        for d in deps:
                            k = id(d.sem)
                            if waited.get(k, -1) >= d.val:
                                continue
                            e.wait_ge(d.sem, d.val)
                            waited[k] = d.val
                        if fn is None:
                            continue
                        ins = fn(e)
                        if tok is not None:
                            ins.then_inc(tok.sem, 16 if tok.dma else 1)

                getattr(blk, eng)(body)
        self.stack.close()
        P.dfree.extend(self.my_ds)


class Ring:
    def __init__(self, ph, name, n, shape=None, dt=None, dma=False, bufs=None):
        self.n = n
        self.bufs = bufs if bufs is not None else [ph.sb("%s%d" % (name, i), shape, dt) for i in range(n)]
        self.free = [[] for _ in range(n)]
        self.ds = [ph.dsem() for _ in range(n)] if dma else None
        self.i = 0

    def next(self):
        s = self.i % self.n
        self.i += 1
        deps = self.free[s]
        self.free[s] = []
        return s, self.bufs[s], deps

    def release(self, s, tok):
        if tok is not None:
            self.free[s].append(tok)


def nsplits(t0, n):
    out = []
    while n > 0:
        m = min(512, n)
        out.append((t0, m))
        t0 += m
        n -= m
    return out


class EpiCopy:
    def __init__(self, dst, dt=BF16):
        self.dst = dst
        self.dt = dt

    def setup(self, ph, psr):
        self.psr = psr
        self.st = Ring(ph, "epst", 4, [128, 512], self.dt, dma=True)
        self.k = 0

    def __call__(self, ph, t0, n, tiles, petok, uinfo):
        for j, (s, ps, w) in enumerate(tiles):
            ss, stg, deps = self.st.next()
            eng = "scalar" if self.k % 2 == 0 else "vector"
            self.k += 1
            if eng == "scalar":
                tk = ph.op(eng, lambda e, stg=stg, ps=ps, w=w, n=n: e.activation(out=stg[:w, :n], in_=ps[:w, :n], func=AF.Copy),
                           deps=[petok] + deps)
            else:
                tk = ph.op(eng, lambda e, stg=stg, ps=ps, w=w, n=n: e.tensor_copy(out=stg[:w, :n], in_=ps[:w, :n]),
                           deps=[petok] + deps)
            self.psr.release(s, tk)
            dtok = ph.dma(self.dst(t0, n, j, uinfo), stg[:w, :n], self.st.ds[ss], deps=[tk])
            self.st.release(ss, dtok)


class EpiSwiGLU:
    def __init__(self, dst, cb=None):
        self.dst = dst
        self.cb = cb

    def setup(self, ph, psr):
        self.psr = psr
        self.sg = Ring(ph, "sg", 3, [128, 512], F32)
        self.st = Ring(ph, "epst", 3, [128, 512], BF16, dma=True)
        if self.cb is not None:
            self.cbr = Ring(ph, "cbr", 3, [128, 512], F32, dma=True)
            self.t2 = Ring(ph, "t2", 2, [128, 512], F32)

    def __call__(self, ph, t0, n, tiles, petok, uinfo):
        (sa, pa, w), (sb_, pb, _) = tiles
        s1, sg, d1 = self.sg.next()
        t1 = ph.op("scalar", lambda e: e.activation(out=sg[:w, :n], in_=pa[:w, :n], func=AF.Silu), deps=[petok] + d1)
        self.psr.release(sa, t1)
        ss, stg, d2 = self.st.next()
        if self.cb is None:
            t2 = ph.op("vector", lambda e: e.tensor_tensor(out=stg[:w, :n], in0=pb[:w, :n], in1=sg[:w, :n], op=ALU.mult),
                       deps=[petok, t1] + d2)
            self.psr.release(sb_, t2)
            self.sg.release(s1, t2)
        else:
            sc, cbt, d3 = self.cbr.next()
            tc = ph.dma(cbt[:w, :n], self.cb(t0, n, uinfo), self.cbr.ds[sc], deps=d3)
            sx, tx, d4 = self.t2.next()
            ta = ph.op("vector", lambda e: e.tensor_tensor(out=tx[:w, :n], in0=pb[:w, :n], in1=sg[:w, :n], op=ALU.mult),
                       deps=[petok, t1] + d4)
            self.psr.release(sb_, ta)
            self.sg.release(s1, ta)
            t2 = ph.op("gpsimd", lambda e: e.tensor_tensor(out=stg[:w, :n], in0=tx[:w, :n], in1=cbt[:w, :n], op=ALU.mult),
                       deps=[ta, tc] + d2)
            self.t2.release(sx, t2)
            self.cbr.release(sc, t2)
        dtok = ph.dma(self.dst(t0, n, 0, uinfo), stg[:w, :n], self.st.ds[ss], deps=[t2])
        self.st.release(ss, dtok)


class EpiResid:
    def __init__(self, hT, gcol):
        self.hT = hT
        self.gcol = gcol

    def setup(self, ph, psr):
        self.psr = psr
        self.hin = Ring(ph, "hin", 3, [128, 512], F32, dma=True)
        self.hout = Ring(ph, "hout", 3, [128, 512], F32, dma=True)

    def __call__(self, ph, t0, n, tiles, petok, uinfo):
        (s, ps, w), = tiles
        f0 = uinfo["f0"]
        si, hin, d1 = self.hin.next()
        tl = ph.dma(hin[:w, :n], self.hT[f0:f0 + w, t0:t0 + n], self.hin.ds[si], deps=d1)
        so, hout, d2 = self.hout.next()
        g = self.gcol(f0 // 128, t0 >= SEQ)
        tk = ph.op("vector", lambda e: e.scalar_tensor_tensor(out=hout[:w, :n], in0=ps[:w, :n], scalar=g, in1=hin[:w, :n],
                                                                 op0=ALU.mult, op1=ALU.add), deps=[petok, tl] + d2)
        self.psr.release(s, tk)
        self.hin.release(si, tk)
        dtok = ph.dma(self.hT[f0:f0 + w, t0:t0 + n], hout[:w, :n], self.hout.ds[so], deps=[tk])
        self.hout.release(so, dtok)


class EpiMod:
    def __init__(self, modsb, bcol):
        self.modsb = modsb
        self.bcol = bcol

    def setup(self, ph, psr):
        self.psr = psr

    def __call__(self, ph, t0, n, tiles, petok, uinfo):
        (s, ps, w), = tiles
        fc = uinfo["f0"] // 128
        tk = ph.op("vector", lambda e: e.tensor_scalar(out=self.modsb[:, fc, 0:2], in0=ps[:, 0:2], scalar1=self.bcol(fc),
                                                        scalar2=None, op0=ALU.add), deps=[petok])
        self.psr.release(s, tk)


def linear(P, name, K, groups, blocks, xT=None, x_sb=None, cast_engs=("gpsimd", "vector", "gpsimd", "scalar"), stgcfg=(3, 1024)):
    ph = Phase(P, name)
    KCn = K // 128
    gmax = max(g[1] for g in groups)
    psr = Ring(ph, "ps", 8, bufs=P.psb)
    epis = []
    for b in blocks:
        for u in b["units"]:
            if u["epi"] not in epis:
                epis.append(u["epi"])
    for ep in epis:
        ep.setup(ph, psr)
    if x_sb is None:
        X = ph.sb("X", [128, KCn, gmax], BF16)
        xds = ph.dsem()
    else:
        X = x_sb
    Wr = Ring(ph, "Wb", 2, [128, KCn, 512], BF16)
    Sr = Ring(ph, "Ws", stgcfg[0], [128, stgcfg[1]], F32, dma=True)

    steps = [(g, b) for g in range(len(groups)) for b in range(len(blocks))]
    xfree = []
    xtok = {}
    loaded = {}
    ncast = [0]

    def load_w(step):
        g, bi = steps[step]
        blk = blocks[bi]
        ws, Wb, wdeps = Wr.next()
        off = 0
        lasts = {}
        for (W, c0, w) in blk["segs"]:
            Wv = W.rearrange("(kc p) f -> p kc f", p=128)
            kcs = max(1, stgcfg[1] // w)
            k0 = 0
            while k0 < KCn:
                kn = min(kcs, KCn - k0)
                ss, stg, sdeps = Sr.next()
                sv = stg[:, 0:kn * w].rearrange("p (k w) -> p k w", w=w)
                td = ph.dma(sv, Wv[:, k0:k0 + kn, c0:c0 + w], Sr.ds[ss], deps=sdeps)
                ceng = cast_engs[ncast[0] % len(cast_engs)]
                ncast[0] += 1
                if ceng == "scalar":
                    tc = ph.op(ceng, lambda e, Wb=Wb, k0=k0, kn=kn, off=off, w=w, sv=sv: e.activation(
                        out=Wb[:, k0:k0 + kn, off:off + w], in_=sv, func=AF.Copy), deps=[td] + wdeps)
                else:
                    tc = ph.op(ceng, lambda e, Wb=Wb, k0=k0, kn=kn, off=off, w=w, sv=sv: e.tensor_copy(
                        out=Wb[:, k0:k0 + kn, off:off + w], in_=sv), deps=[td] + wdeps)
                Sr.release(ss, tc)
                lasts[ceng] = tc
                k0 += kn
            off += w
        loaded[step] = (ws, Wb, list(lasts.values()))

    load_w(0)
    for step, (g, bi) in enumerate(steps):
        g0, gn = groups[g]
        if bi == 0 and x_sb is None:
            xt = None
            for q in range(0, KCn, 8):
                qn = min(8, KCn - q)
                xt = ph.dma(X[:, q:q + qn, 0:gn], xT.rearrange("(kc p) t -> p kc t", p=128)[:, q:q + qn, g0:g0 + gn], xds,
                            deps=xfree if q == 0 else [])
            xfree = []
            xtok[g] = xt
        if step + 1 < len(steps):
            load_w(step + 1)
        ws, Wb, wtoks = loaded.pop(step)
        first_deps = wtoks + ([xtok[g]] if x_sb is None else [])
        lastpe = None
        for u in blocks[bi]["units"]:
            if u["mode"] == "F":
                for (t0, n) in nsplits(g0, gn):
                    tiles = []
                    pet = None
                    for (off, w) in u["chunks"]:
                        s, ps, pdeps = psr.next()
                        for kc in range(KCn):
                            lastk = kc == KCn - 1
                            pet = ph.op("tensor", lambda e, ps=ps, Wb=Wb, kc=kc, off=off, w=w, t0=t0, n=n, g0=g0, lastk=lastk:
                                        e.matmul(ps[:w, :n], lhsT=Wb[:, kc, off:off + w], rhs=X[:, kc, t0 - g0:t0 - g0 + n],
                                                 start=(kc == 0), stop=lastk),
                                        deps=(pdeps + first_deps) if kc == 0 else (), signal=lastk)
                        tiles.append((s, ps, w))
                    lastpe = pet
                    u["epi"](ph, t0, n, tiles, pet, u)
            else:
                (off, w), = u["chunks"]
                for tt in range(g0, g0 + gn, 128):
                    s, ps, pdeps = psr.next()
                    pet = None
                    for kc in range(KCn):
                        lastk = kc == KCn - 1
                        pet = ph.op("tensor", lambda e, ps=ps, Wb=Wb, kc=kc, off=off, w=w, tt=tt, g0=g0, lastk=lastk:
                                    e.matmul(ps[:, :w], lhsT=X[:, kc, tt - g0:tt - g0 + 128], rhs=Wb[:, kc, off:off + w],
                                             start=(kc == 0), stop=lastk),
                                    deps=(pdeps + first_deps) if kc == 0 else (), signal=lastk)
                    lastpe = pet
                    u["epi"](ph, tt, w, [(s, ps, 128)], pet, u)
        Wr.release(ws, lastpe)
        if bi == len(blocks) - 1:
            xfree = [lastpe]
    ph.run()


def fblocks(W, c0, c1, epi, base_f0=None, extra=None):
    out = []
    c = c0
    while c < c1:
        bw = min(512, c1 - c)
        units = []
        o = 0
        while o < bw:
            w = min(128, bw - o)
            u = dict(mode="F", chunks=[(o, w)], epi=epi, f0=(c + o) if base_f0 is None else base_f0 + (c + o - c0))
            if extra:
                u.update(extra)
            units.append(u)
            o += w
        out.append(dict(segs=[(W, c, bw)], units=units))
        c += bw
    return out


def tblocks(W, c0, c1, epi, dcol0=0):
    out = []
    c = c0
    while c < c1:
        bw = min(512, c1 - c)
        out.append(dict(segs=[(W, c, bw)], units=[dict(mode="T", chunks=[(0, bw)], epi=epi, f0=dcol0 + c - c0)]))
        c += bw
    return out


class Ctx:
    pass


def init_phase(P, C):
    ph = Phase(P, "init")
    ds = ph.dsem()
    for q in range(0, D, 512):
        ph.dma(C.hT[q:q + 512, :], C.xT[q:q + 512, :], ds)
    cv = ph.sb("cv", [128, KC, 2], F32)
    d2 = ph.dsem()
    t = ph.dma(cv[:], C.cvec, d2)
    ph.op("scalar", lambda e: e.activation(out=C.scsb[:], in_=cv[:], func=AF.Silu), deps=[t])
    d3 = ph.dsem()
    sts = []
    t = None
    for (dst, src) in ((C.ones_bf, C.c_ones), (C.r128, C.c_r128), (C.r64, C.c_r64)):
        st = ph.sb("cst", list(src.shape), F32)
        t = ph.dma(st[:], src, d3)
        sts.append((dst, st))
    for (dst, st) in sts:
        ph.op("vector", lambda e, dst=dst, st=st: e.tensor_copy(out=dst[:], in_=st[:]), deps=[t])
    d4 = ph.dsem()
    ph.dma(C.ident[:], C.c_ident, d4)
    ph.dma(C.sel[:], C.c_sel, d4)
    ph.dma(C.masks[:], C.c_masks, d4)
    ph.run()


def layer_vec_phase(P, C, l):
    ph = Phase(P, "vec")
    ds = ph.dsem()
    t = ph.dma(C.vsb[:], C.vecs[l], ds)
    lam_init = 0.8 - 0.6 * math.exp(-0.3 * l)
    pr = ph.sb("pr", [128, 2], F32)
    t1 = ph.op("vector", lambda e: e.tensor_tensor(out=pr[:, 0:1], in0=C.vsb[:, V_LAM:V_LAM + 1], in1=C.vsb[:, V_LAM + 1:V_LAM + 2],
                                                    op=ALU.mult), deps=[t])
    t2 = ph.op("vector", lambda e: e.tensor_tensor(out=pr[:, 1:2], in0=C.vsb[:, V_LAM + 2:V_LAM + 3], in1=C.vsb[:, V_LAM + 3:V_LAM + 4],
                                                    op=ALU.mult), deps=[t, t1])
    ps = P.psb[0]
    t3 = ph.op("tensor", lambda e: e.matmul(ps[:, 0:2], lhsT=C.ones32[:], rhs=pr[:, 0:2], start=True, stop=True), deps=[t2])
    ex = ph.sb("ex", [128, 2], F32)
    t4 = ph.op("scalar", lambda e: e.activation(out=ex[:], in_=ps[:, 0:2], func=AF.Exp), deps=[t3])
    t5 = ph.op("vector", lambda e: e.tensor_tensor(out=C.lam[:, 0:1], in0=ex[:, 0:1], in1=ex[:, 1:2], op=ALU.subtract), deps=[t4])
    t6 = ph.op("vector", lambda e: e.tensor_scalar(out=C.lam[:, 0:1], in0=C.lam[:, 0:1], scalar1=lam_init, scalar2=None, op0=ALU.add),
               deps=[t5])
    t7 = ph.op("vector", lambda e: e.tensor_scalar(out=C.lam[:, 1:2], in0=C.lam[:, 0:1], scalar1=-1.0, scalar2=None, op0=ALU.mult),
               deps=[t6])
    ph.op("scalar", lambda e: e.activation(out=C.esink[:], in_=C.vsb[:, V_SINK:V_SINK + 12], func=AF.Exp), deps=[t, t4])
    ph.op("vector", lambda e: e.tensor_scalar(out=C.subg[:], in0=C.vsb[:, V_SUB:V_SUB + 2], scalar1=1.0 - lam_init, scalar2=None,
                                              op0=ALU.mult), deps=[t, t7])
    ph.run()


def mod_phase(P, C, l):
    epi = EpiMod(C.modsb, lambda fc: C.vsb[:, V_ADAB + fc:V_ADAB + fc + 1])
    blocks = fblocks(C.ada_w[l], 0, 6 * D, epi)
    linear(P, "mod", D, [(0, 2)], blocks, x_sb=C.scsb, cast_engs=("gpsimd", "vector", "scalar"), stgcfg=(8, 2048))


def norm_phase(P, C, l, which):
    ph = Phase(P, "norm")
    gcol = V_N1 if which == 1 else V_N2
    sh_i, sc_i = (0, 1) if which == 1 else (3, 4)
    AB = ph.sb("AB", [128, 4, KC], F32)
    abt = None
    for ci in (0, 1):
        t = ph.op("vector", lambda e, ci=ci: e.tensor_scalar(out=AB[:, 2 * ci, :], in0=C.modsb[:, sc_i * KC:(sc_i + 1) * KC, ci],
                                                               scalar1=1.0, scalar2=None, op0=ALU.add), deps=[abt])
        t = ph.op("vector", lambda e, ci=ci: e.tensor_tensor(out=AB[:, 2 * ci, :], in0=AB[:, 2 * ci, :], in1=C.vsb[:, gcol:gcol + KC],
                                                               op=ALU.mult), deps=[t])
        abt = ph.op("vector", lambda e, ci=ci: e.tensor_copy(out=AB[:, 2 * ci + 1, :], in_=C.modsb[:, sh_i * KC:(sh_i + 1) * KC, ci]),
                    deps=[t])
    hr = Ring(ph, "hb", 2, [128, KC, 512], F32)
    hsem = [[ph.dsem() for _ in range(4)] for _ in range(2)]
    sqr = Ring(ph, "sq", 3, [128, 8, 512], BF16)
    rsr = Ring(ph, "rs", 3, [128, 512], F32)
    tmr = Ring(ph, "tm", 4, [128, 512], F32)
    osr = Ring(ph, "os", 3, [128, 8, 512], BF16, dma=True)
    psr = Ring(ph, "ps", 8, bufs=P.psb)
    hv = C.hT.rearrange("(kc p) t -> p kc t", p=128)
    xv = C.xnT.rearrange("(kc p) t -> p kc t", p=128)

    def stage_a(t0, n):
        hs, hb, hdeps = hr.next()
        s, ps, pdeps = psr.next()
        ltoks = []
        pet = None
        for q in range(0, KC, 8):
            lt = ph.dma(hb[:, q:q + 8, :n], hv[:, q:q + 8, t0:t0 + n], hsem[hs][q // 8], deps=hdeps)
            ltoks.append(lt)
        for q in range(0, KC, 8):
            ss, sq, sdeps = sqr.next()
            ta = ph.op("scalar", lambda e, sq=sq, hb=hb, q=q, n=n: e.activation(out=sq[:, :, :n], in_=hb[:, q:q + 8, :n], func=AF.Square),
                       deps=[ltoks[q // 8]] + sdeps)
            for j in range(8):
                lastk = (q + j == KC - 1)
                pet = ph.op("tensor", lambda e, ps=ps, sq=sq, j=j, n=n, q=q, lastk=lastk: e.matmul(
                    ps[:, :n], lhsT=C.ones_bf[:], rhs=sq[:, j, :n], start=(q + j == 0), stop=lastk),
                    deps=([ta] + (pdeps if q == 0 else [])) if j == 0 else (), signal=(j == 7))
            sqr.release(ss, pet)
        rs_, rs, rdeps = rsr.next()
        t1 = ph.op("scalar", lambda e, rs=rs, ps=ps, n=n: e.activation(out=rs[:, :n], in_=ps[:, :n], func=AF.Sqrt, bias=C.epsc[:, 0:1],
                                                                      scale=1.0 / D), deps=[pet] + rdeps)
        psr.release(s, t1)
        t2 = ph.op("vector", lambda e, rs=rs, n=n: e.reciprocal(out=rs[:, :n], in_=rs[:, :n]), deps=[t1])
        return (t0, n, hs, hb, ltoks, rs_, rs, t2)

    def stage_b(info):
        t0, n, hs, hb, ltoks, rs_, rs, t2 = info
        ci = 1 if t0 >= SEQ else 0
        lastact = None
        for q in range(0, KC, 8):
            os_, ost, odeps = osr.next()
            for j in range(8):
                kc = q + j
                ts_, tm, tdeps = tmr.next()
                tv = ph.op("vector", lambda e, tm=tm, hb=hb, kc=kc, rs=rs, n=n: e.tensor_tensor(out=tm[:, :n], in0=hb[:, kc, :n],
                                                                                              in1=rs[:, :n], op=ALU.mult),
                           deps=[t2, ltoks[q // 8]] + tdeps)
                lastact = ph.op("scalar", lambda e, ost=ost, j=j, tm=tm, kc=kc, n=n, ci=ci: e.activation(
                    out=ost[:, j, :n], in_=tm[:, :n], func=AF.Identity, bias=AB[:, 2 * ci + 1, kc:kc + 1],
                    scale=AB[:, 2 * ci, kc:kc + 1]), deps=[tv, abt] + (odeps if j == 0 else []))
                tmr.release(ts_, lastact)
            dt_ = ph.dma(xv[:, q:q + 8, t0:t0 + n], ost[:, :, :n], osr.ds[os_], deps=[lastact])
            osr.release(os_, dt_)
        hr.release(hs, lastact)
        rsr.release(rs_, lastact)

    infos = [stage_a(*SUBT[0])]
    for i in range(len(SUBT)):
        if i + 1 < len(SUBT):
            infos.append(stage_a(*SUBT[i + 1]))
        stage_b(infos[i])
    ph.run()


def rmsrope_phase(P, C, items):
    ph = Phase(P, "rr")
    tabH = Ring(ph, "tabH", 2, [128, 2, 512], F32, dma=True)
    tabM = Ring(ph, "tabM", 2, [64, 2, 512], F32, dma=True)
    zr = Ring(ph, "z", 16, [128, 512], BF16, dma=True)
    sqr = Ring(ph, "sq", 8, [128, 512], BF16)
    rsr = Ring(ph, "rs", 6, [128, 512], F32)
    znr = Ring(ph, "zn", 6, [128, 512], BF16)
    t1r = Ring(ph, "t1", 6, [128, 512], F32)
    t2r = Ring(ph, "t2", 6, [128, 512], F32)
    osr = Ring(ph, "os", 16, [128, 512], BF16, dma=True)
    psr = Ring(ph, "ps", 8, bufs=P.psb)
    work = [(si, it) for si in range(len(SUBT)) for it in items]
    tabs = {}
    st = {}

    def stage_a(u):
        si, it = work[u]
        t0, n = SUBT[si]
        if si not in tabs:
            hs, tH, hd = tabH.next()
            tokH = ph.dma(tH[:, :, :n], C.ropeH.rearrange("c p t -> p c t")[:, :, t0:t0 + n], tabH.ds[hs], deps=hd)
            ms, tM, md = tabM.next()
            tokM = ph.dma(tM[:, :, :n], C.ropeM.rearrange("c p t -> p c t")[:, :, t0:t0 + n], tabM.ds[ms], deps=md)
            tabs[si] = (hs, tH, tokH, ms, tM, tokM)
        nch = len(it["src"])
        s, ps, pdeps = psr.next()
        zs = []
        pet = None
        for j, (src, w) in enumerate(it["src"]):
            zs_, z, zd = zr.next()
            lt = ph.dma(z[:w, :n], src[:, t0:t0 + n], zr.ds[zs_], deps=zd)
            ss, sq, sd = sqr.next()
            ta = ph.op("scalar", lambda e, sq=sq, z=z, w=w, n=n: e.activation(out=sq[:w, :n], in_=z[:w, :n], func=AF.Square),
                       deps=[lt] + sd)
            pet = ph.op("tensor", lambda e, ps=ps, sq=sq, w=w, n=n, j=j, nch=nch: e.matmul(
                ps[:, :n], lhsT=C.ones_bf[:w, :], rhs=sq[:w, :n], start=(j == 0), stop=(j == nch - 1)),
                deps=[ta] + (pdeps if j == 0 else []))
            sqr.release(ss, pet)
            zs.append((zs_, z, lt, w))
        st[u] = dict(s=s, ps=ps, pet=pet, zs=zs)

    def stage_b(u):
        si, it = work[u]
        t0, n = SUBT[si]
        d = st[u]
        ps, pet = d["ps"], d["pet"]
        rs_, rs, rd = rsr.next()
        tq = ph.op("scalar", lambda e, rs=rs, ps=ps, n=n, dim=it["dim"]: e.activation(
            out=rs[:, :n], in_=ps[:, :n], func=AF.Ln, bias=C.epsc[:, 0:1], scale=1.0 / dim), deps=[pet] + rd)
        psr.release(d["s"], tq)
        tr = ph.op("scalar", lambda e, rs=rs, n=n: e.activation(out=rs[:, :n], in_=rs[:, :n], func=AF.Exp, scale=-0.5), deps=[tq])
        d["rs"] = (rs_, rs)
        d["cwork"] = []
        lastu = tr
        for j, (zs_, z, lt, w) in enumerate(d["zs"]):
            g = it["g"][j]
            rope = it["rope"][j]
            os_, ost, od = osr.next()
            if rope is None:
                tn = ph.op("vector", lambda e, ost=ost, z=z, g=g, rs=rs, w=w, n=n: e.scalar_tensor_tensor(
                    out=ost[:w, :n], in0=z[:w, :n], scalar=g, in1=rs[:w, :n], op0=ALU.mult, op1=ALU.mult), deps=[tr, lt] + od)
                zr.release(zs_, tn)
                dtk = ph.dma(it["dst"][j][:, t0:t0 + n], ost[:w, :n], osr.ds[os_], deps=[tn])
                osr.release(os_, dtk)
                lastu = tn
            else:
                ns_, zn, nd = znr.next()
                tn = ph.op("vector", lambda e, zn=zn, z=z, g=g, rs=rs, w=w, n=n: e.scalar_tensor_tensor(
                    out=zn[:w, :n], in0=z[:w, :n], scalar=g, in1=rs[:w, :n], op0=ALU.mult, op1=ALU.mult), deps=[tr, lt] + nd)
                zr.release(zs_, tn)
                s2, ps2, pd2 = psr.next()
                Rm = C.r128 if rope == "H" else C.r64
                tp = ph.op("tensor", lambda e, ps2=ps2, Rm=Rm, zn=zn, w=w, n=n: e.matmul(
                    ps2[:w, :n], lhsT=Rm[:w, :w], rhs=zn[:w, :n], start=True, stop=True), deps=[tn] + pd2)
                d["cwork"].append((j, w, rope, os_, ost, od, ns_, zn, tn, s2, ps2, tp))
                lastu = tn
        d["lastu"] = lastu

    def stage_c(u):
        si, it = work[u]
        t0, n = SUBT[si]
        d = st.pop(u)
        hs, tH, tokH, ms, tM, tokM = tabs[si]
        lastu = d["lastu"]
        for (j, w, rope, os_, ost, od, ns_, zn, tn, s2, ps2, tp) in d["cwork"]:
            tab, ttok = (tH, tokH) if rope == "H" else (tM, tokM)
            a_, t1, ad = t1r.next()
            ta1 = ph.op("gpsimd", lambda e, t1=t1, zn=zn, tab=tab, w=w, n=n: e.tensor_tensor(
                out=t1[:w, :n], in0=zn[:w, :n], in1=tab[:w, 0, :n], op=ALU.mult), deps=[tn, ttok] + ad)
            b_, t2, bd = t2r.next()
            ta2 = ph.op("vector", lambda e, t2=t2, ps2=ps2, tab=tab, w=w, n=n: e.tensor_tensor(
                out=t2[:w, :n], in0=ps2[:w, :n], in1=tab[:w, 1, :n], op=ALU.mult), deps=[tp, ttok] + bd)
            psr.release(s2, ta2)
            fin = ph.op("gpsimd", lambda e, ost=ost, t1=t1, t2=t2, w=w, n=n: e.tensor_tensor(
                out=ost[:w, :n], in0=t1[:w, :n], in1=t2[:w, :n], op=ALU.add), deps=[ta1, ta2] + od)
            znr.release(ns_, fin)
            t1r.release(a_, fin)
            t2r.release(b_, fin)
            dtk = ph.dma(it["dst"][j][:, t0:t0 + n], ost[:w, :n], osr.ds[os_], deps=[fin])
            osr.release(os_, dtk)
            lastu = fin
        rs_, rs = d["rs"]
        rsr.release(rs_, lastu)
        if u + 1 == len(work) or work[u + 1][0] != si:
            tabH.release(hs, lastu)
            tabM.release(ms, lastu)

    nw = len(work)
    for tau in range(nw + 4):
        if tau < nw:
            stage_a(tau)
        if 0 <= tau - 3 < nw:
            stage_b(tau - 3)
        if 0 <= tau - 4 < nw:
            stage_c(tau - 4)
    ph.run()


def attn_phase(P, C, vheads):
    ph = Phase(P, "attn")
    NKT = NT // 128
    LOOK, DEFER = ATT_LOOK, ATT_DEFER
    k128 = Ring(ph, "k128", 3, [128, NT], BF16, dma=True)
    k64 = Ring(ph, "k64", 3, [64, NT], BF16, dma=True)
    vr = Ring(ph, "v", 3, [128, NKT, 256], BF16, dma=True)
    q128 = Ring(ph, "q128", 3, [128, 512], BF16, dma=True)
    q64 = Ring(ph, "q64", 3, [64, 512], BF16, dma=True)
    pr = Ring(ph, "p", 6, [128, 512], BF16)
    rvr = Ring(ph, "rv", 3, [128, 512], F32)
    accR = Ring(ph, "acc", 4, bufs=P.psb[0:4])
    sR = Ring(ph, "s", 4, bufs=P.psb[4:8])
    saD = Ring(ph, "saD", 3, [128, 512], F32)
    saP = Ring(ph, "saP", 3, [128, 512], F32)
    ostb = Ring(ph, "ob", 4, [128, 512], BF16, dma=True)
    ostf = Ring(ph, "of", 4, [128, 512], F32, dma=True)

    groups = []
    for v in range(len(vheads)):
        for (t0, n) in SUBT:
            kts = list(range(NKT)) if t0 < SEQ else [NKT - 2, NKT - 1]
            groups.append(dict(v=v, t0=t0, n=n, kts=kts))
    tiles = [(gi, ti) for gi, g in enumerate(groups) for ti in range(len(g["kts"]))]
    kv = {}
    gst = {}
    pinfo = {}

    def load_kv(v):
        if v >= len(vheads) or v in kv:
            return
        vh = vheads[v]
        kparts = []
        for (src, w) in vh["k"]:
            ring = k128 if w == 128 else k64
            ks, kb, kd = ring.next()
            tk = None
            for q in range(0, NT, 1088):
                tk = ph.dma(kb[:w, q:q + 1088], src[:, q:q + 1088], ring.ds[ks], deps=kd)
            kparts.append((ring, ks, kb, tk, w))
        vs, vb, vd = vr.next()
        tv = None
        Vv = vh["V"].rearrange("(kt p) e -> p kt e", p=128)
        for q in range(0, NKT, 17):
            tv = ph.dma(vb[:, q:q + 17, :vh["ew"]], Vv[:, q:q + 17, :], vr.ds[vs], deps=vd)
        kv[v] = dict(kparts=kparts, vs=vs, vb=vb, tv=tv)

    def start_group(gi):
        if gi >= len(groups) or gi in gst:
            return
        g = groups[gi]
        vh = vheads[g["v"]]
        qparts = []
        for (src, w) in vh["q"]:
            ring = q128 if w == 128 else q64
            qs, qb, qd = ring.next()
            tq = ph.dma(qb[:w, :g["n"]], src[:, g["t0"]:g["t0"] + g["n"]], ring.ds[qs], deps=qd)
            qparts.append((ring, qs, qb, tq, w))
        gst[gi] = dict(qparts=qparts, accs=None, sa={}, lastpv=None)

    def emit_s(idx):
        gi, ti = tiles[idx]
        g = groups[gi]
        v = g["v"]
        vh = vheads[v]
        n = g["n"]
        if ti == 0:
            start_group(gi)
            start_group(gi + 1)
            if gi == 0 or groups[gi - 1]["v"] != v:
                load_kv(v)
                load_kv(v + 1)
        kt = g["kts"][ti]
        kparts = kv[v]["kparts"]
        qparts = gst[gi]["qparts"]
        s, ps, pd = sR.next()
        pet = None
        npart = len(kparts)
        for j in range(npart):
            _, _, kb, tk, w = kparts[j]
            _, _, qb, tq, _ = qparts[j]
            pet = ph.op("tensor", lambda e, ps=ps, kb=kb, qb=qb, w=w, kt=kt, n=n, j=j, npart=npart: e.matmul(
                ps[:, :n], lhsT=kb[:w, kt * 128:(kt + 1) * 128], rhs=qb[:w, :n], start=(j == 0), stop=(j == npart - 1)),
                deps=([tk, tq] + (pd if j == 0 else [])), signal=(j == npart - 1))
        p_, pb, ppd = pr.next()
        ta = ph.op("scalar", lambda e, pb=pb, ps=ps, n=n, sc=vh["scale"]: e.activation(out=pb[:, :n], in_=ps[:, :n], func=AF.Exp,
                                                                                      scale=sc), deps=[pet] + ppd)
        sR.release(s, ta)
        pinfo[idx] = (p_, pb, ta)
        if ti == len(g["kts"]) - 1:
            for (ring, qs, qb, tq, w) in qparts:
                ring.release(qs, pet)
            if gi + 1 == len(groups) or groups[gi + 1]["v"] != v:
                for (ring, ks, kb, tk, w) in kparts:
                    ring.release(ks, pet)

    def emit_pv(idx):
        gi, ti = tiles[idx]
        g = groups[gi]
        v = g["v"]
        vh = vheads[v]
        n = g["n"]
        nec = vh["ew"] // 128
        st_ = gst[gi]
        kt = g["kts"][ti]
        first = ti == 0
        last = ti == len(g["kts"]) - 1
        if first:
            st_["accs"] = [accR.next() for _ in range(nec)]
        p_, pb, ta = pinfo.pop(idx)
        vb, tv = kv[v]["vb"], kv[v]["tv"]
        pet = None
        for ec in range(nec):
            a_, acc, ad = st_["accs"][ec]
            lhs = vb[:, kt, ec * 128:(ec + 1) * 128]
            pet = ph.op("tensor", lambda e, acc=acc, lhs=lhs, pb=pb, n=n, first=first, last=last: e.matmul(
                acc[:, :n], lhsT=lhs, rhs=pb[:, :n], start=first, stop=last),
                deps=[ta, tv] + (ad if first else []), signal=(ec == nec - 1))
        eng = "gpsimd" if (ATT_POOL and ti % 3 == 2) else "vector"
        if eng not in st_["sa"]:
            ring = saP if eng == "gpsimd" else saD
            x_, sa, xd = ring.next()
            tacc = ph.op(eng, lambda e, sa=sa, pb=pb, n=n: e.tensor_copy(out=sa[:, :n], in_=pb[:, :n]), deps=[ta] + xd)
            st_["sa"][eng] = [ring, x_, sa, tacc]
        else:
            rec = st_["sa"][eng]
            sa = rec[2]
            rec[3] = ph.op(eng, lambda e, sa=sa, pb=pb, n=n: e.tensor_tensor(out=sa[:, :n], in0=sa[:, :n], in1=pb[:, :n], op=ALU.add),
                           deps=[ta, rec[3]])
            tacc = rec[3]
        pr.release(p_, pet)
        pr.release(p_, tacc)
        st_["lastpv"] = pet
        if last and (gi + 1 == len(groups) or groups[gi + 1]["v"] != v):
            vr.release(kv[v]["vs"], pet)

    def finalize(gi):
        g = groups[gi]
        vh = vheads[g["v"]]
        n, t0 = g["n"], g["t0"]
        nec = vh["ew"] // 128
        st_ = gst.pop(gi)
        s, ps, pd = sR.next()
        recs = list(st_["sa"].values())
        tsum = None
        for k, (ring, x_, sa, tacc) in enumerate(recs):
            tsum = ph.op("tensor", lambda e, ps=ps, sa=sa, n=n, k=k, nr=len(recs): e.matmul(
                ps[:, :n], lhsT=C.ones32[:], rhs=sa[:, :n], start=(k == 0), stop=(k == nr - 1)),
                deps=[tacc] + (pd if k == 0 else []), signal=(k == len(recs) - 1))
        for (ring, x_, sa, tacc) in recs:
            ring.release(x_, tsum)
        r_, rv, rd = rvr.next()
        t1 = ph.op("vector", lambda e, rv=rv, ps=ps, n=n: e.reciprocal(out=rv[:, :n], in_=ps[:, :n]), deps=[tsum] + rd)
        sR.release(s, t1)
        lt = None
        for ec in range(nec):
            a_, acc, _ = st_["accs"][ec]
            oring = ostb if vh["dt"] == BF16 else ostf
            o_, ob, od = oring.next()
            lt = ph.op("vector", lambda e, ob=ob, acc=acc, rv=rv, n=n: e.tensor_tensor(out=ob[:, :n], in0=acc[:, :n], in1=rv[:, :n],
                                                                                     op=ALU.mult), deps=[t1, st_["lastpv"]] + od)
            accR.release(a_, lt)
            dk = ph.dma(vh["dst"](ec)[:, t0:t0 + n], ob[:, :n], oring.ds[o_], deps=[lt])
            oring.release(o_, dk)
        rvr.release(r_, lt)

    ntl = len(tiles)
    finals = []
    for idx in range(min(LOOK, ntl)):
        emit_s(idx)
    for i in range(ntl):
        if i + LOOK < ntl:
            emit_s(i + LOOK)
        while finals and finals[0][0] <= i:
            finalize(finals.pop(0)[1])
        emit_pv(i)
        gi, ti = tiles[i]
        if ti == len(groups[gi]["kts"]) - 1:
            finals.append((i + DEFER, gi))
    while finals:
        finalize(finals.pop(0)[1])
    ph.run()


def swa_phase(P, C):
    ph = Phase(P, "swa")
    NKT = NT // 128
    NB = SEQ // 128
    scale = 128 ** -0.5
    kr = Ring(ph, "k", 2, [128, NT], BF16, dma=True)
    vr = Ring(ph, "v", 2, [128, NKT, 128], BF16, dma=True)
    qr = Ring(ph, "q", 2, [128, 3, NT], BF16, dma=True)
    pr = Ring(ph, "p", 6, [128, 384], BF16)
    dnr = Ring(ph, "dn", 3, [128, 128], F32)
    osr = Ring(ph, "os", 4, [128, 128], BF16, dma=True)
    accR = Ring(ph, "acc", 4, bufs=P.psb[0:4])
    sR = Ring(ph, "s", 4, bufs=P.psb[4:8])
    for g in range(4):
        ks, kb, kd = kr.next()
        tk = None
        for q in range(0, NT, 1088):
            tk = ph.dma(kb[:, q:q + 1088], C.skT[g * 128:(g + 1) * 128, q:q + 1088], kr.ds[ks], deps=kd if q == 0 else [])
        vs, vb, vd = vr.next()
        tv = None
        Vv = C.svV[:, g * 128:(g + 1) * 128].rearrange("(kt p) e -> p kt e", p=128)
        for q in range(0, NKT, 17):
            tv = ph.dma(vb[:, q:q + 17, :], Vv[:, q:q + 17, :], vr.ds[vs], deps=vd if q == 0 else [])
        qs, qb, qd = qr.next()
        tq = None
        for r in range(3):
            h = 3 * g + r
            tq = ph.dma(qb[:, r, :], C.sqT[h * 128:(h + 1) * 128, :], qr.ds[qs], deps=qd if r == 0 else [])
        lastpe = None
        for nb in range(NKT):
            if nb < NB:
                kts = []
                if nb > 0:
                    kts.append((nb - 1, 0))
                kts.append((nb, None))
                if nb < NB - 1:
                    kts.append((nb + 1, 1))
                kts += [(NKT - 2, None), (NKT - 1, None)]
            else:
                kts = [(NKT - 2, None), (NKT - 1, None)]
            a0, acc_o, ad0 = accR.next()
            a1, acc_s, ad1 = accR.next()
            pinfos = []
            for (kt, mk) in kts:
                s, ps, pd = sR.next()
                pet = ph.op("tensor", lambda e, ps=ps, kb=kb, qb=qb, kt=kt, nb=nb: e.matmul(
                    ps[:, 0:384].rearrange("p (r q) -> p r q", r=3), lhsT=kb[:, kt * 128:(kt + 1) * 128],
                    rhs=qb[:, :, nb * 128:(nb + 1) * 128], start=True, stop=True), deps=[tk, tq] + pd)
                p_, pb, ppd = pr.next()
                ta = ph.op("scalar", lambda e, pb=pb, ps=ps: e.activation(out=pb[:, :], in_=ps[:, 0:384], func=AF.Exp, scale=scale),
                           deps=[pet] + ppd)
                sR.release(s, ta)
                if mk is not None:
                    ta = ph.op("gpsimd", lambda e, pb=pb, mk=mk: e.tensor_tensor(out=pb[:, :], in0=pb[:, :], in1=C.masks[:, mk, :],
                                                                                 op=ALU.mult), deps=[ta])
                pinfos.append((p_, pb, ta, kt))
            for i, (p_, pb, ta, kt) in enumerate(pinfos):
                first = i == 0
                last = i == len(pinfos) - 1
                ph.op("tensor", lambda e, acc_o=acc_o, vb=vb, kt=kt, pb=pb, first=first, last=last: e.matmul(
                    acc_o[:, 0:384], lhsT=vb[:, kt, :], rhs=pb[:, :], start=first, stop=last),
                    deps=[ta, tv] + (ad0 if first else []), signal=False)
                lastpe = ph.op("tensor", lambda e, acc_s=acc_s, pb=pb, first=first, last=last: e.matmul(
                    acc_s[:, 0:384], lhsT=C.ones_bf[:], rhs=pb[:, :], start=first, stop=last),
                    deps=(ad1 if first else []))
                pr.release(p_, lastpe)
            lt = None
            for r in range(3):
                h = 3 * g + r
                d_, dn, dd = dnr.next()
                t1 = ph.op("vector", lambda e, dn=dn, acc_s=acc_s, r=r, h=h: e.tensor_scalar(
                    out=dn[:, :], in0=acc_s[:, r * 128:(r + 1) * 128], scalar1=C.esink[:, h:h + 1], scalar2=None, op0=ALU.add),
                    deps=[lastpe] + dd)
                t2 = ph.op("vector", lambda e, dn=dn: e.reciprocal(out=dn[:, :], in_=dn[:, :]), deps=[t1])
                o_, ob, od = osr.next()
                lt = ph.op("vector", lambda e, ob=ob, acc_o=acc_o, dn=dn, r=r: e.tensor_tensor(
                    out=ob[:, :], in0=acc_o[:, r * 128:(r + 1) * 128], in1=dn[:, :], op=ALU.mult), deps=[t2] + od)
                dnr.release(d_, lt)
                dk = ph.dma(C.yT[h * 128:(h + 1) * 128, nb * 128:(nb + 1) * 128], ob[:, :], osr.ds[o_], deps=[lt])
                osr.release(o_, dk)
            accR.release(a0, lt)
            accR.release(a1, lt)
        kr.release(ks, lastpe)
        vr.release(vs, lastpe)
        qr.release(qs, lastpe)
    ph.run()


def dif_merge_phase(P, C):
    ph = Phase(P, "dmrg")
    yr = Ring(ph, "y", 8, [128, 512], F32, dma=True)
    ydr = Ring(ph, "yd", 4, [128, 512], F32)
    sqr = Ring(ph, "sq", 4, [128, 512], BF16)
    rsr = Ring(ph, "rs", 2, [128, 512], F32)
    osr = Ring(ph, "os", 4, [128, 512], BF16, dma=True)
    psr = Ring(ph, "ps", 8, bufs=P.psb)
    for h in range(5):
        for (t0, n) in SUBT:
            s, ps, pd = psr.next()
            yds = []
            pet = None
            for c in range(2):
                a_, ya, ad = yr.next()
                la = ph.dma(ya[:, :n], C.dyT[h, 0, c * 128:(c + 1) * 128, t0:t0 + n], yr.ds[a_], deps=ad)
                b_, yb, bd = yr.next()
                lb = ph.dma(yb[:, :n], C.dyT[h, 1, c * 128:(c + 1) * 128, t0:t0 + n], yr.ds[b_], deps=bd)
                d_, yd, dd = ydr.next()
                t1 = ph.op("vector", lambda e, yd=yd, yb=yb, ya=ya, n=n: e.scalar_tensor_tensor(
                    out=yd[:, :n], in0=yb[:, :n], scalar=C.lam[:, 1:2], in1=ya[:, :n], op0=ALU.mult, op1=ALU.add), deps=[la, lb] + dd)
                yr.release(a_, t1)
                yr.release(b_, t1)
                q_, sq, qd = sqr.next()
                t2 = ph.op("scalar", lambda e, sq=sq, yd=yd, n=n: e.activation(out=sq[:, :n], in_=yd[:, :n], func=AF.Square),
                           deps=[t1] + qd)
                pet = ph.op("tensor", lambda e, ps=ps, sq=sq, n=n, c=c: e.matmul(ps[:, :n], lhsT=C.ones_bf[:], rhs=sq[:, :n],
                                                                               start=(c == 0), stop=(c == 1)),
                            deps=[t2] + (pd if c == 0 else []))
                sqr.release(q_, pet)
                yds.append((d_, yd, t1))
            r_, rs, rd = rsr.next()
            tq = ph.op("scalar", lambda e, rs=rs, ps=ps, n=n: e.activation(out=rs[:, :n], in_=ps[:, :n], func=AF.Sqrt,
                                                                          bias=C.epsc[:, 0:1], scale=1.0 / 256), deps=[pet] + rd)
            psr.release(s, tq)
            tr = ph.op("vector", lambda e, rs=rs, n=n: e.reciprocal(out=rs[:, :n], in_=rs[:, :n]), deps=[tq])
            lt = None
            for c, (d_, yd, t1) in enumerate(yds):
                o_, ob, od = osr.next()
                lt = ph.op("vector", lambda e, ob=ob, yd=yd, rs=rs, n=n, c=c: e.scalar_tensor_tensor(
                    out=ob[:, :n], in0=yd[:, :n], scalar=C.subg[:, c:c + 1], in1=rs[:, :n], op0=ALU.mult, op1=ALU.mult),
                    deps=[tr, t1] + od)
                ydr.release(d_, lt)
                r0 = 2816 + h * 256 + c * 128
                dk = ph.dma(C.yT[r0:r0 + 128, t0:t0 + n], ob[:, :n], osr.ds[o_], deps=[lt])
                osr.release(o_, dk)
            rsr.release(r_, lt)
    ph.run()


def moe_gate_phase(P, C):
    ph = Phase(P, "gate")
    NKT = NT // 128
    lg = ph.sb("lg", [128, NKT, 8], F32)
    ds = ph.dsem()
    tl = ph.dma(lg[:], C.lgT.rearrange("(kt p) e -> p kt e", p=128), ds)
    combT = ph.sb("combT", [8, NT], F32)
    wk = Ring(ph, "wk", 3, [128, 48], F32)
    psr = Ring(ph, "ps", 8, bufs=P.psb)
    last = None
    for kt in range(NKT):
        w_, w, wd = wk.next()
        L = lg[:, kt, :]
        m1, eq1, l2, m2, eq2, dd, g1, cmb = (w[:, 0:1], w[:, 8:16], w[:, 16:24], w[:, 1:2], w[:, 24:32], w[:, 2:3], w[:, 3:4], w[:, 32:40])
        g2 = w[:, 4:5]
        t = ph.op("vector", lambda e, m1=m1, L=L: e.tensor_reduce(out=m1, in_=L, axis=AX.X, op=ALU.max), deps=[tl] + wd)
        t = ph.op("vector", lambda e, eq1=eq1, L=L, m1=m1: e.tensor_scalar(out=eq1, in0=L, scalar1=m1, scalar2=None, op0=ALU.is_equal), deps=[t])
        t = ph.op("vector", lambda e, l2=l2, eq1=eq1, L=L: e.scalar_tensor_tensor(out=l2, in0=eq1, scalar=-1e30, in1=L, op0=ALU.mult,
                                                                                 op1=ALU.add), deps=[t])
        t = ph.op("vector", lambda e, m2=m2, l2=l2: e.tensor_reduce(out=m2, in_=l2, axis=AX.X, op=ALU.max), deps=[t])
        t = ph.op("vector", lambda e, eq2=eq2, l2=l2, m2=m2: e.tensor_scalar(out=eq2, in0=l2, scalar1=m2, scalar2=None, op0=ALU.is_equal),
                  deps=[t])
        t = ph.op("vector", lambda e, dd=dd, m2=m2, m1=m1: e.tensor_tensor(out=dd, in0=m2, in1=m1, op=ALU.subtract), deps=[t])
        t = ph.op("scalar", lambda e, dd=dd: e.activation(out=dd, in_=dd, func=AF.Exp), deps=[t])
        t = ph.op("vector", lambda e, g1=g1, dd=dd: e.tensor_scalar(out=g1, in0=dd, scalar1=1.0, scalar2=None, op0=ALU.add), deps=[t])
        t = ph.op("vector", lambda e, g1=g1: e.reciprocal(out=g1, in_=g1), deps=[t])
        t = ph.op("vector", lambda e, g2=g2, dd=dd, g1=g1: e.tensor_tensor(out=g2, in0=dd, in1=g1, op=ALU.mult), deps=[t])
        t = ph.op("vector", lambda e, cmb=cmb, eq1=eq1, g1=g1: e.tensor_scalar(out=cmb, in0=eq1, scalar1=g1, scalar2=None, op0=ALU.mult),
                  deps=[t])
        t = ph.op("vector", lambda e, cmb=cmb, eq2=eq2, g2=g2: e.scalar_tensor_tensor(out=cmb, in0=eq2, scalar=g2, in1=cmb, op0=ALU.mult,
                                                                                      op1=ALU.add), deps=[t])
        s, ps, pd = psr.next()
        tp = ph.op("tensor", lambda e, ps=ps, cmb=cmb: e.transpose(out=ps[0:8, 0:128], in_=cmb, identity=C.ident[:]), deps=[t] + pd)
        last = ph.op("vector", lambda e, ps=ps, kt=kt: e.tensor_copy(out=combT[0:8, kt * 128:(kt + 1) * 128], in_=ps[0:8, 0:128]),
                     deps=[tp])
        psr.release(s, last)
        wk.release(w_, tp)
    osr = Ring(ph, "os", 3, [128, 512], F32, dma=True)
    for ex in range(NEXP):
        for (t0, n) in SUBT:
            s, ps, pd = psr.next()
            tp = ph.op("tensor", lambda e, ps=ps, ex=ex, t0=t0, n=n: e.matmul(ps[:, :n], lhsT=C.sel[0:8, ex, :], rhs=combT[0:8, t0:t0 + n],
                                                                             start=True, stop=True), deps=[last] + pd)
            o_, ob, od = osr.next()
            tc = ph.op("scalar", lambda e, ob=ob, ps=ps, n=n: e.activation(out=ob[:, :n], in_=ps[:, :n], func=AF.Copy), deps=[tp] + od)
            psr.release(s, tc)
            dk = ph.dma(C.cb[ex, :, t0:t0 + n], ob[:, :n], osr.ds[o_], deps=[tc])
            osr.release(o_, dk)
    ph.run()


def final_phase(P, C):
    ph = Phase(P, "fin")
    ds = ph.dsem()
    for q in range(0, D, 512):
        ph.dma(C.outT[q:q + 512, :], C.hT[q:q + 512, 0:SEQ], ds)
    ph.run()


GROUPS = [(0, 1024), (1024, 1024), (2048, 1024), (3072, 1280)]


def build(n_layers=DEPTH, dbg=(), stop=None, Lw=DEPTH):
    nc = bass.Bass("TRN2", target_bir_lowering=False)
    C = Ctx()
    L = Lw
    L2 = max(1, Lw // 2)

    def din(name, shape, dt=F32):
        return nc.dram_tensor(name, list(shape), dt, kind="ExternalInput").ap()

    def dscr(name, shape, dt):
        kind = "ExternalOutput" if name in dbg else "Internal"
        return nc.dram_tensor(name, list(shape), dt, kind=kind).ap()

    C.xT = din("xT", [D, NT])
    C.cvec = din("cvec", [128, KC, 2])
    C.ada_w = din("ada_w", [L, D, 6 * D])
    C.w_in = din("w_in", [L, D, IN_W])
    C.w_out = din("w_out", [L, D, D])
    C.w_uq = din("mla_w_uq", [L, 768, 1920])
    C.w_ukv = din("mla_w_ukv", [L, 512, 2560])
    C.ffn_w1 = din("ffn_w1", [L2, D, D])
    C.ffn_w3 = din("ffn_w3", [L2, D, D])
    C.ffn_w2 = din("ffn_w2", [L2, D, D])
    C.router = din("moe_router", [L2, D, NEXP])
    C.moe_w1 = din("moe_w1", [L2, NEXP, D, DFE])
    C.moe_w3 = din("moe_w3", [L2, NEXP, D, DFE])
    C.moe_w2 = din("moe_w2", [L2, NEXP, DFE, D])
    C.vecs = din("vecs", [L, 128, NV])
    C.ropeH = din("ropeH", [2, 128, NT])
    C.ropeM = din("ropeM", [2, 64, NT])
    C.c_ones = din("c_ones", [128, 128])
    C.c_r128 = din("c_r128", [128, 128])
    C.c_r64 = din("c_r64", [64, 64])
    C.c_ident = din("c_ident", [128, 128])
    C.c_sel = din("c_sel", [8, NEXP, 128])
    C.c_masks = din("c_masks", [128, 2, 384])
    C.outT = nc.dram_tensor("outT", [D, SEQ], F32, kind="ExternalOutput").ap()

    C.hT = dscr("hT", [D, NT], F32)
    C.xnT = dscr("xnT", [D, NT], BF16)
    C.zT = dscr("zT", [IN_W, NT], BF16)
    C.sqT = dscr("sqT", [1536, NT], BF16)
    C.skT = dscr("skT", [512, NT], BF16)
    C.svV = dscr("svV", [NT, 512], BF16)
    C.dvV = dscr("dvV", [NT, 1280], BF16)
    C.cqnT = dscr("cqnT", [768, NT], BF16)
    C.ckvnT = dscr("ckvnT", [512, NT], BF16)
    C.mqraw = dscr("mqraw", [1920, NT], BF16)
    C.mkraw = dscr("mkraw", [1280, NT], BF16)
    C.mvV = dscr("mvV", [NT, 1280], BF16)
    C.mqT = dscr("mqT", [1920, NT], BF16)
    C.mkT = dscr("mkT", [1920, NT], BF16)
    C.dqT = dscr("dqT", [1280, NT], BF16)
    C.dkT = dscr("dkT", [1280, NT], BF16)
    C.dyT = dscr("dyT", [5, 2, 256, NT], F32)
    C.yT = dscr("yT", [D, NT], BF16)
    C.hidT = dscr("hidT", [2 * D, NT], BF16)
    C.lgT = dscr("lgT", [NT, NEXP], F32)
    C.cb = dscr("cb", [NEXP, 128, NT], F32)

    P = Prog(nc)
    with ExitStack() as st:
        def sbp(name, shape, dt):
            return st.enter_context(nc.sbuf_tensor(name, list(shape), dt))

        P.psb = [st.enter_context(nc.psum_tensor("psb%d" % i, [128, 512], F32)) for i in range(8)]
        C.scsb = sbp("scsb", [128, KC, 2], BF16)
        C.modsb = sbp("modsb", [128, 6 * KC, 2], F32)
        C.vsb = sbp("vsb", [128, NV], F32)
        C.ones_bf = sbp("ones_bf", [128, 128], BF16)
        C.ones32 = sbp("ones32", [128, 128], F32)
        C.r128 = sbp("r128", [128, 128], BF16)
        C.r64 = sbp("r64", [64, 64], BF16)
        C.ident = sbp("ident", [128, 128], F32)
        C.sel = sbp("sel", [8, NEXP, 128], F32)
        C.masks = sbp("masks", [128, 2, 384], F32)
        C.lam = sbp("lam", [128, 2], F32)
        C.esink = sbp("esink", [128, 12], F32)
        C.subg = sbp("subg", [128, 2], F32)
        C.epsc = sbp("epsc", [128, 1], F32)

        count = [0]

        def go():
            count[0] += 1
            return stop is None or count[0] <= stop

        ph = Phase(P, "c0")
        ph.op("vector", lambda e: e.memset(C.epsc[:], EPS))
        ph.op("vector", lambda e: e.memset(C.ones32[:], 1.0))
        ph.run()
        init_phase(P, C)

        def zdst(t0, n, j, u):
            w = u["chunks"][j][1]
            return C.zT[u["f0"]:u["f0"] + w, t0:t0 + n]

        for l in range(n_layers):
            if not go(): break
            layer_vec_phase(P, C, l)
            if not go(): break
            mod_phase(P, C, l)
            if not go(): break
            norm_phase(P, C, l, 1)
            if not go(): break
            W = C.w_in[l]
            epiZ = EpiCopy(zdst)
            epiSV = EpiCopy(lambda t0, n, j, u: C.svV[t0:t0 + 128, u["f0"]:u["f0"] + n])
            epiDV = EpiCopy(lambda t0, n, j, u: C.dvV[t0:t0 + 128, u["f0"]:u["f0"] + n])
            blocks = (fblocks(W, 0, C_SV, epiZ) + tblocks(W, C_SV, C_CQ, epiSV) + fblocks(W, C_CQ, C_DQ, epiZ)
                      + fblocks(W, C_DQ, C_DV, epiZ) + tblocks(W, C_DV, IN_W, epiDV))
            linear(P, "inp", D, GROUPS, blocks, xT=C.xnT)
            if not go(): break
            items = []
            for h in range(12):
                items.append(dict(src=[(C.zT[h * 128:(h + 1) * 128, :], 128)], g=[C.vsb[:, V_SQN:V_SQN + 1]], dim=128, rope=["H"],
                                  dst=[C.sqT[h * 128:(h + 1) * 128, :]]))
            for g in range(4):
                r0 = C_SK + g * 128
                items.append(dict(src=[(C.zT[r0:r0 + 128, :], 128)], g=[C.vsb[:, V_SKN:V_SKN + 1]], dim=128, rope=["H"],
                                  dst=[C.skT[g * 128:(g + 1) * 128, :]]))
            for c in range(10):
                m = c % 2
                items.append(dict(src=[(C.zT[C_DQ + c * 128:C_DQ + (c + 1) * 128, :], 128)], g=[C.vsb[:, V_DQN + m:V_DQN + m + 1]],
                                  dim=128, rope=["H"], dst=[C.dqT[c * 128:(c + 1) * 128, :]]))
                items.append(dict(src=[(C.zT[C_DK + c * 128:C_DK + (c + 1) * 128, :], 128)], g=[C.vsb[:, V_DKN + m:V_DKN + m + 1]],
                                  dim=128, rope=["H"], dst=[C.dkT[c * 128:(c + 1) * 128, :]]))
            items.append(dict(src=[(C.zT[C_CQ + j * 128:C_CQ + (j + 1) * 128, :], 128) for j in range(6)],
                              g=[C.vsb[:, V_CQ + j:V_CQ + j + 1] for j in range(6)], dim=768, rope=[None] * 6,
                              dst=[C.cqnT[j * 128:(j + 1) * 128, :] for j in range(6)]))
            items.append(dict(src=[(C.zT[C_CKV + j * 128:C_CKV + (j + 1) * 128, :], 128) for j in range(4)],
                              g=[C.vsb[:, V_CKV + j:V_CKV + j + 1] for j in range(4)], dim=512, rope=[None] * 4,
                              dst=[C.ckvnT[j * 128:(j + 1) * 128, :] for j in range(4)]))
            rmsrope_phase(P, C, items)
            if not go(): break
            epq = EpiCopy(lambda t0, n, j, u: C.mqraw[u["f0"]:u["f0"] + u["chunks"][j][1], t0:t0 + n])
            linear(P, "upq", 768, GROUPS, fblocks(C.w_uq[l], 0, 1920, epq), xT=C.cqnT)
            if not go(): break
            epk = EpiCopy(lambda t0, n, j, u: C.mkraw[u["f0"]:u["f0"] + 128, t0:t0 + n])
            epv = EpiCopy(lambda t0, n, j, u: C.mvV[t0:t0 + 128, u["f0"]:u["f0"] + n])
            blocks = []
            for hp in range(5):
                units = []
                for r in range(2):
                    h = hp * 2 + r
                    units.append(dict(mode="F", chunks=[(r * 256, 128)], epi=epk, f0=h * 128))
                    units.append(dict(mode="T", chunks=[(r * 256 + 128, 128)], epi=epv, f0=h * 128))
                blocks.append(dict(segs=[(C.w_ukv[l], hp * 512, 512)], units=units))
            linear(P, "upkv", 512, GROUPS, blocks, xT=C.ckvnT)
            if not go(): break
            items = []
            for h in range(10):
                items.append(dict(src=[(C.mqraw[h * 192:h * 192 + 128, :], 128), (C.mqraw[h * 192 + 128:(h + 1) * 192, :], 64)],
                                  g=[C.vsb[:, V_MQN:V_MQN + 1], C.vsb[0:64, V_MQN + 1:V_MQN + 2]], dim=192, rope=[None, "M"],
                                  dst=[C.mqT[h * 192:h * 192 + 128, :], C.mqT[h * 192 + 128:(h + 1) * 192, :]]))
                items.append(dict(src=[(C.mkraw[h * 128:(h + 1) * 128, :], 128), (C.zT[C_KR:C_KR + 64, :], 64)],
                                  g=[C.vsb[:, V_MKN:V_MKN + 1], C.vsb[0:64, V_MKN + 1:V_MKN + 2]], dim=192, rope=[None, "M"],
                                  dst=[C.mkT[h * 192:h * 192 + 128, :], C.mkT[h * 192 + 128:(h + 1) * 192, :]]))
            rmsrope_phase(P, C, items)
            if not go(): break
            vheads = []
            for h in range(10):
                vheads.append(dict(q=[(C.mqT[h * 192:h * 192 + 128, :], 128), (C.mqT[h * 192 + 128:(h + 1) * 192, :], 64)],
                                   k=[(C.mkT[h * 192:h * 192 + 128, :], 128), (C.mkT[h * 192 + 128:(h + 1) * 192, :], 64)],
                                   V=C.mvV[:, h * 128:(h + 1) * 128], ew=128, scale=192 ** -0.5, dt=BF16,
                                   dst=(lambda ec, h=h: C.yT[1536 + h * 128:1536 + (h + 1) * 128, :])))
            for h in range(5):
                for m in range(2):
                    c = h * 2 + m
                    vheads.append(dict(q=[(C.dqT[c * 128:(c + 1) * 128, :], 128)], k=[(C.dkT[c * 128:(c + 1) * 128, :], 128)],
                                       V=C.dvV[:, h * 256:(h + 1) * 256], ew=256, scale=128 ** -0.5, dt=F32,
                                       dst=(lambda ec, h=h, m=m: C.dyT[h, m, ec * 128:(ec + 1) * 128, :])))
            attn_phase(P, C, vheads)
            if not go(): break
            swa_phase(P, C)
            if not go(): break
            dif_merge_phase(P, C)
            if not go(): break
            eo = EpiResid(C.hT, lambda fc, isc: C.modsb[:, 2 * KC + fc, (1 if isc else 0):(2 if isc else 1)])
            linear(P, "outp", D, GROUPS, fblocks(C.w_out[l], 0, D, eo), xT=C.yT)
            if not go(): break
            norm_phase(P, C, l, 2)
            if not go(): break
            e2 = EpiResid(C.hT, lambda fc, isc: C.modsb[:, 5 * KC + fc, (1 if isc else 0):(2 if isc else 1)])
            i = l // 2
            if l % 2 == 0:
                es = EpiSwiGLU(lambda t0, n, j, u: C.hidT[u["f0"]:u["f0"] + 128, t0:t0 + n])
                blocks = []
                for f in range(0, D, 256):
                    units = [dict(mode="F", chunks=[(o, 128), (256 + o, 128)], epi=es, f0=f + o) for o in (0, 128)]
                    blocks.append(dict(segs=[(C.ffn_w1[i], f, 256), (C.ffn_w3[i], f, 256)], units=units))
                linear(P, "ffu", D, GROUPS, blocks, xT=C.xnT)
                if not go(): break
                linear(P, "ffd", D, GROUPS, fblocks(C.ffn_w2[i], 0, D, e2), xT=C.hidT[0:D, :])
            else:
                er = EpiCopy(lambda t0, n, j, u: C.lgT[t0:t0 + 128, 0:n], dt=F32)
                linear(P, "rtr", D, GROUPS, tblocks(C.router[i], 0, NEXP, er), xT=C.xnT)
                if not go(): break
                moe_gate_phase(P, C)
                if not go(): break
                es = EpiSwiGLU(lambda t0, n, j, u: C.hidT[u["f0"]:u["f0"] + 128, t0:t0 + n],
                               cb=lambda t0, n, u: C.cb[u["ex"], :, t0:t0 + n])
                blocks = []
                for ex in range(NEXP):
                    for f in range(0, DFE, 256):
                        units = [dict(mode="F", chunks=[(o, 128), (256 + o, 128)], epi=es, f0=ex * DFE + f + o, ex=ex) for o in (0, 128)]
                        blocks.append(dict(segs=[(C.moe_w1[i, ex], f, 256), (C.moe_w3[i, ex], f, 256)], units=units))
                linear(P, "mou", D, GROUPS, blocks, xT=C.xnT)
                if not go(): break
                W2 = C.moe_w2[i].rearrange("e k d -> (e k) d")
                linear(P, "mod1", D, GROUPS, fblocks(W2[0:D, :], 0, D, e2), xT=C.hidT[0:D, :])
                e3 = EpiResid(C.hT, lambda fc, isc: C.modsb[:, 5 * KC + fc, (1 if isc else 0):(2 if isc else 1)])
                linear(P, "mod2", D, GROUPS, fblocks(W2[D:2 * D, :], 0, D, e3), xT=C.hidT[D:2 * D, :])
        final_phase(P, C)
    return nc


def _rope_tables(dim):
    rows = SEQ // GRID_W
    t = np.arange(SEQ)
    t_row = (t // GRID_W).astype(np.float32)
    t_col = (t % GRID_W).astype(np.float32)
    quarter = dim // 4
    inv = (np.float32(10000.0) ** (-np.arange(quarter, dtype=np.float32) / np.float32(quarter))).astype(np.float32)
    ang = np.concatenate([t_row[:, None] * inv, t_col[:, None] * inv], axis=-1).astype(np.float32)
    cos = np.cos(ang).astype(np.float32)
    sin = np.sin(ang).astype(np.float32)
    tab = np.zeros((2, dim, NT), np.float32)
    tab[0, :, SEQ:] = 1.0
    half = dim // 2
    tab[0, :half, :SEQ] = cos.T
    tab[0, half:, :SEQ] = cos.T
    tab[1, :half, :SEQ] = sin.T
    tab[1, half:, :SEQ] = sin.T
    return tab


def _rot_lhsT(dim):
    half = dim // 2
    m = np.zeros((dim, dim), np.float32)
    for i in range(half):
        m[i + half, i] = -1.0
        m[i, i + half] = 1.0
    return m


def _host_inputs(inp, L=DEPTH, ncores=NCORES):
    f = lambda a: np.ascontiguousarray(np.asarray(a, dtype=np.float32))
    vecs = np.zeros((L, 128, NV), np.float32)
    for l in range(L):
        vecs[l, :, V_N1:V_N1 + KC] = f(inp["norm1_g"])[l].reshape(KC, 128).T
        vecs[l, :, V_N2:V_N2 + KC] = f(inp["norm2_g"])[l].reshape(KC, 128).T
        vecs[l, :, V_ADAB:V_ADAB + 6 * KC] = f(inp["ada_b"])[l].reshape(6 * KC, 128).T
        vecs[l, :, V_CQ:V_CQ + 6] = f(inp["mla_cq_norm"])[l].reshape(6, 128).T
        vecs[l, :, V_CKV:V_CKV + 4] = f(inp["mla_ckv_norm"])[l].reshape(4, 128).T
        vecs[l, :, V_SQN] = f(inp["swa_q_norm"])[l]
        vecs[l, :, V_SKN] = f(inp["swa_k_norm"])[l]
        vecs[l, :, V_MQN] = f(inp["mla_q_norm"])[l][:128]
        vecs[l, :64, V_MQN + 1] = f(inp["mla_q_norm"])[l][128:]
        vecs[l, :, V_MKN] = f(inp["mla_k_norm"])[l][:128]
        vecs[l, :64, V_MKN + 1] = f(inp["mla_k_norm"])[l][128:]
        vecs[l, :, V_DQN:V_DQN + 2] = f(inp["dif_q_norm"])[l].T
        vecs[l, :, V_DKN:V_DKN + 2] = f(inp["dif_k_norm"])[l].T
        vecs[l, :, V_SUB:V_SUB + 2] = f(inp["dif_subln"])[l].reshape(2, 128).T
        vecs[l, :, V_LAM:V_LAM + 4] = f(inp["dif_lambda"])[l].T
        vecs[l, :, V_SINK:V_SINK + 12] = f(inp["swa_sink"])[l][None, :]
    masks = np.zeros((128, 2, 384), np.float32)
    k = np.arange(128)[:, None]
    q = np.arange(128)[None, :]
    masks[:, 0, :] = np.tile((q <= k).astype(np.float32), (1, 3))
    masks[:, 1, :] = np.tile((k <= q).astype(np.float32), (1, 3))
    sel = np.zeros((8, NEXP, 128), np.float32)
    for e in range(NEXP):
        sel[e, e, :] = 1.0
    shared = dict(
        ada_w=f(inp["ada_w"]), w_in=f(inp["w_in"]), w_out=f(inp["w_out"]), mla_w_uq=f(inp["mla_w_uq"]),
        mla_w_ukv=f(inp["mla_w_ukv"]), ffn_w1=f(inp["ffn_w1"]), ffn_w3=f(inp["ffn_w3"]), ffn_w2=f(inp["ffn_w2"]),
        moe_router=f(inp["moe_router"]), moe_w1=f(inp["moe_w1"]), moe_w3=f(inp["moe_w3"]), moe_w2=f(inp["moe_w2"]),
        vecs=vecs, ropeH=_rope_tables(128), ropeM=_rope_tables(64), c_ones=np.ones((128, 128), np.float32),
        c_r128=_rot_lhsT(128), c_r64=_rot_lhsT(64), c_ident=np.eye(128, dtype=np.float32), c_sel=sel, c_masks=masks)
    x = f(inp["x"])
    ctx = f(inp["ctx"])
    c = f(inp["c"])
    cc = f(inp["c_ctx"])
    maps = []
    for b in range(ncores):
        xT = np.ascontiguousarray(np.concatenate([x[b].T, ctx[b].T], axis=1))
        cvec = np.ascontiguousarray(np.stack([c[b].reshape(KC, 128).T, cc.reshape(KC, 128).T], axis=-1))
        m = dict(shared)
        m["xT"] = xT
        m["cvec"] = cvec
        maps.append(m)
    return maps


def kernel(**inputs):
    maps = _host_inputs(inputs)
    nc = build()
    res = run_bass_kernel_spmd(nc, maps, core_ids=list(range(NCORES)))
    out = np.stack([np.ascontiguousarray(res.results[b]["outT"].T) for b in range(NCORES)], axis=0)
    return out.astype(np.float32)
```

```python
import math
from contextlib import ExitStack

import numpy as np
import concourse.bass as bass
import concourse.mybir as mybir
from concourse.bass_utils import run_bass_kernel_spmd

F32 = mybir.dt.float32
BF16 = mybir.dt.bfloat16
AF = mybir.ActivationFunctionType
ALU = mybir.AluOpType
AX = mybir.AxisListType

D = 4096
KC = D // 128
SEQ = 4096
CTX = 256
NT = SEQ + CTX
DEPTH = 4
NCORES = 4
EPS = 1e-6
GRID_W = 64
IN_W = 7744
NEXP = 8
DFE = 1024

C_SQ, C_SK, C_SV, C_CQ, C_CKV, C_KR, C_DQ, C_DK, C_DV = 0, 1536, 2048, 2560, 3328, 3840, 3904, 5184, 6464

V_N1, V_N2, V_ADAB, V_CQ, V_CKV, V_SQN, V_SKN, V_MQN, V_MKN, V_DQN, V_DKN, V_SUB, V_LAM, V_SINK = (
    0, 32, 64, 256, 262, 266, 267, 268, 270, 272, 274, 276, 278, 282)
NV = 294

GROUPS = [(0, 1024), (1024, 1024), (2048, 1024), (3072, 1280)]
SUBT = [(i * 512, 512) for i in range(8)] + [(4096, 256)]

ENGS = ("sync", "scalar", "vector", "gpsimd", "tensor")
ATT_LOOK, ATT_DEFER, ATT_POOL = 2, 2, 1
NDS = 40


class Tok:
    __slots__ = ("sem", "val", "dma")

    def __init__(self, sem, val, dma=False):
        self.sem = sem
        self.val = val
        self.dma = dma


class Prog:
    def __init__(self, nc):
        self.nc = nc
        self.esem = {}
        self.ecnt = {}
        for e in ("scalar", "vector", "gpsimd", "tensor"):
            self.esem[e] = nc.alloc_semaphore(name="sem_" + e)
            self.ecnt[e] = 0
        self.dsem = [nc.alloc_semaphore(name="dsem%d" % i) for i in range(NDS)]
        self.dcnt = [0] * NDS
        self.dfree = list(range(NDS))
        self.nph = 0


class Phase:
    def __init__(self, P, name):
        self.P = P
        self.nc = P.nc
        P.nph += 1
        self.name = "%s%d" % (name, P.nph)
        self.tasks = {e: [] for e in ENGS}
        self.stack = ExitStack()
        self.my_ds = []
        self.nt = 0

    def sb(self, name, shape, dt):
        self.nt += 1
        return self.stack.enter_context(self.nc.sbuf_tensor("%s_%s%d" % (self.name, name, self.nt), list(shape), dt))

    def dsem(self):
        i = self.P.dfree.pop()
        self.my_ds.append(i)
        return i

    def op(self, eng, fn, deps=(), signal=True):
        P = self.P
        tok = None
        if signal:
            P.ecnt[eng] += 1
            tok = Tok(P.esem[eng], P.ecnt[eng])
        self.tasks[eng].append((fn, [d for d in deps if d is not None], tok))
        return tok

    def dma(self, out, in_, si, deps=(), q="sync"):
        P = self.P
        P.dcnt[si] += 16
        tok = Tok(P.dsem[si], P.dcnt[si], True)
        self.tasks[q].append((lambda e: e.dma_start(out=out, in_=in_), [d for d in deps if d is not None], tok))
        return tok

    def run(self):
        P = self.P
        finals = [Tok(P.dsem[i], P.dcnt[i], True) for i in self.my_ds]
        self.tasks["sync"].append((None, finals, None))
        with self.nc.Block() as blk:
            for eng in ENGS:
                tasks = self.tasks[eng]
                if not tasks:
                    continue

                def body(e, tasks=tasks):
                    waited = {}
                    for fn, deps, tok in tasks:
                        for d in deps:
                            k = id(d.sem)
                            if waited.get(k, -1) >= d.val:
                                continue
                            e.wait_ge(d.sem, d.val)
                            waited[k] = d.val
                        if fn is None:
                            continue
                        ins = fn(e)
                        if tok is not None:
                            ins.then_inc(tok.sem, 16 if tok.dma else 1)

                getattr(blk, eng)(body)
        self.stack.close()
        P.dfree.extend(self.my_ds)


class Ring:
    def __init__(self, ph, name, n, shape=None, dt=None, dma=False, bufs=None):
        self.n = n
        self.bufs = bufs if bufs is not None else [ph.sb("%s%d" % (name, i), shape, dt) for i in range(n)]
        self.free = [[] for _ in range(n)]
        self.ds = [ph.dsem() for _ in range(n)] if dma else None
        self.i = 0

    def next(self):
        s = self.i % self.n
        self.i += 1
        deps = self.free[s]
        self.free[s] = []
        return s, self.bufs[s], deps

    def release(self, s, tok):
        if tok is not None:
            self.free[s].append(tok)


def nsplits(t0, n):
    out = []
    while n > 0:
        m = min(512, n)
        out.append((t0, m))
        t0 += m
        n -= m
    return out


class EpiCopy:
    def __init__(self, dst, dt=BF16):
        self.dst = dst
        self.dt = dt

    def setup(self, ph, psr):
        self.psr = psr
        self.st = Ring(ph, "epst", 4, [128, 512], self.dt, dma=True)
        self.k = 0

    def __call__(self, ph, t0, n, tiles, petok, uinfo):
        for j, (s, ps, w) in enumerate(tiles):
            ss, stg, deps = self.st.next()
            eng = "scalar" if self.k % 2 == 0 else "vector"
            self.k += 1
            if eng == "scalar":
                tk = ph.op(eng, lambda e, stg=stg, ps=ps, w=w, n=n: e.activation(out=stg[:w, :n], in_=ps[:w, :n], func=AF.Copy),
                           deps=[petok] + deps)
            else:
                tk = ph.op(eng, lambda e, stg=stg, ps=ps, w=w, n=n: e.tensor_copy(out=stg[:w, :n], in_=ps[:w, :n]),
                           deps=[petok] + deps)
            self.psr.release(s, tk)
            dtok = ph.dma(self.dst(t0, n, j, uinfo), stg[:w, :n], self.st.ds[ss], deps=[tk])
            self.st.release(ss, dtok)


class EpiSwiGLU:
    def __init__(self, dst, cb=None):
        self.dst = dst
        self.cb = cb

    def setup(self, ph, psr):
        self.psr = psr
        self.sg = Ring(ph, "sg", 3, [128, 512], F32)
        self.st = Ring(ph, "epst", 3, [128, 512], BF16, dma=True)
        if self.cb is not None:
            self.cbr = Ring(ph, "cbr", 3, [128, 512], F32, dma=True)
            self.t2 = Ring(ph, "t2", 2, [128, 512], F32)

    def __call__(self, ph, t0, n, tiles, petok, uinfo):
        (sa, pa, w), (sb_, pb, _) = tiles
        s1, sg, d1 = self.sg.next()
        t1 = ph.op("scalar", lambda e: e.activation(out=sg[:w, :n], in_=pa[:w, :n], func=AF.Silu), deps=[petok] + d1)
        self.psr.release(sa, t1)
        ss, stg, d2 = self.st.next()
        if self.cb is None:
            t2 = ph.op("vector", lambda e: e.tensor_tensor(out=stg[:w, :n], in0=pb[:w, :n], in1=sg[:w, :n], op=ALU.mult),
                       deps=[petok, t1] + d2)
            self.psr.release(sb_, t2)
            self.sg.release(s1, t2)
        else:
            sc, cbt, d3 = self.cbr.next()
            tc = ph.dma(cbt[:w, :n], self.cb(t0, n, uinfo), self.cbr.ds[sc], deps=d3)
            sx, tx, d4 = self.t2.next()
            ta = ph.op("vector", lambda e: e.tensor_tensor(out=tx[:w, :n], in0=pb[:w, :n], in1=sg[:w, :n], op=ALU.mult),
                       deps=[petok, t1] + d4)
            self.psr.release(sb_, ta)
            self.sg.release(s1, ta)
            t2 = ph.op("gpsimd", lambda e: e.tensor_tensor(out=stg[:w, :n], in0=tx[:w, :n], in1=cbt[:w, :n], op=ALU.mult),
                       deps=[ta, tc] + d2)
            self.t2.release(sx, t2)
            self.cbr.release(sc, t2)
        dtok = ph.dma(self.dst(t0, n, 0, uinfo), stg[:w, :n], self.st.ds[ss], deps=[t2])
        self.st.release(ss, dtok)


class EpiResid:
    def __init__(self, hT, gcol):
        self.hT = hT
        self.gcol = gcol

    def setup(self, ph, psr):
        self.psr = psr
        self.hin = Ring(ph, "hin", 3, [128, 512], F32, dma=True)
        self.hout = Ring(ph, "hout", 3, [128, 512], F32, dma=True)

    def __call__(self, ph, t0, n, tiles, petok, uinfo):
        (s, ps, w), = tiles
        f0 = uinfo["f0"]
        si, hin, d1 = self.hin.next()
        tl = ph.dma(hin[:w, :n], self.hT[f0:f0 + w, t0:t0 + n], self.hin.ds[si], deps=d1)
        so, hout, d2 = self.hout.next()
        g = self.gcol(f0 // 128, t0 >= SEQ)
        tk = ph.op("vector", lambda e: e.scalar_tensor_tensor(out=hout[:w, :n], in0=ps[:w, :n], scalar=g, in1=hin[:w, :n],
                                                                 op0=ALU.mult, op1=ALU.add), deps=[petok, tl] + d2)
        self.psr.release(s, tk)
        self.hin.release(si, tk)
        dtok = ph.dma(self.hT[f0:f0 + w, t0:t0 + n], hout[:w, :n], self.hout.ds[so], deps=[tk])
        self.hout.release(so, dtok)


class EpiMod:
    def __init__(self, modsb, bcol):
        self.modsb = modsb
        self.bcol = bcol

    def setup(self, ph, psr):
        self.psr = psr

    def __call__(self, ph, t0, n, tiles, petok, uinfo):
        (s, ps, w), = tiles
        fc = uinfo["f0"] // 128
        tk = ph.op("vector", lambda e: e.tensor_scalar(out=self.modsb[:, fc, 0:2], in0=ps[:, 0:2], scalar1=self.bcol(fc),
                                                        scalar2=None, op0=ALU.add), deps=[petok])
        self.psr.release(s, tk)


def linear(P, name, K, groups, blocks, xT=None, x_sb=None, cast_engs=("gpsimd", "vector", "scalar", "vector"), stgcfg=(3, 1024)):
    ph = Phase(P, name)
    KCn = K // 128
    gmax = max(g[1] for g in groups)
    psr = Ring(ph, "ps", 8, bufs=P.psb)
    epis = []
    for b in blocks:
        for u in b["units"]:
            if u["epi"] not in epis:
                epis.append(u["epi"])
    for ep in epis:
        ep.setup(ph, psr)
    if x_sb is None:
        X = ph.sb("X", [128, KCn, gmax], BF16)
        xds = ph.dsem()
    else:
        X = x_sb
    Wr = Ring(ph, "Wb", 2, [128, KCn, 512], BF16)
    Sr = Ring(ph, "Ws", stgcfg[0], [128, stgcfg[1]], F32, dma=True)

    steps = [(g, b) for g in range(len(groups)) for b in range(len(blocks))]
    xfree = []
    xtok = {}
    loaded = {}
    ncast = [0]

    def load_w(step):
        g, bi = steps[step]
        blk = blocks[bi]
        ws, Wb, wdeps = Wr.next()
        off = 0
        lasts = {}
        for (W, c0, w) in blk["segs"]:
            Wv = W.rearrange("(kc p) f -> p kc f", p=128)
            kcs = max(1, stgcfg[1] // w)
            k0 = 0
            while k0 < KCn:
                kn = min(kcs, KCn - k0)
                ss, stg, sdeps = Sr.next()
                sv = stg[:, 0:kn * w].rearrange("p (k w) -> p k w", w=w)
                td = ph.dma(sv, Wv[:, k0:k0 + kn, c0:c0 + w], Sr.ds[ss], deps=sdeps)
                ceng = cast_engs[ncast[0] % len(cast_engs)]
                ncast[0] += 1
                if ceng == "scalar":
                    tc = ph.op(ceng, lambda e, Wb=Wb, k0=k0, kn=kn, off=off, w=w, sv=sv: e.activation(
                        out=Wb[:, k0:k0 + kn, off:off + w], in_=sv, func=AF.Copy), deps=[td] + wdeps)
                else:
                    tc = ph.op(ceng, lambda e, Wb=Wb, k0=k0, kn=kn, off=off, w=w, sv=sv: e.tensor_copy(
                        out=Wb[:, k0:k0 + kn, off:off + w], in_=sv), deps=[td] + wdeps)
                Sr.release(ss, tc)
                lasts[ceng] = tc
                k0 += kn
            off += w
        loaded[step] = (ws, Wb, list(lasts.values()))

    load_w(0)
    for step, (g, bi) in enumerate(steps):
        g0, gn = groups[g]
        if bi == 0 and x_sb is None:
            xt = None
            for q in range(0, KCn, 8):
                qn = min(8, KCn - q)
                xt = ph.dma(X[:, q:q + qn, 0:gn], xT.rearrange("(kc p) t -> p kc t", p=128)[:, q:q + qn, g0:g0 + gn], xds,
                            deps=xfree if q == 0 else [])
            xfree = []
            xtok[g] = xt
        if step + 1 < len(steps):
            load_w(step + 1)
        ws, Wb, wtoks = loaded.pop(step)
        first_deps = wtoks + ([xtok[g]] if x_sb is None else [])
        lastpe = None
        for u in blocks[bi]["units"]:
            if u["mode"] == "F":
                for (t0, n) in nsplits(g0, gn):
                    tiles = []
                    pet = None
                    for (off, w) in u["chunks"]:
                        s, ps, pdeps = psr.next()
                        for kc in range(KCn):
                            lastk = kc == KCn - 1
                            pet = ph.op("tensor", lambda e, ps=ps, Wb=Wb, kc=kc, off=off, w=w, t0=t0, n=n, g0=g0, lastk=lastk:
                                        e.matmul(ps[:w, :n], lhsT=Wb[:, kc, off:off + w], rhs=X[:, kc, t0 - g0:t0 - g0 + n],
                                                 start=(kc == 0), stop=lastk),
                                        deps=(pdeps + first_deps) if kc == 0 else (), signal=lastk)
                        tiles.append((s, ps, w))
                    lastpe = pet
                    u["epi"](ph, t0, n, tiles, pet, u)
            else:
                (off, w), = u["chunks"]
                for tt in range(g0, g0 + gn, 128):
                    s, ps, pdeps = psr.next()
                    pet = None
                    for kc in range(KCn):
                        lastk = kc == KCn - 1
                        pet = ph.op("tensor", lambda e, ps=ps, Wb=Wb, kc=kc, off=off, w=w, tt=tt, g0=g0, lastk=lastk:
                                    e.matmul(ps[:, :w], lhsT=X[:, kc, tt - g0:tt - g0 + 128], rhs=Wb[:, kc, off:off + w],
                                             start=(kc == 0), stop=lastk),
                                    deps=(pdeps + first_deps) if kc == 0 else (), signal=lastk)
                    lastpe = pet
                    u["epi"](ph, tt, w, [(s, ps, 128)], pet, u)
        Wr.release(ws, lastpe)
        if bi == len(blocks) - 1:
            xfree = [lastpe]
    ph.run()


def fblocks(W, c0, c1, epi, base_f0=None, extra=None):
    out = []
    c = c0
    while c < c1:
        bw = min(512, c1 - c)
        units = []
        o = 0
        while o < bw:
            w = min(128, bw - o)
            u = dict(mode="F", chunks=[(o, w)], epi=epi, f0=(c + o) if base_f0 is None else base_f0 + (c + o - c0))
            if extra:
                u.update(extra)
            units.append(u)
            o += w
        out.append(dict(segs=[(W, c, bw)], units=units))
        c += bw
    return out


def tblocks(W, c0, c1, epi, dcol0=0):
    out = []
    c = c0
    while c < c1:
        bw = min(512, c1 - c)
        out.append(dict(segs=[(W, c, bw)], units=[dict(mode="T", chunks=[(0, bw)], epi=epi, f0=dcol0 + c - c0)]))
        c += bw
    return out


class Ctx:
    pass


def init_phase(P, C):
    ph = Phase(P, "init")
    ds = ph.dsem()
    for q in range(0, D, 512):
        ph.dma(C.hT[q:q + 512, :], C.xT[q:q + 512, :], ds)
    cv = ph.sb("cv", [128, KC, 2], F32)
    d2 = ph.dsem()
    t = ph.dma(cv[:], C.cvec, d2)
    ph.op("scalar", lambda e: e.activation(out=C.scsb[:], in_=cv[:], func=AF.Silu), deps=[t])
    d3 = ph.dsem()
    sts = []
    t = None
    for (dst, src) in ((C.ones_bf, C.c_ones), (C.r128, C.c_r128), (C.r64, C.c_r64)):
        st = ph.sb("cst", list(src.shape), F32)
        t = ph.dma(st[:], src, d3)
        sts.append((dst, st))
    for (dst, st) in sts:
        ph.op("vector", lambda e, dst=dst, st=st: e.tensor_copy(out=dst[:], in_=st[:]), deps=[t])
    d4 = ph.dsem()
    ph.dma(C.ident[:], C.c_ident, d4)
    ph.dma(C.sel[:], C.c_sel, d4)
    ph.dma(C.masks[:], C.c_masks, d4)
    ph.run()


def layer_vec_phase(P, C, l):
    ph = Phase(P, "vec")
    ds = ph.dsem()
    t = ph.dma(C.vsb[:], C.vecs[l], ds)
    lam_init = 0.8 - 0.6 * math.exp(-0.3 * l)
    pr = ph.sb("pr", [128, 2], F32)
    t1 = ph.op("vector", lambda e: e.tensor_tensor(out=pr[:, 0:1], in0=C.vsb[:, V_LAM:V_LAM + 1], in1=C.vsb[:, V_LAM + 1:V_LAM + 2],
                                                    op=ALU.mult), deps=[t])
    t2 = ph.op("vector", lambda e: e.tensor_tensor(out=pr[:, 1:2], in0=C.vsb[:, V_LAM + 2:V_LAM + 3], in1=C.vsb[:, V_LAM + 3:V_LAM + 4],
                                                    op=ALU.mult), deps=[t, t1])
    ps = P.psb[0]
    t3 = ph.op("tensor", lambda e: e.matmul(ps[:, 0:2], lhsT=C.ones32[:], rhs=pr[:, 0:2], start=True, stop=True), deps=[t2])
    ex = ph.sb("ex", [128, 2], F32)
    t4 = ph.op("scalar", lambda e: e.activation(out=ex[:], in_=ps[:, 0:2], func=AF.Exp), deps=[t3])
    t5 = ph.op("vector", lambda e: e.tensor_tensor(out=C.lam[:, 0:1], in0=ex[:, 0:1], in1=ex[:, 1:2], op=ALU.subtract), deps=[t4])
    t6 = ph.op("vector", lambda e: e.tensor_scalar(out=C.lam[:, 0:1], in0=C.lam[:, 0:1], scalar1=lam_init, scalar2=None, op0=ALU.add),
               deps=[t5])
    t7 = ph.op("vector", lambda e: e.tensor_scalar(out=C.lam[:, 1:2], in0=C.lam[:, 0:1], scalar1=-1.0, scalar2=None, op0=ALU.mult),
               deps=[t6])
    ph.op("scalar", lambda e: e.activation(out=C.esink[:], in_=C.vsb[:, V_SINK:V_SINK + 12], func=AF.Exp), deps=[t, t4])
    ph.op("vector", lambda e: e.tensor_scalar(out=C.subg[:], in0=C.vsb[:, V_SUB:V_SUB + 2], scalar1=1.0 - lam_init, scalar2=None,
                                              op0=ALU.mult), deps=[t, t7])
    ph.run()


def mod_phase(P, C, l):
    epi = EpiMod(C.modsb, lambda fc: C.vsb[:, V_ADAB + fc:V_ADAB + fc + 1])
    blocks = fblocks(C.ada_w[l], 0, 6 * D, epi)
    linear(P, "mod", D, [(0, 2)], blocks, x_sb=C.scsb, cast_engs=("gpsimd", "vector", "scalar"), stgcfg=(8, 2048))


def norm_phase(P, C, l, which):
    ph = Phase(P, "norm")
    gcol = V_N1 if which == 1 else V_N2
    sh_i, sc_i = (0, 1) if which == 1 else (3, 4)
    AB = ph.sb("AB", [128, 4, KC], F32)
    abt = None
    for ci in (0, 1):
        t = ph.op("vector", lambda e, ci=ci: e.tensor_scalar(out=AB[:, 2 * ci, :], in0=C.modsb[:, sc_i * KC:(sc_i + 1) * KC, ci],
                                                               scalar1=1.0, scalar2=None, op0=ALU.add), deps=[abt])
        t = ph.op("vector", lambda e, ci=ci: e.tensor_tensor(out=AB[:, 2 * ci, :], in0=AB[:, 2 * ci, :], in1=C.vsb[:, gcol:gcol + KC],
                                                               op=ALU.mult), deps=[t])
        abt = ph.op("vector", lambda e, ci=ci: e.tensor_copy(out=AB[:, 2 * ci + 1, :], in_=C.modsb[:, sh_i * KC:(sh_i + 1) * KC, ci]),
                    deps=[t])
    hr = Ring(ph, "hb", 2, [128, KC, 512], F32)
    hsem = [[ph.dsem() for _ in range(4)] for _ in range(2)]
    sqr = Ring(ph, "sq", 3, [128, 8, 512], BF16)
    rsr = Ring(ph, "rs", 3, [128, 512], F32)
    tmr = Ring(ph, "tm", 4, [128, 512], F32)
    osr = Ring(ph, "os", 3, [128, 8, 512], BF16, dma=True)
    psr = Ring(ph, "ps", 8, bufs=P.psb)
    hv = C.hT.rearrange("(kc p) t -> p kc t", p=128)
    xv = C.xnT.rearrange("(kc p) t -> p kc t", p=128)

    def stage_a(t0, n):
        hs, hb, hdeps = hr.next()
        s, ps, pdeps = psr.next()
        ltoks = []
        pet = None
        for q in range(0, KC, 8):
            lt = ph.dma(hb[:, q:q + 8, :n], hv[:, q:q + 8, t0:t0 + n], hsem[hs][q // 8], deps=hdeps)
            ltoks.append(lt)
        for q in range(0, KC, 8):
            ss, sq, sdeps = sqr.next()
            ta = ph.op("scalar", lambda e, sq=sq, hb=hb, q=q, n=n: e.activation(out=sq[:, :, :n], in_=hb[:, q:q + 8, :n], func=AF.Square),
                       deps=[ltoks[q // 8]] + sdeps)
            for j in range(8):
                lastk = (q + j == KC - 1)
                pet = ph.op("tensor", lambda e, ps=ps, sq=sq, j=j, n=n, q=q, lastk=lastk: e.matmul(
                    ps[:, :n], lhsT=C.ones_bf[:], rhs=sq[:, j, :n], start=(q + j == 0), stop=lastk),
                    deps=([ta] + (pdeps if q == 0 else [])) if j == 0 else (), signal=(j == 7))
            sqr.release(ss, pet)
        rs_, rs, rdeps = rsr.next()
        t1 = ph.op("scalar", lambda e, rs=rs, ps=ps, n=n: e.activation(out=rs[:, :n], in_=ps[:, :n], func=AF.Sqrt, bias=C.epsc[:, 0:1],
                                                                      scale=1.0 / D), deps=[pet] + rdeps)
        psr.release(s, t1)
        t2 = ph.op("vector", lambda e, rs=rs, n=n: e.reciprocal(out=rs[:, :n], in_=rs[:, :n]), deps=[t1])
        return (t0, n, hs, hb, ltoks, rs_, rs, t2)

    def stage_b(info):
        t0, n, hs, hb, ltoks, rs_, rs, t2 = info
        ci = 1 if t0 >= SEQ else 0
        lastact = None
        for q in range(0, KC, 8):
            os_, ost, odeps = osr.next()
            for j in range(8):
                kc = q + j
                ts_, tm, tdeps = tmr.next()
                tv = ph.op("vector", lambda e, tm=tm, hb=hb, kc=kc, rs=rs, n=n: e.tensor_tensor(out=tm[:, :n], in0=hb[:, kc, :n],
                                                                                              in1=rs[:, :n], op=ALU.mult),
                           deps=[t2, ltoks[q // 8]] + tdeps)
                lastact = ph.op("scalar", lambda e, ost=ost, j=j, tm=tm, kc=kc, n=n, ci=ci: e.activation(
                    out=ost[:, j, :n], in_=tm[:, :n], func=AF.Identity, bias=AB[:, 2 * ci + 1, kc:kc + 1],
                    scale=AB[:, 2 * ci, kc:kc + 1]), deps=[tv, abt] + (odeps if j == 0 else []))
                tmr.release(ts_, lastact)
            dt_ = ph.dma(xv[:, q:q + 8, t0:t0 + n], ost[:, :, :n], osr.ds[os_], deps=[lastact])
            osr.release(os_, dt_)
        hr.release(hs, lastact)
        rsr.release(rs_, lastact)

    infos = [stage_a(*SUBT[0])]
    for i in range(len(SUBT)):
        if i + 1 < len(SUBT):
            infos.append(stage_a(*SUBT[i + 1]))
        stage_b(infos[i])
    ph.run()


def rmsrope_phase(P, C, items):
    ph = Phase(P, "rr")
    tabH = Ring(ph, "tabH", 2, [128, 2, 512], F32, dma=True)
    tabM = Ring(ph, "tabM", 2, [64, 2, 512], F32, dma=True)
    zr = Ring(ph, "z", 16, [128, 512], BF16, dma=True)
    sqr = Ring(ph, "sq", 8, [128, 512], BF16)
    rsr = Ring(ph, "rs", 6, [128, 512], F32)
    znr = Ring(ph, "zn", 6, [128, 512], BF16)
    t1r = Ring(ph, "t1", 6, [128, 512], F32)
    t2r = Ring(ph, "t2", 6, [128, 512], F32)
    osr = Ring(ph, "os", 16, [128, 512], BF16, dma=True)
    psr = Ring(ph, "ps", 8, bufs=P.psb)
    work = [(si, it) for si in range(len(SUBT)) for it in items]
    tabs = {}
    st = {}

    def stage_a(u):
        si, it = work[u]
        t0, n = SUBT[si]
        if si not in tabs:
            hs, tH, hd = tabH.next()
            tokH = ph.dma(tH[:, :, :n], C.ropeH.rearrange("c p t -> p c t")[:, :, t0:t0 + n], tabH.ds[hs], deps=hd)
            ms, tM, md = tabM.next()
            tokM = ph.dma(tM[:, :, :n], C.ropeM.rearrange("c p t -> p c t")[:, :, t0:t0 + n], tabM.ds[ms], deps=md)
            tabs[si] = (hs, tH, tokH, ms, tM, tokM)
        nch = len(it["src"])
        s, ps, pdeps = psr.next()
        zs = []
        pet = None
        for j, (src, w) in enumerate(it["src"]):
            zs_, z, zd = zr.next()
            lt = ph.dma(z[:w, :n], src[:, t0:t0 + n], zr.ds[zs_], deps=zd)
            ss, sq, sd = sqr.next()
            ta = ph.op("scalar", lambda e, sq=sq, z=z, w=w, n=n: e.activation(out=sq[:w, :n], in_=z[:w, :n], func=AF.Square),
                       deps=[lt] + sd)
            pet = ph.op("tensor", lambda e, ps=ps, sq=sq, w=w, n=n, j=j, nch=nch: e.matmul(
                ps[:, :n], lhsT=C.ones_bf[:w, :], rhs=sq[:w, :n], start=(j == 0), stop=(j == nch - 1)),
                deps=[ta] + (pdeps if j == 0 else []))
            sqr.release(ss, pet)
            zs.append((zs_, z, lt, w))
        st[u] = dict(s=s, ps=ps, pet=pet, zs=zs)

    def stage_b(u):
        si, it = work[u]
        t0, n = SUBT[si]
        d = st[u]
        ps, pet = d["ps"], d["pet"]
        rs_, rs, rd = rsr.next()
        tq = ph.op("scalar", lambda e, rs=rs, ps=ps, n=n, dim=it["dim"]: e.activation(
            out=rs[:, :n], in_=ps[:, :n], func=AF.Ln, bias=C.epsc[:, 0:1], scale=1.0 / dim), deps=[pet] + rd)
        psr.release(d["s"], tq)
        tr = ph.op("scalar", lambda e, rs=rs, n=n: e.activation(out=rs[:, :n], in_=rs[:, :n], func=AF.Exp, scale=-0.5), deps=[tq])
        d["rs"] = (rs_, rs)
        d["cwork"] = []
        lastu = tr
        for j, (zs_, z, lt, w) in enumerate(d["zs"]):
            g = it["g"][j]
            rope = it["rope"][j]
            os_, ost, od = osr.next()
            if rope is None:
                tn = ph.op("vector", lambda e, ost=ost, z=z, g=g, rs=rs, w=w, n=n: e.scalar_tensor_tensor(
                    out=ost[:w, :n], in0=z[:w, :n], scalar=g, in1=rs[:w, :n], op0=ALU.mult, op1=ALU.mult), deps=[tr, lt] + od)
                zr.release(zs_, tn)
                dtk = ph.dma(it["dst"][j][:, t0:t0 + n], ost[:w, :n], osr.ds[os_], deps=[tn])
                osr.release(os_, dtk)
                lastu = tn
            else:
                ns_, zn, nd = znr.next()
                tn = ph.op("vector", lambda e, zn=zn, z=z, g=g, rs=rs, w=w, n=n: e.scalar_tensor_tensor(
                    out=zn[:w, :n], in0=z[:w, :n], scalar=g, in1=rs[:w, :n], op0=ALU.mult, op1=ALU.mult), deps=[tr, lt] + nd)
                zr.release(zs_, tn)
                s2, ps2, pd2 = psr.next()
                Rm = C.r128 if rope == "H" else C.r64
                tp = ph.op("tensor", lambda e, ps2=ps2, Rm=Rm, zn=zn, w=w, n=n: e.matmul(
                    ps2[:w, :n], lhsT=Rm[:w, :w], rhs=zn[:w, :n], start=True, stop=True), deps=[tn] + pd2)
                d["cwork"].append((j, w, rope, os_, ost, od, ns_, zn, tn, s2, ps2, tp))
                lastu = tn
        d["lastu"] = lastu

    def stage_c(u):
        si, it = work[u]
        t0, n = SUBT[si]
        d = st.pop(u)
        hs, tH, tokH, ms, tM, tokM = tabs[si]
        lastu = d["lastu"]
        for (j, w, rope, os_, ost, od, ns_, zn, tn, s2, ps2, tp) in d["cwork"]:
            tab, ttok = (tH, tokH) if rope == "H" else (tM, tokM)
            a_, t1, ad = t1r.next()
            ta1 = ph.op("gpsimd", lambda e, t1=t1, zn=zn, tab=tab, w=w, n=n: e.tensor_tensor(
                out=t1[:w, :n], in0=zn[:w, :n], in1=tab[:w, 0, :n], op=ALU.mult), deps=[tn, ttok] + ad)
            b_, t2, bd = t2r.next()
            ta2 = ph.op("vector", lambda e, t2=t2, ps2=ps2, tab=tab, w=w, n=n: e.tensor_tensor(
                out=t2[:w, :n], in0=ps2[:w, :n], in1=tab[:w, 1, :n], op=ALU.mult), deps=[tp, ttok] + bd)
            psr.release(s2, ta2)
            fin = ph.op("gpsimd", lambda e, ost=ost, t1=t1, t2=t2, w=w, n=n: e.tensor_tensor(
                out=ost[:w, :n], in0=t1[:w, :n], in1=t2[:w, :n], op=ALU.add), deps=[ta1, ta2] + od)
            znr.release(ns_, fin)
            t1r.release(a_, fin)
            t2r.release(b_, fin)
            dtk = ph.dma(it["dst"][j][:, t0:t0 + n], ost[:w, :n], osr.ds[os_], deps=[fin])
            osr.release(os_, dtk)
            lastu = fin
        rs_, rs = d["rs"]
        rsr.release(rs_, lastu)
        if u + 1 == len(work) or work[u + 1][0] != si:
            tabH.release(hs, lastu)
            tabM.release(ms, lastu)

    nw = len(work)
    for tau in range(nw + 4):
        if tau < nw:
            stage_a(tau)
        if 0 <= tau - 3 < nw:
            stage_b(tau - 3)
        if 0 <= tau - 4 < nw:
            stage_c(tau - 4)
    ph.run()


def attn_phase(P, C, vheads):
    ph = Phase(P, "attn")
    NKT = NT // 128
    LOOK, DEFER = ATT_LOOK, ATT_DEFER
    k128 = Ring(ph, "k128", 3, [128, NT], BF16, dma=True)
    k64 = Ring(ph, "k64", 3, [64, NT], BF16, dma=True)
    vr = Ring(ph, "v", 3, [128, NKT, 256], BF16, dma=True)
    q128 = Ring(ph, "q128", 3, [128, 512], BF16, dma=True)
    q64 = Ring(ph, "q64", 3, [64, 512], BF16, dma=True)
    pr = Ring(ph, "p", 6, [128, 512], BF16)
    rvr = Ring(ph, "rv", 3, [128, 512], F32)
    accR = Ring(ph, "acc", 4, bufs=P.psb[0:4])
    sR = Ring(ph, "s", 4, bufs=P.psb[4:8])
    saD = Ring(ph, "saD", 3, [128, 512], F32)
    saP = Ring(ph, "saP", 3, [128, 512], F32)
    ostb = Ring(ph, "ob", 4, [128, 512], BF16, dma=True)
    ostf = Ring(ph, "of", 4, [128, 512], F32, dma=True)

    groups = []
    for v in range(len(vheads)):
        for (t0, n) in SUBT:
            kts = list(range(NKT)) if t0 < SEQ else [NKT - 2, NKT - 1]
            groups.append(dict(v=v, t0=t0, n=n, kts=kts))
    tiles = [(gi, ti) for gi, g in enumerate(groups) for ti in range(len(g["kts"]))]
    kv = {}
    gst = {}
    pinfo = {}

    def load_kv(v):
        if v >= len(vheads) or v in kv:
            return
        vh = vheads[v]
        kparts = []
        for (src, w) in vh["k"]:
            ring = k128 if w == 128 else k64
            ks, kb, kd = ring.next()
            tk = None
            for q in range(0, NT, 1088):
                tk = ph.dma(kb[:w, q:q + 1088], src[:, q:q + 1088], ring.ds[ks], deps=kd)
            kparts.append((ring, ks, kb, tk, w))
        vs, vb, vd = vr.next()
        tv = None
        Vv = vh["V"].rearrange("(kt p) e -> p kt e", p=128)
        for q in range(0, NKT, 17):
            tv = ph.dma(vb[:, q:q + 17, :vh["ew"]], Vv[:, q:q + 17, :], vr.ds[vs], deps=vd)
        kv[v] = dict(kparts=kparts, vs=vs, vb=vb, tv=tv)

    def start_group(gi):
        if gi >= len(groups) or gi in gst:
            return
        g = groups[gi]
        vh = vheads[g["v"]]
        qparts = []
        for (src, w) in vh["q"]:
            ring = q128 if w == 128 else q64
            qs, qb, qd = ring.next()
            tq = ph.dma(qb[:w, :g["n"]], src[:, g["t0"]:g["t0"] + g["n"]], ring.ds[qs], deps=qd)
            qparts.append((ring, qs, qb, tq, w))
        gst[gi] = dict(qparts=qparts, accs=None, sa={}, lastpv=None)

    def emit_s(idx):
        gi, ti = tiles[idx]
        g = groups[gi]
        v = g["v"]
        vh = vheads[v]
        n = g["n"]
        if ti == 0:
            start_group(gi)
            start_group(gi + 1)
            if gi == 0 or groups[gi - 1]["v"] != v:
                load_kv(v)
                load_kv(v + 1)
        kt = g["kts"][ti]
        kparts = kv[v]["kparts"]
        qparts = gst[gi]["qparts"]
        s, ps, pd = sR.next()
        pet = None
        npart = len(kparts)
        for j in range(npart):
            _, _, kb, tk, w = kparts[j]
            _, _, qb, tq, _ = qparts[j]
            pet = ph.op("tensor", lambda e, ps=ps, kb=kb, qb=qb, w=w, kt=kt, n=n, j=j, npart=npart: e.matmul(
                ps[:, :n], lhsT=kb[:w, kt * 128:(kt + 1) * 128], rhs=qb[:w, :n], start=(j == 0), stop=(j == npart - 1)),
                deps=([tk, tq] + (pd if j == 0 else [])), signal=(j == npart - 1))
        p_, pb, ppd = pr.next()
        ta = ph.op("scalar", lambda e, pb=pb, ps=ps, n=n, sc=vh["scale"]: e.activation(out=pb[:, :n], in_=ps[:, :n], func=AF.Exp,
                                                                                      scale=sc), deps=[pet] + ppd)
        sR.release(s, ta)
        pinfo[idx] = (p_, pb, ta)
        if ti == len(g["kts"]) - 1:
            for (ring, qs, qb, tq, w) in qparts:
                ring.release(qs, pet)
            if gi + 1 == len(groups) or groups[gi + 1]["v"] != v:
                for (ring, ks, kb, tk, w) in kparts:
                    ring.release(ks, pet)

    def emit_pv(idx):
        gi, ti = tiles[idx]
        g = groups[gi]
        v = g["v"]
        vh = vheads[v]
        n = g["n"]
        nec = vh["ew"] // 128
        st_ = gst[gi]
        kt = g["kts"][ti]
        first = ti == 0
        last = ti == len(g["kts"]) - 1
        if first:
            st_["accs"] = [accR.next() for _ in range(nec)]
        p_, pb, ta = pinfo.pop(idx)
        vb, tv = kv[v]["vb"], kv[v]["tv"]
        pet = None
        for ec in range(nec):
            a_, acc, ad = st_["accs"][ec]
            lhs = vb[:, kt, ec * 128:(ec + 1) * 128]
            pet = ph.op("tensor", lambda e, acc=acc, lhs=lhs, pb=pb, n=n, first=first, last=last: e.matmul(
                acc[:, :n], lhsT=lhs, rhs=pb[:, :n], start=first, stop=last),
                deps=[ta, tv] + (ad if first else []), signal=(ec == nec - 1))
        per = 2 if vh["ew"] == 256 else 3
        eng = "gpsimd" if (ATT_POOL and ti % per == per - 1) else "vector"
        if eng not in st_["sa"]:
            ring = saP if eng == "gpsimd" else saD
            x_, sa, xd = ring.next()
            tacc = ph.op(eng, lambda e, sa=sa, pb=pb, n=n: e.tensor_copy(out=sa[:, :n], in_=pb[:, :n]), deps=[ta] + xd)
            st_["sa"][eng] = [ring, x_, sa, tacc]
        else:
            rec = st_["sa"][eng]
            sa = rec[2]
            rec[3] = ph.op(eng, lambda e, sa=sa, pb=pb, n=n: e.tensor_tensor(out=sa[:, :n], in0=sa[:, :n], in1=pb[:, :n], op=ALU.add),
                           deps=[ta, rec[3]])
            tacc = rec[3]
        pr.release(p_, pet)
        pr.release(p_, tacc)
        st_["lastpv"] = pet
        if last and (gi + 1 == len(groups) or groups[gi + 1]["v"] != v):
            vr.release(kv[v]["vs"], pet)

    def finalize(gi):
        g = groups[gi]
        vh = vheads[g["v"]]
        n, t0 = g["n"], g["t0"]
        nec = vh["ew"] // 128
        st_ = gst.pop(gi)
        s, ps, pd = sR.next()
        recs = list(st_["sa"].values())
        tsum = None
        for k, (ring, x_, sa, tacc) in enumerate(recs):
            tsum = ph.op("tensor", lambda e, ps=ps, sa=sa, n=n, k=k, nr=len(recs): e.matmul(
                ps[:, :n], lhsT=C.ones32[:], rhs=sa[:, :n], start=(k == 0), stop=(k == nr - 1)),
                deps=[tacc] + (pd if k == 0 else []), signal=(k == len(recs) - 1))
        for (ring, x_, sa, tacc) in recs:
            ring.release(x_, tsum)
        r_, rv, rd = rvr.next()
        t1 = ph.op("vector", lambda e, rv=rv, ps=ps, n=n: e.reciprocal(out=rv[:, :n], in_=ps[:, :n]), deps=[tsum] + rd)
        sR.release(s, t1)
        lt = None
        for ec in range(nec):
            a_, acc, _ = st_["accs"][ec]
            oring = ostb if vh["dt"] == BF16 else ostf
            o_, ob, od = oring.next()
            lt = ph.op("vector", lambda e, ob=ob, acc=acc, rv=rv, n=n: e.tensor_tensor(out=ob[:, :n], in0=acc[:, :n], in1=rv[:, :n],
                                                                                     op=ALU.mult), deps=[t1, st_["lastpv"]] + od)
            accR.release(a_, lt)
            dk = ph.dma(vh["dst"](ec)[:, t0:t0 + n], ob[:, :n], oring.ds[o_], deps=[lt])
            oring.release(o_, dk)
        rvr.release(r_, lt)

    ntl = len(tiles)
    finals = []
    for idx in range(min(LOOK, ntl)):
        emit_s(idx)
    for i in range(ntl):
        if i + LOOK < ntl:
            emit_s(i + LOOK)
        while finals and finals[0][0] <= i:
            finalize(finals.pop(0)[1])
        emit_pv(i)
        gi, ti = tiles[i]
        if ti == len(groups[gi]["kts"]) - 1:
            finals.append((i + DEFER, gi))
    while finals:
        finalize(finals.pop(0)[1])
    ph.run()


def swa_phase(P, C):
    ph = Phase(P, "swa")
    NKT = NT // 128
    NB = SEQ // 128
    scale = 128 ** -0.5
    kr = Ring(ph, "k", 2, [128, NT], BF16, dma=True)
    vr = Ring(ph, "v", 2, [128, NKT, 128], BF16, dma=True)
    qr = Ring(ph, "q", 2, [128, 3, NT], BF16, dma=True)
    pr = Ring(ph, "p", 6, [128, 384], BF16)
    dnr = Ring(ph, "dn", 3, [128, 128], F32)
    osr = Ring(ph, "os", 4, [128, 128], BF16, dma=True)
    accR = Ring(ph, "acc", 4, bufs=P.psb[0:4])
    sR = Ring(ph, "s", 4, bufs=P.psb[4:8])
    for g in range(4):
        ks, kb, kd = kr.next()
        tk = None
        for q in range(0, NT, 1088):
            tk = ph.dma(kb[:, q:q + 1088], C.skT[g * 128:(g + 1) * 128, q:q + 1088], kr.ds[ks], deps=kd if q == 0 else [])
        vs, vb, vd = vr.next()
        tv = None
        Vv = C.svV[:, g * 128:(g + 1) * 128].rearrange("(kt p) e -> p kt e", p=128)
        for q in range(0, NKT, 17):
            tv = ph.dma(vb[:, q:q + 17, :], Vv[:, q:q + 17, :], vr.ds[vs], deps=vd if q == 0 else [])
        qs, qb, qd = qr.next()
        tq = None
        for r in range(3):
            h = 3 * g + r
            tq = ph.dma(qb[:, r, :], C.sqT[h * 128:(h + 1) * 128, :], qr.ds[qs], deps=qd if r == 0 else [])
        lastpe = None
        for nb in range(NKT):
            if nb < NB:
                kts = []
                if nb > 0:
                    kts.append((nb - 1, 0))
                kts.append((nb, None))
                if nb < NB - 1:
                    kts.append((nb + 1, 1))
                kts += [(NKT - 2, None), (NKT - 1, None)]
            else:
                kts = [(NKT - 2, None), (NKT - 1, None)]
            a0, acc_o, ad0 = accR.next()
            a1, acc_s, ad1 = accR.next()
            pinfos = []
            for (kt, mk) in kts:
                s, ps, pd = sR.next()
                pet = ph.op("tensor", lambda e, ps=ps, kb=kb, qb=qb, kt=kt, nb=nb: e.matmul(
                    ps[:, 0:384].rearrange("p (r q) -> p r q", r=3), lhsT=kb[:, kt * 128:(kt + 1) * 128],
                    rhs=qb[:, :, nb * 128:(nb + 1) * 128], start=True, stop=True), deps=[tk, tq] + pd)
                p_, pb, ppd = pr.next()
                ta = ph.op("scalar", lambda e, pb=pb, ps=ps: e.activation(out=pb[:, :], in_=ps[:, 0:384], func=AF.Exp, scale=scale),
                           deps=[pet] + ppd)
                sR.release(s, ta)
                if mk is not None:
                    ta = ph.op("gpsimd", lambda e, pb=pb, mk=mk: e.tensor_tensor(out=pb[:, :], in0=pb[:, :], in1=C.masks[:, mk, :],
                                                                                 op=ALU.mult), deps=[ta])
                pinfos.append((p_, pb, ta, kt))
            for i, (p_, pb, ta, kt) in enumerate(pinfos):
                first = i == 0
                last = i == len(pinfos) - 1
                ph.op("tensor", lambda e, acc_o=acc_o, vb=vb, kt=kt, pb=pb, first=first, last=last: e.matmul(
                    acc_o[:, 0:384], lhsT=vb[:, kt, :], rhs=pb[:, :], start=first, stop=last),
                    deps=[ta, tv] + (ad0 if first else []), signal=False)
                lastpe = ph.op("tensor", lambda e, acc_s=acc_s, pb=pb, first=first, last=last: e.matmul(
                    acc_s[:, 0:384], lhsT=C.ones_bf[:], rhs=pb[:, :], start=first, stop=last),
                    deps=(ad1 if first else []))
                pr.release(p_, lastpe)
            lt = None
            for r in range(3):
                h = 3 * g + r
                d_, dn, dd = dnr.next()
                t1 = ph.op("vector", lambda e, dn=dn, acc_s=acc_s, r=r, h=h: e.tensor_scalar(
                    out=dn[:, :], in0=acc_s[:, r * 128:(r + 1) * 128], scalar1=C.esink[:, h:h + 1], scalar2=None, op0=ALU.add),
                    deps=[lastpe] + dd)
                t2 = ph.op("vector", lambda e, dn=dn: e.reciprocal(out=dn[:, :], in_=dn[:, :]), deps=[t1])
                o_, ob, od = osr.next()
                lt = ph.op("vector", lambda e, ob=ob, acc_o=acc_o, dn=dn, r=r: e.tensor_tensor(
                    out=ob[:, :], in0=acc_o[:, r * 128:(r + 1) * 128], in1=dn[:, :], op=ALU.mult), deps=[t2] + od)
                dnr.release(d_, lt)
                dk = ph.dma(C.yT[h * 128:(h + 1) * 128, nb * 128:(nb + 1) * 128], ob[:, :], osr.ds[o_], deps=[lt])
                osr.release(o_, dk)
            accR.release(a0, lt)
            accR.release(a1, lt)
        kr.release(ks, lastpe)
        vr.release(vs, lastpe)
        qr.release(qs, lastpe)
    ph.run()


def dif_merge_phase(P, C):
    ph = Phase(P, "dmrg")
    yr = Ring(ph, "y", 8, [128, 512], F32, dma=True)
    ydr = Ring(ph, "yd", 4, [128, 512], F32)
    sqr = Ring(ph, "sq", 4, [128, 512], BF16)
    rsr = Ring(ph, "rs", 2, [128, 512], F32)
    osr = Ring(ph, "os", 4, [128, 512], BF16, dma=True)
    psr = Ring(ph, "ps", 8, bufs=P.psb)
    for h in range(5):
        for (t0, n) in SUBT:
            s, ps, pd = psr.next()
            yds = []
            pet = None
            for c in range(2):
                a_, ya, ad = yr.next()
                la = ph.dma(ya[:, :n], C.dyT[h, 0, c * 128:(c + 1) * 128, t0:t0 + n], yr.ds[a_], deps=ad)
                b_, yb, bd = yr.next()
                lb = ph.dma(yb[:, :n], C.dyT[h, 1, c * 128:(c + 1) * 128, t0:t0 + n], yr.ds[b_], deps=bd)
                d_, yd, dd = ydr.next()
                t1 = ph.op("vector", lambda e, yd=yd, yb=yb, ya=ya, n=n: e.scalar_tensor_tensor(
                    out=yd[:, :n], in0=yb[:, :n], scalar=C.lam[:, 1:2], in1=ya[:, :n], op0=ALU.mult, op1=ALU.add), deps=[la, lb] + dd)
                yr.release(a_, t1)
                yr.release(b_, t1)
                q_, sq, qd = sqr.next()
                t2 = ph.op("scalar", lambda e, sq=sq, yd=yd, n=n: e.activation(out=sq[:, :n], in_=yd[:, :n], func=AF.Square),
                           deps=[t1] + qd)
                pet = ph.op("tensor", lambda e, ps=ps, sq=sq, n=n, c=c: e.matmul(ps[:, :n], lhsT=C.ones_bf[:], rhs=sq[:, :n],
                                                                               start=(c == 0), stop=(c == 1)),
                            deps=[t2] + (pd if c == 0 else []))
                sqr.release(q_, pet)
                yds.append((d_, yd, t1))
            r_, rs, rd = rsr.next()
            tq = ph.op("scalar", lambda e, rs=rs, ps=ps, n=n: e.activation(out=rs[:, :n], in_=ps[:, :n], func=AF.Sqrt,
                                                                          bias=C.epsc[:, 0:1], scale=1.0 / 256), deps=[pet] + rd)
            psr.release(s, tq)
            tr = ph.op("vector", lambda e, rs=rs, n=n: e.reciprocal(out=rs[:, :n], in_=rs[:, :n]), deps=[tq])
            lt = None
            for c, (d_, yd, t1) in enumerate(yds):
                o_, ob, od = osr.next()
                lt = ph.op("vector", lambda e, ob=ob, yd=yd, rs=rs, n=n, c=c: e.scalar_tensor_tensor(
                    out=ob[:, :n], in0=yd[:, :n], scalar=C.subg[:, c:c + 1], in1=rs[:, :n], op0=ALU.mult, op1=ALU.mult),
                    deps=[tr, t1] + od)
                ydr.release(d_, lt)
                r0 = 2816 + h * 256 + c * 128
                dk = ph.dma(C.yT[r0:r0 + 128, t0:t0 + n], ob[:, :n], osr.ds[o_], deps=[lt])
                osr.release(o_, dk)
            rsr.release(r_, lt)
    ph.run()


def moe_gate_phase(P, C):
    ph = Phase(P, "gate")
    NKT = NT // 128
    lg = ph.sb("lg", [128, NKT, 8], F32)
    ds = ph.dsem()
    tl = ph.dma(lg[:], C.lgT.rearrange("(kt p) e -> p kt e", p=128), ds)
    combT = ph.sb("combT", [8, NT], F32)
    wk = Ring(ph, "wk", 3, [128, 48], F32)
    psr = Ring(ph, "ps", 8, bufs=P.psb)
    last = None
    for kt in range(NKT):
        w_, w, wd = wk.next()
        L = lg[:, kt, :]
        m1, eq1, l2, m2, eq2, dd, g1, cmb = (w[:, 0:1], w[:, 8:16], w[:, 16:24], w[:, 1:2], w[:, 24:32], w[:, 2:3], w[:, 3:4], w[:, 32:40])
        g2 = w[:, 4:5]
        t = ph.op("vector", lambda e, m1=m1, L=L: e.tensor_reduce(out=m1, in_=L, axis=AX.X, op=ALU.max), deps=[tl] + wd)
        t = ph.op("vector", lambda e, eq1=eq1, L=L, m1=m1: e.tensor_scalar(out=eq1, in0=L, scalar1=m1, scalar2=None, op0=ALU.is_equal), deps=[t])
        t = ph.op("vector", lambda e, l2=l2, eq1=eq1, L=L: e.scalar_tensor_tensor(out=l2, in0=eq1, scalar=-1e30, in1=L, op0=ALU.mult,
                                                                                 op1=ALU.add), deps=[t])
        t = ph.op("vector", lambda e, m2=m2, l2=l2: e.tensor_reduce(out=m2, in_=l2, axis=AX.X, op=ALU.max), deps=[t])
        t = ph.op("vector", lambda e, eq2=eq2, l2=l2, m2=m2: e.tensor_scalar(out=eq2, in0=l2, scalar1=m2, scalar2=None, op0=ALU.is_equal),
                  deps=[t])
        t = ph.op("vector", lambda e, dd=dd, m2=m2, m1=m1: e.tensor_tensor(out=dd, in0=m2, in1=m1, op=ALU.subtract), deps=[t])
        t = ph.op("scalar", lambda e, dd=dd: e.activation(out=dd, in_=dd, func=AF.Exp), deps=[t])
        t = ph.op("vector", lambda e, g1=g1, dd=dd: e.tensor_scalar(out=g1, in0=dd, scalar1=1.0, scalar2=None, op0=ALU.add), deps=[t])
        t = ph.op("vector", lambda e, g1=g1: e.reciprocal(out=g1, in_=g1), deps=[t])
        t = ph.op("vector", lambda e, g2=g2, dd=dd, g1=g1: e.tensor_tensor(out=g2, in0=dd, in1=g1, op=ALU.mult), deps=[t])
        t = ph.op("vector", lambda e, cmb=cmb, eq1=eq1, g1=g1: e.tensor_scalar(out=cmb, in0=eq1, scalar1=g1, scalar2=None, op0=ALU.mult),
                  deps=[t])
        t = ph.op("vector", lambda e, cmb=cmb, eq2=eq2, g2=g2: e.scalar_tensor_tensor(out=cmb, in0=eq2, scalar=g2, in1=cmb, op0=ALU.mult,
                                                                                      op1=ALU.add), deps=[t])
        s, ps, pd = psr.next()
        tp = ph.op("tensor", lambda e, ps=ps, cmb=cmb: e.transpose(out=ps[0:8, 0:128], in_=cmb, identity=C.ident[:]), deps=[t] + pd)
        last = ph.op("vector", lambda e, ps=ps, kt=kt: e.tensor_copy(out=combT[0:8, kt * 128:(kt + 1) * 128], in_=ps[0:8, 0:128]),
                     deps=[tp])
        psr.release(s, last)
        wk.release(w_, tp)
    osr = Ring(ph, "os", 3, [128, 512], F32, dma=True)
    for ex in range(NEXP):
        for (t0, n) in SUBT:
            s, ps, pd = psr.next()
            tp = ph.op("tensor", lambda e, ps=ps, ex=ex, t0=t0, n=n: e.matmul(ps[:, :n], lhsT=C.sel[0:8, ex, :], rhs=combT[0:8, t0:t0 + n],
                                                                             start=True, stop=True), deps=[last] + pd)
            o_, ob, od = osr.next()
            tc = ph.op("scalar", lambda e, ob=ob, ps=ps, n=n: e.activation(out=ob[:, :n], in_=ps[:, :n], func=AF.Copy), deps=[tp] + od)
            psr.release(s, tc)
            dk = ph.dma(C.cb[ex, :, t0:t0 + n], ob[:, :n], osr.ds[o_], deps=[tc])
            osr.release(o_, dk)
    ph.run()


def final_phase(P, C):
    ph = Phase(P, "fin")
    ds = ph.dsem()
    for q in range(0, D, 512):
        ph.dma(C.outT[q:q + 512, :], C.hT[q:q + 512, 0:SEQ], ds)
    ph.run()


GROUPS = [(0, 1024), (1024, 1024), (2048, 1024), (3072, 1280)]


def build(n_layers=DEPTH, dbg=(), stop=None, Lw=DEPTH):
    nc = bass.Bass("TRN2", target_bir_lowering=False)
    C = Ctx()
    L = Lw
    L2 = max(1, Lw // 2)

    def din(name, shape, dt=F32):
        return nc.dram_tensor(name, list(shape), dt, kind="ExternalInput").ap()

    def dscr(name, shape, dt):
        kind = "ExternalOutput" if name in dbg else "Internal"
        return nc.dram_tensor(name, list(shape), dt, kind=kind).ap()

    C.xT = din("xT", [D, NT])
    C.cvec = din("cvec", [128, KC, 2])
    C.ada_w = din("ada_w", [L, D, 6 * D])
    C.w_in = din("w_in", [L, D, IN_W])
    C.w_out = din("w_out", [L, D, D])
    C.w_uq = din("mla_w_uq", [L, 768, 1920])
    C.w_ukv = din("mla_w_ukv", [L, 512, 2560])
    C.ffn_w1 = din("ffn_w1", [L2, D, D])
    C.ffn_w3 = din("ffn_w3", [L2, D, D])
    C.ffn_w2 = din("ffn_w2", [L2, D, D])
    C.router = din("moe_router", [L2, D, NEXP])
    C.moe_w1 = din("moe_w1", [L2, NEXP, D, DFE])
    C.moe_w3 = din("moe_w3", [L2, NEXP, D, DFE])
    C.moe_w2 = din("moe_w2", [L2, NEXP, DFE, D])
    C.vecs = din("vecs", [L, 128, NV])
    C.ropeH = din("ropeH", [2, 128, NT])
    C.ropeM = din("ropeM", [2, 64, NT])
    C.c_ones = din("c_ones", [128, 128])
    C.c_r128 = din("c_r128", [128, 128])
    C.c_r64 = din("c_r64", [64, 64])
    C.c_ident = din("c_ident", [128, 128])
    C.c_sel = din("c_sel", [8, NEXP, 128])
    C.c_masks = din("c_masks", [128, 2, 384])
    C.outT = nc.dram_tensor("outT", [D, SEQ], F32, kind="ExternalOutput").ap()

    C.hT = dscr("hT", [D, NT], F32)
    C.xnT = dscr("xnT", [D, NT], BF16)
    C.zT = dscr("zT", [IN_W, NT], BF16)
    C.sqT = dscr("sqT", [1536, NT], BF16)
    C.skT = dscr("skT", [512, NT], BF16)
    C.svV = dscr("svV", [NT, 512], BF16)
    C.dvV = dscr("dvV", [NT, 1280], BF16)
    C.cqnT = dscr("cqnT", [768, NT], BF16)
    C.ckvnT = dscr("ckvnT", [512, NT], BF16)
    C.mqraw = dscr("mqraw", [1920, NT], BF16)
    C.mkraw = dscr("mkraw", [1280, NT], BF16)
    C.mvV = dscr("mvV", [NT, 1280], BF16)
    C.mqT = dscr("mqT", [1920, NT], BF16)
    C.mkT = dscr("mkT", [1920, NT], BF16)
    C.dqT = dscr("dqT", [1280, NT], BF16)
    C.dkT = dscr("dkT", [1280, NT], BF16)
    C.dyT = dscr("dyT", [5, 2, 256, NT], F32)
    C.yT = dscr("yT", [D, NT], BF16)
    C.hidT = dscr("hidT", [2 * D, NT], BF16)
    C.lgT = dscr("lgT", [NT, NEXP], F32)
    C.cb = dscr("cb", [NEXP, 128, NT], F32)

    P = Prog(nc)
    with ExitStack() as st:
        def sbp(name, shape, dt):
            return st.enter_context(nc.sbuf_tensor(name, list(shape), dt))

        P.psb = [st.enter_context(nc.psum_tensor("psb%d" % i, [128, 512], F32)) for i in range(8)]
        C.scsb = sbp("scsb", [128, KC, 2], BF16)
        C.modsb = sbp("modsb", [128, 6 * KC, 2], F32)
        C.vsb = sbp("vsb", [128, NV], F32)
        C.ones_bf = sbp("ones_bf", [128, 128], BF16)
        C.ones32 = sbp("ones32", [128, 128], F32)
        C.r128 = sbp("r128", [128, 128], BF16)
        C.r64 = sbp("r64", [64, 64], BF16)
        C.ident = sbp("ident", [128, 128], F32)
        C.sel = sbp("sel", [8, NEXP, 128], F32)
        C.masks = sbp("masks", [128, 2, 384], F32)
        C.lam = sbp("lam", [128, 2], F32)
        C.esink = sbp("esink", [128, 12], F32)
        C.subg = sbp("subg", [128, 2], F32)
        C.epsc = sbp("epsc", [128, 1], F32)

        count = [0]

        def go():
            count[0] += 1
            return stop is None or count[0] <= stop

        ph = Phase(P, "c0")
        ph.op("vector", lambda e: e.memset(C.epsc[:], EPS))
        ph.op("vector", lambda e: e.memset(C.ones32[:], 1.0))
        ph.run()
        init_phase(P, C)

        def zdst(t0, n, j, u):
            w = u["chunks"][j][1]
            return C.zT[u["f0"]:u["f0"] + w, t0:t0 + n]

        for l in range(n_layers):
            if not go(): break
            layer_vec_phase(P, C, l)
            if not go(): break
            mod_phase(P, C, l)
            if not go(): break
            norm_phase(P, C, l, 1)
            if not go(): break
            W = C.w_in[l]
            epiZ = EpiCopy(zdst)
            epiSV = EpiCopy(lambda t0, n, j, u: C.svV[t0:t0 + 128, u["f0"]:u["f0"] + n])
            epiDV = EpiCopy(lambda t0, n, j, u: C.dvV[t0:t0 + 128, u["f0"]:u["f0"] + n])
            blocks = (fblocks(W, 0, C_SV, epiZ) + tblocks(W, C_SV, C_CQ, epiSV) + fblocks(W, C_CQ, C_DQ, epiZ)
                      + fblocks(W, C_DQ, C_DV, epiZ) + tblocks(W, C_DV, IN_W, epiDV))
            linear(P, "inp", D, GROUPS, blocks, xT=C.xnT)
            if not go(): break
            items = []
            for h in range(12):
                items.append(dict(src=[(C.zT[h * 128:(h + 1) * 128, :], 128)], g=[C.vsb[:, V_SQN:V_SQN + 1]], dim=128, rope=["H"],
                                  dst=[C.sqT[h * 128:(h + 1) * 128, :]]))
            for g in range(4):
                r0 = C_SK + g * 128
                items.append(dict(src=[(C.zT[r0:r0 + 128, :], 128)], g=[C.vsb[:, V_SKN:V_SKN + 1]], dim=128, rope=["H"],
                                  dst=[C.skT[g * 128:(g + 1) * 128, :]]))
            for c in range(10):
                m = c % 2
                items.append(dict(src=[(C.zT[C_DQ + c * 128:C_DQ + (c + 1) * 128, :], 128)], g=[C.vsb[:, V_DQN + m:V_DQN + m + 1]],
                                  dim=128, rope=["H"], dst=[C.dqT[c * 128:(c + 1) * 128, :]]))
                items.append(dict(src=[(C.zT[C_DK + c * 128:C_DK + (c + 1) * 128, :], 128)], g=[C.vsb[:, V_DKN + m:V_DKN + m + 1]],
                                  dim=128, rope=["H"], dst=[C.dkT[c * 128:(c + 1) * 128, :]]))
            items.append(dict(src=[(C.zT[C_CQ + j * 128:C_CQ + (j + 1) * 128, :], 128) for j in range(6)],
                              g=[C.vsb[:, V_CQ + j:V_CQ + j + 1] for j in range(6)], dim=768, rope=[None] * 6,
                              dst=[C.cqnT[j * 128:(j + 1) * 128, :] for j in range(6)]))
            items.append(dict(src=[(C.zT[C_CKV + j * 128:C_CKV + (j + 1) * 128, :], 128) for j in range(4)],
                              g=[C.vsb[:, V_CKV + j:V_CKV + j + 1] for j in range(4)], dim=512, rope=[None] * 4,
                              dst=[C.ckvnT[j * 128:(j + 1) * 128, :] for j in range(4)]))
            rmsrope_phase(P, C, items)
            if not go(): break
            epq = EpiCopy(lambda t0, n, j, u: C.mqraw[u["f0"]:u["f0"] + u["chunks"][j][1], t0:t0 + n])
            linear(P, "upq", 768, GROUPS, fblocks(C.w_uq[l], 0, 1920, epq), xT=C.cqnT)
            if not go(): break
            epk = EpiCopy(lambda t0, n, j, u: C.mkraw[u["f0"]:u["f0"] + 128, t0:t0 + n])
            epv = EpiCopy(lambda t0, n, j, u: C.mvV[t0:t0 + 128, u["f0"]:u["f0"] + n])
            blocks = []
            for hp in range(5):
                units = []
                for r in range(2):
                    h = hp * 2 + r
                    units.append(dict(mode="F", chunks=[(r * 256, 128)], epi=epk, f0=h * 128))
                    units.append(dict(mode="T", chunks=[(r * 256 + 128, 128)], epi=epv, f0=h * 128))
                blocks.append(dict(segs=[(C.w_ukv[l], hp * 512, 512)], units=units))
            linear(P, "upkv", 512, GROUPS, blocks, xT=C.ckvnT)
            if not go(): break
            items = []
            for h in range(10):
                items.append(dict(src=[(C.mqraw[h * 192:h * 192 + 128, :], 128), (C.mqraw[h * 192 + 128:(h + 1) * 192, :], 64)],
                                  g=[C.vsb[:, V_MQN:V_MQN + 1], C.vsb[0:64, V_MQN + 1:V_MQN + 2]], dim=192, rope=[None, "M"],
                                  dst=[C.mqT[h * 192:h * 192 + 128, :], C.mqT[h * 192 + 128:(h + 1) * 192, :]]))
                items.append(dict(src=[(C.mkraw[h * 128:(h + 1) * 128, :], 128), (C.zT[C_KR:C_KR + 64, :], 64)],
                                  g=[C.vsb[:, V_MKN:V_MKN + 1], C.vsb[0:64, V_MKN + 1:V_MKN + 2]], dim=192, rope=[None, "M"],
                                  dst=[C.mkT[h * 192:h * 192 + 128, :], C.mkT[h * 192 + 128:(h + 1) * 192, :]]))
            rmsrope_phase(P, C, items)
            if not go(): break
            vheads = []
            for h in range(10):
                vheads.append(dict(q=[(C.mqT[h * 192:h * 192 + 128, :], 128), (C.mqT[h * 192 + 128:(h + 1) * 192, :], 64)],
                                   k=[(C.mkT[h * 192:h * 192 + 128, :], 128), (C.mkT[h * 192 + 128:(h + 1) * 192, :], 64)],
                                   V=C.mvV[:, h * 128:(h + 1) * 128], ew=128, scale=192 ** -0.5, dt=BF16,
                                   dst=(lambda ec, h=h: C.yT[1536 + h * 128:1536 + (h + 1) * 128, :])))
            for h in range(5):
                for m in range(2):
                    c = h * 2 + m
                    vheads.append(dict(q=[(C.dqT[c * 128:(c + 1) * 128, :], 128)], k=[(C.dkT[c * 128:(c + 1) * 128, :], 128)],
                                       V=C.dvV[:, h * 256:(h + 1) * 256], ew=256, scale=128 ** -0.5, dt=F32,
                                       dst=(lambda ec, h=h, m=m: C.dyT[h, m, ec * 128:(ec + 1) * 128, :])))
            attn_phase(P, C, vheads)
            if not go(): break
            swa_phase(P, C)
            if not go(): break
            dif_merge_phase(P, C)
            if not go(): break
            eo = EpiResid(C.hT, lambda fc, isc: C.modsb[:, 2 * KC + fc, (1 if isc else 0):(2 if isc else 1)])
            linear(P, "outp", D, GROUPS, fblocks(C.w_out[l], 0, D, eo), xT=C.yT)
            if not go(): break
            norm_phase(P, C, l, 2)
            if not go(): break
            e2 = EpiResid(C.hT, lambda fc, isc: C.modsb[:, 5 * KC + fc, (1 if isc else 0):(2 if isc else 1)])
            i = l // 2
            if l % 2 == 0:
                es = EpiSwiGLU(lambda t0, n, j, u: C.hidT[u["f0"]:u["f0"] + 128, t0:t0 + n])
                blocks = []
                for f in range(0, D, 256):
                    units = [dict(mode="F", chunks=[(o, 128), (256 + o, 128)], epi=es, f0=f + o) for o in (0, 128)]
                    blocks.append(dict(segs=[(C.ffn_w1[i], f, 256), (C.ffn_w3[i], f, 256)], units=units))
                linear(P, "ffu", D, GROUPS, blocks, xT=C.xnT)
                if not go(): break
                linear(P, "ffd", D, GROUPS, fblocks(C.ffn_w2[i], 0, D, e2), xT=C.hidT[0:D, :])
            else:
                er = EpiCopy(lambda t0, n, j, u: C.lgT[t0:t0 + 128, 0:n], dt=F32)
                linear(P, "rtr", D, GROUPS, tblocks(C.router[i], 0, NEXP, er), xT=C.xnT)
                if not go(): break
                moe_gate_phase(P, C)
                if not go(): break
                es = EpiSwiGLU(lambda t0, n, j, u: C.hidT[u["f0"]:u["f0"] + 128, t0:t0 + n],
                               cb=lambda t0, n, u: C.cb[u["ex"], :, t0:t0 + n])
                blocks = []
                for ex in range(NEXP):
                    for f in range(0, DFE, 256):
                        units = [dict(mode="F", chunks=[(o, 128), (256 + o, 128)], epi=es, f0=ex * DFE + f + o, ex=ex) for o in (0, 128)]
                        blocks.append(dict(segs=[(C.moe_w1[i, ex], f, 256), (C.moe_w3[i, ex], f, 256)], units=units))
                linear(P, "mou", D, GROUPS, blocks, xT=C.xnT)
                if not go(): break
                W2 = C.moe_w2[i].rearrange("e k d -> (e k) d")
                linear(P, "mod1", D, GROUPS, fblocks(W2[0:D, :], 0, D, e2), xT=C.hidT[0:D, :])
                e3 = EpiResid(C.hT, lambda fc, isc: C.modsb[:, 5 * KC + fc, (1 if isc else 0):(2 if isc else 1)])
                linear(P, "mod2", D, GROUPS, fblocks(W2[D:2 * D, :], 0, D, e3), xT=C.hidT[D:2 * D, :])
        final_phase(P, C)
    return nc


def _rope_tables(dim):
    rows = SEQ // GRID_W
    t = np.arange(SEQ)
    t_row = (t // GRID_W).astype(np.float32)
    t_col = (t % GRID_W).astype(np.float32)
    quarter = dim // 4
    inv = (np.float32(10000.0) ** (-np.arange(quarter, dtype=np.float32) / np.float32(quarter))).astype(np.float32)
    ang = np.concatenate([t_row[:, None] * inv, t_col[:, None] * inv], axis=-1).astype(np.float32)
    cos = np.cos(ang).astype(np.float32)
    sin = np.sin(ang).astype(np.float32)
    tab = np.zeros((2, dim, NT), np.float32)
    tab[0, :, SEQ:] = 1.0
    half = dim // 2
    tab[0, :half, :SEQ] = cos.T
    tab[0, half:, :SEQ] = cos.T
    tab[1, :half, :SEQ] = sin.T
    tab[1, half:, :SEQ] = sin.T
    return tab


def _rot_lhsT(dim):
    half = dim // 2
    m = np.zeros((dim, dim), np.float32)
    for i in range(half):
        m[i + half, i] = -1.0
        m[i, i + half] = 1.0
    return m


def _host_inputs(inp, L=DEPTH, ncores=NCORES):
    f = lambda a: np.ascontiguousarray(np.asarray(a, dtype=np.float32))
    vecs = np.zeros((L, 128, NV), np.float32)
    for l in range(L):
        vecs[l, :, V_N1:V_N1 + KC] = f(inp["norm1_g"])[l].reshape(KC, 128).T
        vecs[l, :, V_N2:V_N2 + KC] = f(inp["norm2_g"])[l].reshape(KC, 128).T
        vecs[l, :, V_ADAB:V_ADAB + 6 * KC] = f(inp["ada_b"])[l].reshape(6 * KC, 128).T
        vecs[l, :, V_CQ:V_CQ + 6] = f(inp["mla_cq_norm"])[l].reshape(6, 128).T
        vecs[l, :, V_CKV:V_CKV + 4] = f(inp["mla_ckv_norm"])[l].reshape(4, 128).T
        vecs[l, :, V_SQN] = f(inp["swa_q_norm"])[l]
        vecs[l, :, V_SKN] = f(inp["swa_k_norm"])[l]
        vecs[l, :, V_MQN] = f(inp["mla_q_norm"])[l][:128]
        vecs[l, :64, V_MQN + 1] = f(inp["mla_q_norm"])[l][128:]
        vecs[l, :, V_MKN] = f(inp["mla_k_norm"])[l][:128]
        vecs[l, :64, V_MKN + 1] = f(inp["mla_k_norm"])[l][128:]
        vecs[l, :, V_DQN:V_DQN + 2] = f(inp["dif_q_norm"])[l].T
        vecs[l, :, V_DKN:V_DKN + 2] = f(inp["dif_k_norm"])[l].T
        vecs[l, :, V_SUB:V_SUB + 2] = f(inp["dif_subln"])[l].reshape(2, 128).T
        vecs[l, :, V_LAM:V_LAM + 4] = f(inp["dif_lambda"])[l].T
        vecs[l, :, V_SINK:V_SINK + 12] = f(inp["swa_sink"])[l][None, :]
    masks = np.zeros((128, 2, 384), np.float32)
    k = np.arange(128)[:, None]
    q = np.arange(128)[None, :]
    masks[:, 0, :] = np.tile((q <= k).astype(np.float32), (1, 3))
    masks[:, 1, :] = np.tile((k <= q).astype(np.float32), (1, 3))
    sel = np.zeros((8, NEXP, 128), np.float32)
    for e in range(NEXP):
        sel[e, e, :] = 1.0
    shared = dict(
        ada_w=f(inp["ada_w"]), w_in=f(inp["w_in"]), w_out=f(inp["w_out"]), mla_w_uq=f(inp["mla_w_uq"]),
        mla_w_ukv=f(inp["mla_w_ukv"]), ffn_w1=f(inp["ffn_w1"]), ffn_w3=f(inp["ffn_w3"]), ffn_w2=f(inp["ffn_w2"]),
        moe_router=f(inp["moe_router"]), moe_w1=f(inp["moe_w1"]), moe_w3=f(inp["moe_w3"]), moe_w2=f(inp["moe_w2"]),
        vecs=vecs, ropeH=_rope_tables(128), ropeM=_rope_tables(64), c_ones=np.ones((128, 128), np.float32),
        c_r128=_rot_lhsT(128), c_r64=_rot_lhsT(64), c_ident=np.eye(128, dtype=np.float32), c_sel=sel, c_masks=masks)
    x = f(inp["x"])
    ctx = f(inp["ctx"])
    c = f(inp["c"])
    cc = f(inp["c_ctx"])
    maps = []
    for b in range(ncores):
        xT = np.ascontiguousarray(np.concatenate([x[b].T, ctx[b].T], axis=1))
        cvec = np.ascontiguousarray(np.stack([c[b].reshape(KC, 128).T, cc.reshape(KC, 128).T], axis=-1))
        m = dict(shared)
        m["xT"] = xT
        m["cvec"] = cvec
        maps.append(m)
    return maps


def kernel(**inputs):
    maps = _host_inputs(inputs)
    nc = build()
    res = run_bass_kernel_spmd(nc, maps, core_ids=list(range(NCORES)))
    out = np.stack([np.ascontiguousarray(res.results[b]["outT"].T) for b in range(NCORES)], axis=0)
    return out.astype(np.float32)
```

```python
import math
from contextlib import ExitStack

import numpy as np
import concourse.bass as bass
import concourse.mybir as mybir
from concourse.bass_utils import run_bass_kernel_spmd

F32 = mybir.dt.float32
BF16 = mybir.dt.bfloat16
AF = mybir.ActivationFunctionType
ALU = mybir.AluOpType
AX = mybir.AxisListType

D = 4096
KC = D // 128
SEQ = 4096
CTX = 256
NT = SEQ + CTX
DEPTH = 4
NCORES = 4
EPS = 1e-6
GRID_W = 64
IN_W = 7744
NEXP = 8
DFE = 1024

C_SQ, C_SK, C_SV, C_CQ, C_CKV, C_KR, C_DQ, C_DK, C_DV = 0, 1536, 2048, 2560, 3328, 3840, 3904, 5184, 6464

V_N1, V_N2, V_ADAB, V_CQ, V_CKV, V_SQN, V_SKN, V_MQN, V_MKN, V_DQN, V_DKN, V_SUB, V_LAM, V_SINK = (
    0, 32, 64, 256, 262, 266, 267, 268, 270, 272, 274, 276, 278, 282)
NV = 294

GROUPS = [(0, 1024), (1024, 1024), (2048, 1024), (3072, 1280)]
SUBT = [(i * 512, 512) for i in range(8)] + [(4096, 256)]

ENGS = ("sync", "scalar", "vector", "gpsimd", "tensor")
ATT_LOOK, ATT_DEFER, ATT_POOL = 2, 2, 1
NDS = 64


class Tok:
    __slots__ = ("sem", "val", "dma")

    def __init__(self, sem, val, dma=False):
        self.sem = sem
        self.val = val
        self.dma = dma


class Prog:
    def __init__(self, nc):
        self.nc = nc
        self.esem = {}
        self.ecnt = {}
        for e in ("scalar", "vector", "gpsimd", "tensor"):
            self.esem[e] = nc.alloc_semaphore(name="sem_" + e)
            self.ecnt[e] = 0
        self.dsem = [nc.alloc_semaphore(name="dsem%d" % i) for i in range(NDS)]
        self.dcnt = [0] * NDS
        self.dfree = list(range(NDS))
        self.nph = 0


class Phase:
    def __init__(self, P, name):
        self.P = P
        self.nc = P.nc
        P.nph += 1
        self.name = "%s%d" % (name, P.nph)
        self.tasks = {e: [] for e in ENGS}
        self.stack = ExitStack()
        self.my_ds = []
        self.nt = 0

    def sb(self, name, shape, dt):
        self.nt += 1
        return self.stack.enter_context(self.nc.sbuf_tensor("%s_%s%d" % (self.name, name, self.nt), list(shape), dt))

    def dsem(self):
        i = self.P.dfree.pop()
        self.my_ds.append(i)
        return i

    def op(self, eng, fn, deps=(), signal=True):
        P = self.P
        tok = None
        if signal:
            P.ecnt[eng] += 1
            tok = Tok(P.esem[eng], P.ecnt[eng])
        self.tasks[eng].append((fn, [d for d in deps if d is not None], tok))
        return tok

    def dma(self, out, in_, si, deps=(), q="sync"):
        P = self.P
        P.dcnt[si] += 16
        tok = Tok(P.dsem[si], P.dcnt[si], True)
        self.tasks[q].append((lambda e: e.dma_start(out=out, in_=in_), [d for d in deps if d is not None], tok))
        return tok

    def run(self):
        P = self.P
        finals = [Tok(P.dsem[i], P.dcnt[i], True) for i in self.my_ds]
        self.tasks["sync"].append((None, finals, None))
        with self.nc.Block() as blk:
            for eng in ENGS:
                tasks = self.tasks[eng]
                if not tasks:
                    continue

                def body(e, tasks=tasks):
                    waited = {}
                    for fn, deps, tok in tasks:
                        for d in deps:
                            k = id(d.sem)
                            if waited.get(k, -1) >= d.val:
                                continue
                            e.wait_ge(d.sem, d.val)
                            waited[k] = d.val
                        if fn is None:
                            continue
                        ins = fn(e)
                        if tok is not None:
                            ins.then_inc(tok.sem, 16 if tok.dma else 1)

                getattr(blk, eng)(body)
        self.stack.close()
        P.dfree.extend(self.my_ds)


class Ring:
    def __init__(self, ph, name, n, shape=None, dt=None, dma=False, bufs=None):
        self.n = n
        self.bufs = bufs if bufs is not None else [ph.sb("%s%d" % (name, i), shape, dt) for i in range(n)]
        self.free = [[] for _ in range(n)]
        self.ds = [ph.dsem() for _ in range(n)] if dma else None
        self.i = 0

    def next(self):
        s = self.i % self.n
        self.i += 1
        deps = self.free[s]
        self.free[s] = []
        return s, self.bufs[s], deps

    def release(self, s, tok):
        if tok is not None:
            self.free[s].append(tok)


def nsplits(t0, n):
    out = []
    while n > 0:
        m = min(512, n)
        out.append((t0, m))
        t0 += m
        n -= m
    return out


class EpiCopy:
    def __init__(self, dst, dt=BF16):
        self.dst = dst
        self.dt = dt

    def setup(self, ph, psr):
        self.psr = psr
        self.st = Ring(ph, "epst", 4, [128, 512], self.dt, dma=True)
        self.k = 0

    def __call__(self, ph, t0, n, tiles, petok, uinfo):
        for j, (s, ps, w) in enumerate(tiles):
            ss, stg, deps = self.st.next()
            eng = "scalar" if self.k % 2 == 0 else "vector"
            self.k += 1
            if eng == "scalar":
                tk = ph.op(eng, lambda e, stg=stg, ps=ps, w=w, n=n: e.activation(out=stg[:w, :n], in_=ps[:w, :n], func=AF.Copy),
                           deps=[petok] + deps)
            else:
                tk = ph.op(eng, lambda e, stg=stg, ps=ps, w=w, n=n: e.tensor_copy(out=stg[:w, :n], in_=ps[:w, :n]),
                           deps=[petok] + deps)
            self.psr.release(s, tk)
            dtok = ph.dma(self.dst(t0, n, j, uinfo), stg[:w, :n], self.st.ds[ss], deps=[tk])
            self.st.release(ss, dtok)


class EpiSwiGLU:
    def __init__(self, dst, cb=None):
        self.dst = dst
        self.cb = cb

    def setup(self, ph, psr):
        self.psr = psr
        self.sg = Ring(ph, "sg", 3, [128, 512], F32)
        self.st = Ring(ph, "epst", 3, [128, 512], BF16, dma=True)
        if self.cb is not None:
            self.cbr = Ring(ph, "cbr", 3, [128, 512], F32, dma=True)
            self.t2 = Ring(ph, "t2", 2, [128, 512], F32)

    def __call__(self, ph, t0, n, tiles, petok, uinfo):
        (sa, pa, w), (sb_, pb, _) = tiles
        s1, sg, d1 = self.sg.next()
        t1 = ph.op("scalar", lambda e: e.activation(out=sg[:w, :n], in_=pa[:w, :n], func=AF.Silu), deps=[petok] + d1)
        self.psr.release(sa, t1)
        ss, stg, d2 = self.st.next()
        if self.cb is None:
            t2 = ph.op("vector", lambda e: e.tensor_tensor(out=stg[:w, :n], in0=pb[:w, :n], in1=sg[:w, :n], op=ALU.mult),
                       deps=[petok, t1] + d2)
            self.psr.release(sb_, t2)
            self.sg.release(s1, t2)
        else:
            sc, cbt, d3 = self.cbr.next()
            tc = ph.dma(cbt[:w, :n], self.cb(t0, n, uinfo), self.cbr.ds[sc], deps=d3)
            sx, tx, d4 = self.t2.next()
            ta = ph.op("vector", lambda e: e.tensor_tensor(out=tx[:w, :n], in0=pb[:w, :n], in1=sg[:w, :n], op=ALU.mult),
                       deps=[petok, t1] + d4)
            self.psr.release(sb_, ta)
            self.sg.release(s1, ta)
            t2 = ph.op("gpsimd", lambda e: e.tensor_tensor(out=stg[:w, :n], in0=tx[:w, :n], in1=cbt[:w, :n], op=ALU.mult),
                       deps=[ta, tc] + d2)
            self.t2.release(sx, t2)
            self.cbr.release(sc, t2)
        dtok = ph.dma(self.dst(t0, n, 0, uinfo), stg[:w, :n], self.st.ds[ss], deps=[t2])
        self.st.release(ss, dtok)


class EpiResid:
    def __init__(self, hT, gcol):
        self.hT = hT
        self.gcol = gcol

    def setup(self, ph, psr):
        self.psr = psr
        self.hin = Ring(ph, "hin", 3, [128, 512], F32, dma=True)
        self.hout = Ring(ph, "hout", 3, [128, 512], F32, dma=True)

    def __call__(self, ph, t0, n, tiles, petok, uinfo):
        (s, ps, w), = tiles
        f0 = uinfo["f0"]
        si, hin, d1 = self.hin.next()
        tl = ph.dma(hin[:w, :n], self.hT[f0:f0 + w, t0:t0 + n], self.hin.ds[si], deps=d1)
        so, hout, d2 = self.hout.next()
        g = self.gcol(f0 // 128, t0 >= SEQ)
        tk = ph.op("vector", lambda e: e.scalar_tensor_tensor(out=hout[:w, :n], in0=ps[:w, :n], scalar=g, in1=hin[:w, :n],
                                                                 op0=ALU.mult, op1=ALU.add), deps=[petok, tl] + d2)
        self.psr.release(s, tk)
        self.hin.release(si, tk)
        dtok = ph.dma(self.hT[f0:f0 + w, t0:t0 + n], hout[:w, :n], self.hout.ds[so], deps=[tk])
        self.hout.release(so, dtok)


class EpiMod:
    def __init__(self, modsb, bcol):
        self.modsb = modsb
        self.bcol = bcol

    def setup(self, ph, psr):
        self.psr = psr

    def __call__(self, ph, t0, n, tiles, petok, uinfo):
        (s, ps, w), = tiles
        fc = uinfo["f0"] // 128
        tk = ph.op("vector", lambda e: e.tensor_scalar(out=self.modsb[:, fc, 0:2], in0=ps[:, 0:2], scalar1=self.bcol(fc),
                                                        scalar2=None, op0=ALU.add), deps=[petok])
        self.psr.release(s, tk)


def linear(P, name, K, groups, blocks, xT=None, x_sb=None, cast_engs=("gpsimd", "vector", "gpsimd", "scalar"), stgcfg=(3, 1024)):
    ph = Phase(P, name)
    KCn = K // 128
    gmax = max(g[1] for g in groups)
    psr = Ring(ph, "ps", 8, bufs=P.psb)
    epis = []
    for b in blocks:
        for u in b["units"]:
            if u["epi"] not in epis:
                epis.append(u["epi"])
    for ep in epis:
        ep.setup(ph, psr)
    if x_sb is None:
        X = ph.sb("X", [128, KCn, gmax], BF16)
        xds = ph.dsem()
    else:
        X = x_sb
    Wr = Ring(ph, "Wb", 2, [128, KCn, 512], BF16)
    Sr = Ring(ph, "Ws", stgcfg[0], [128, stgcfg[1]], F32, dma=True)

    steps = [(g, b) for g in range(len(groups)) for b in range(len(blocks))]
    xfree = []
    xtok = {}
    loaded = {}
    ncast = [0]

    def load_w(step):
        g, bi = steps[step]
        blk = blocks[bi]
        ws, Wb, wdeps = Wr.next()
        off = 0
        lasts = {}
        for (W, c0, w) in blk["segs"]:
            Wv = W.rearrange("(kc p) f -> p kc f", p=128)
            kcs = max(1, stgcfg[1] // w)
            k0 = 0
            while k0 < KCn:
                kn = min(kcs, KCn - k0)
                ss, stg, sdeps = Sr.next()
                sv = stg[:, 0:kn * w].rearrange("p (k w) -> p k w", w=w)
                td = ph.dma(sv, Wv[:, k0:k0 + kn, c0:c0 + w], Sr.ds[ss], deps=sdeps)
                ceng = cast_engs[ncast[0] % len(cast_engs)]
                ncast[0] += 1
                if ceng == "scalar":
                    tc = ph.op(ceng, lambda e, Wb=Wb, k0=k0, kn=kn, off=off, w=w, sv=sv: e.activation(
                        out=Wb[:, k0:k0 + kn, off:off + w], in_=sv, func=AF.Copy), deps=[td] + wdeps)
                else:
                    tc = ph.op(ceng, lambda e, Wb=Wb, k0=k0, kn=kn, off=off, w=w, sv=sv: e.tensor_copy(
                        out=Wb[:, k0:k0 + kn, off:off + w], in_=sv), deps=[td] + wdeps)
                Sr.release(ss, tc)
                lasts[ceng] = tc
                k0 += kn
            off += w
        loaded[step] = (ws, Wb, list(lasts.values()))

    load_w(0)
    for step, (g, bi) in enumerate(steps):
        g0, gn = groups[g]
        if bi == 0 and x_sb is None:
            xt = None
            for q in range(0, KCn, 8):
                qn = min(8, KCn - q)
                xt = ph.dma(X[:, q:q + qn, 0:gn], xT.rearrange("(kc p) t -> p kc t", p=128)[:, q:q + qn, g0:g0 + gn], xds,
                            deps=xfree if q == 0 else [])
            xfree = []
            xtok[g] = xt
        if step + 1 < len(steps):
            load_w(step + 1)
        ws, Wb, wtoks = loaded.pop(step)
        first_deps = wtoks + ([xtok[g]] if x_sb is None else [])
        lastpe = None
        for u in blocks[bi]["units"]:
            if u["mode"] == "F":
                for (t0, n) in nsplits(g0, gn):
                    tiles = []
                    pet = None
                    for (off, w) in u["chunks"]:
                        s, ps, pdeps = psr.next()
                        for kc in range(KCn):
                            lastk = kc == KCn - 1
                            pet = ph.op("tensor", lambda e, ps=ps, Wb=Wb, kc=kc, off=off, w=w, t0=t0, n=n, g0=g0, lastk=lastk:
                                        e.matmul(ps[:w, :n], lhsT=Wb[:, kc, off:off + w], rhs=X[:, kc, t0 - g0:t0 - g0 + n],
                                                 start=(kc == 0), stop=lastk),
                                        deps=(pdeps + first_deps) if kc == 0 else (), signal=lastk)
                        tiles.append((s, ps, w))
                    lastpe = pet
                    u["epi"](ph, t0, n, tiles, pet, u)
            else:
                (off, w), = u["chunks"]
                for tt in range(g0, g0 + gn, 128):
                    s, ps, pdeps = psr.next()
                    pet = None
                    for kc in range(KCn):
                        lastk = kc == KCn - 1
                        pet = ph.op("tensor", lambda e, ps=ps, Wb=Wb, kc=kc, off=off, w=w, tt=tt, g0=g0, lastk=lastk:
                                    e.matmul(ps[:, :w], lhsT=X[:, kc, tt - g0:tt - g0 + 128], rhs=Wb[:, kc, off:off + w],
                                             start=(kc == 0), stop=lastk),
                                    deps=(pdeps + first_deps) if kc == 0 else (), signal=lastk)
                    lastpe = pet
                    u["epi"](ph, tt, w, [(s, ps, 128)], pet, u)
        Wr.release(ws, lastpe)
        if bi == len(blocks) - 1:
            xfree = [lastpe]
    ph.run()


def fblocks(W, c0, c1, epi, base_f0=None, extra=None):
    out = []
    c = c0
    while c < c1:
        bw = min(512, c1 - c)
        units = []
        o = 0
        while o < bw:
            w = min(128, bw - o)
            u = dict(mode="F", chunks=[(o, w)], epi=epi, f0=(c + o) if base_f0 is None else base_f0 + (c + o - c0))
            if extra:
                u.update(extra)
            units.append(u)
            o += w
        out.append(dict(segs=[(W, c, bw)], units=units))
        c += bw
    return out


def tblocks(W, c0, c1, epi, dcol0=0):
    out = []
    c = c0
    while c < c1:
        bw = min(512, c1 - c)
        out.append(dict(segs=[(W, c, bw)], units=[dict(mode="T", chunks=[(0, bw)], epi=epi, f0=dcol0 + c - c0)]))
        c += bw
    return out


class Ctx:
    pass


def init_phase(P, C):
    ph = Phase(P, "init")
    ds = ph.dsem()
    for q in range(0, D, 512):
        ph.dma(C.hT[q:q + 512, :], C.xT[q:q + 512, :], ds)
    cv = ph.sb("cv", [128, KC, 2], F32)
    d2 = ph.dsem()
    t = ph.dma(cv[:], C.cvec, d2)
    ph.op("scalar", lambda e: e.activation(out=C.scsb[:], in_=cv[:], func=AF.Silu), deps=[t])
    d3 = ph.dsem()
    sts = []
    t = None
    for (dst, src) in ((C.ones_bf, C.c_ones), (C.r128, C.c_r128), (C.r64, C.c_r64)):
        st = ph.sb("cst", list(src.shape), F32)
        t = ph.dma(st[:], src, d3)
        sts.append((dst, st))
    for (dst, st) in sts:
        ph.op("vector", lambda e, dst=dst, st=st: e.tensor_copy(out=dst[:], in_=st[:]), deps=[t])
    d4 = ph.dsem()
    ph.dma(C.ident[:], C.c_ident, d4)
    ph.dma(C.sel[:], C.c_sel, d4)
    ph.dma(C.masks[:], C.c_masks, d4)
    ph.run()


def layer_vec_phase(P, C, l):
    ph = Phase(P, "vec")
    ds = ph.dsem()
    t = ph.dma(C.vsb[:], C.vecs[l], ds)
    lam_init = 0.8 - 0.6 * math.exp(-0.3 * l)
    pr = ph.sb("pr", [128, 2], F32)
    t1 = ph.op("vector", lambda e: e.tensor_tensor(out=pr[:, 0:1], in0=C.vsb[:, V_LAM:V_LAM + 1], in1=C.vsb[:, V_LAM + 1:V_LAM + 2],
                                                    op=ALU.mult), deps=[t])
    t2 = ph.op("vector", lambda e: e.tensor_tensor(out=pr[:, 1:2], in0=C.vsb[:, V_LAM + 2:V_LAM + 3], in1=C.vsb[:, V_LAM + 3:V_LAM + 4],
                                                    op=ALU.mult), deps=[t, t1])
    ps = P.psb[0]
    t3 = ph.op("tensor", lambda e: e.matmul(ps[:, 0:2], lhsT=C.ones32[:], rhs=pr[:, 0:2], start=True, stop=True), deps=[t2])
    ex = ph.sb("ex", [128, 2], F32)
    t4 = ph.op("scalar", lambda e: e.activation(out=ex[:], in_=ps[:, 0:2], func=AF.Exp), deps=[t3])
    t5 = ph.op("vector", lambda e: e.tensor_tensor(out=C.lam[:, 0:1], in0=ex[:, 0:1], in1=ex[:, 1:2], op=ALU.subtract), deps=[t4])
    t6 = ph.op("vector", lambda e: e.tensor_scalar(out=C.lam[:, 0:1], in0=C.lam[:, 0:1], scalar1=lam_init, scalar2=None, op0=ALU.add),
               deps=[t5])
    t7 = ph.op("vector", lambda e: e.tensor_scalar(out=C.lam[:, 1:2], in0=C.lam[:, 0:1], scalar1=-1.0, scalar2=None, op0=ALU.mult),
               deps=[t6])
    ph.op("scalar", lambda e: e.activation(out=C.esink[:], in_=C.vsb[:, V_SINK:V_SINK + 12], func=AF.Exp), deps=[t, t4])
    ph.op("vector", lambda e: e.tensor_scalar(out=C.subg[:], in0=C.vsb[:, V_SUB:V_SUB + 2], scalar1=1.0 - lam_init, scalar2=None,
                                              op0=ALU.mult), deps=[t, t7])
    ph.run()


def mod_phase(P, C, l):
    epi = EpiMod(C.modsb, lambda fc: C.vsb[:, V_ADAB + fc:V_ADAB + fc + 1])
    blocks = fblocks(C.ada_w[l], 0, 6 * D, epi)
    linear(P, "mod", D, [(0, 2)], blocks, x_sb=C.scsb, cast_engs=("gpsimd", "vector", "scalar"), stgcfg=(8, 2048))


def norm_phase(P, C, l, which):
    ph = Phase(P, "norm")
    gcol = V_N1 if which == 1 else V_N2
    sh_i, sc_i = (0, 1) if which == 1 else (3, 4)
    AB = ph.sb("AB", [128, 4, KC], F32)
    abt = None
    for ci in (0, 1):
        t = ph.op("vector", lambda e, ci=ci: e.tensor_scalar(out=AB[:, 2 * ci, :], in0=C.modsb[:, sc_i * KC:(sc_i + 1) * KC, ci],
                                                               scalar1=1.0, scalar2=None, op0=ALU.add), deps=[abt])
        t = ph.op("vector", lambda e, ci=ci: e.tensor_tensor(out=AB[:, 2 * ci, :], in0=AB[:, 2 * ci, :], in1=C.vsb[:, gcol:gcol + KC],
                                                               op=ALU.mult), deps=[t])
        abt = ph.op("vector", lambda e, ci=ci: e.tensor_copy(out=AB[:, 2 * ci + 1, :], in_=C.modsb[:, sh_i * KC:(sh_i + 1) * KC, ci]),
                    deps=[t])
    hr = Ring(ph, "hb", 2, [128, KC, 512], F32)
    hsem = [[ph.dsem() for _ in range(4)] for _ in range(2)]
    sqr = Ring(ph, "sq", 3, [128, 8, 512], BF16)
    rsr = Ring(ph, "rs", 3, [128, 512], F32)
    tmr = Ring(ph, "tm", 4, [128, 512], F32)
    osr = Ring(ph, "os", 3, [128, 8, 512], BF16, dma=True)
    psr = Ring(ph, "ps", 8, bufs=P.psb)
    hv = C.hT.rearrange("(kc p) t -> p kc t", p=128)
    xv = C.xnT.rearrange("(kc p) t -> p kc t", p=128)

    def stage_a(t0, n):
        hs, hb, hdeps = hr.next()
        s, ps, pdeps = psr.next()
        ltoks = []
        pet = None
        for q in range(0, KC, 8):
            lt = ph.dma(hb[:, q:q + 8, :n], hv[:, q:q + 8, t0:t0 + n], hsem[hs][q // 8], deps=hdeps)
            ltoks.append(lt)
        for q in range(0, KC, 8):
            ss, sq, sdeps = sqr.next()
            ta = ph.op("scalar", lambda e, sq=sq, hb=hb, q=q, n=n: e.activation(out=sq[:, :, :n], in_=hb[:, q:q + 8, :n], func=AF.Square),
                       deps=[ltoks[q // 8]] + sdeps)
            for j in range(8):
                lastk = (q + j == KC - 1)
                pet = ph.op("tensor", lambda e, ps=ps, sq=sq, j=j, n=n, q=q, lastk=lastk: e.matmul(
                    ps[:, :n], lhsT=C.ones_bf[:], rhs=sq[:, j, :n], start=(q + j == 0), stop=lastk),
                    deps=([ta] + (pdeps if q == 0 else [])) if j == 0 else (), signal=(j == 7))
            sqr.release(ss, pet)
        rs_, rs, rdeps = rsr.next()
        t1 = ph.op("scalar", lambda e, rs=rs, ps=ps, n=n: e.activation(out=rs[:, :n], in_=ps[:, :n], func=AF.Sqrt, bias=C.epsc[:, 0:1],
                                                                      scale=1.0 / D), deps=[pet] + rdeps)
        psr.release(s, t1)
        t2 = ph.op("vector", lambda e, rs=rs, n=n: e.reciprocal(out=rs[:, :n], in_=rs[:, :n]), deps=[t1])
        return (t0, n, hs, hb, ltoks, rs_, rs, t2)

    def stage_b(info):
        t0, n, hs, hb, ltoks, rs_, rs, t2 = info
        ci = 1 if t0 >= SEQ else 0
        lastact = None
        for q in range(0, KC, 8):
            os_, ost, odeps = osr.next()
            for j in range(8):
                kc = q + j
                ts_, tm, tdeps = tmr.next()
                tv = ph.op("vector", lambda e, tm=tm, hb=hb, kc=kc, rs=rs, n=n: e.tensor_tensor(out=tm[:, :n], in0=hb[:, kc, :n],
                                                                                              in1=rs[:, :n], op=ALU.mult),
                           deps=[t2, ltoks[q // 8]] + tdeps)
                lastact = ph.op("scalar", lambda e, ost=ost, j=j, tm=tm, kc=kc, n=n, ci=ci: e.activation(
                    out=ost[:, j, :n], in_=tm[:, :n], func=AF.Identity, bias=AB[:, 2 * ci + 1, kc:kc + 1],
                    scale=AB[:, 2 * ci, kc:kc + 1]), deps=[tv, abt] + (odeps if j == 0 else []))
                tmr.release(ts_, lastact)
            dt_ = ph.dma(xv[:, q:q + 8, t0:t0 + n], ost[:, :, :n], osr.ds[os_], deps=[lastact])
            osr.release(os_, dt_)
        hr.release(hs, lastact)
        rsr.release(rs_, lastact)

    infos = [stage_a(*SUBT[0])]
    for i in range(len(SUBT)):
        if i + 1 < len(SUBT):
            infos.append(stage_a(*SUBT[i + 1]))
        stage_b(infos[i])
    ph.run()


def rmsrope_phase(P, C, items):
    ph = Phase(P, "rr")
    tabH = Ring(ph, "tabH", 2, [128, 2, 512], F32, dma=True)
    tabM = Ring(ph, "tabM", 2, [64, 2, 512], F32, dma=True)
    zr = Ring(ph, "z", 32, [128, 512], BF16, dma=True)
    sqr = Ring(ph, "sq", 8, [128, 512], BF16)
    rsr = Ring(ph, "rs", 6, [128, 512], F32)
    znr = Ring(ph, "zn", 6, [128, 512], BF16)
    t1r = Ring(ph, "t1", 6, [128, 512], F32)
    t2r = Ring(ph, "t2", 6, [128, 512], F32)
    osr = Ring(ph, "os", 16, [128, 512], BF16, dma=True)
    psr = Ring(ph, "ps", 8, bufs=P.psb)
    work = [(si, it) for si in range(len(SUBT)) for it in items]
    tabs = {}
    st = {}

    st0 = {}

    def stage_a0(u):
        si, it = work[u]
        t0, n = SUBT[si]
        ent = []
        for j, (src, w) in enumerate(it["src"]):
            zs_, z, zd = zr.next()
            lt = ph.dma(z[:w, :n], src[:, t0:t0 + n], zr.ds[zs_], deps=zd, q="scalar")
            ent.append((zs_, z, lt, w))
        st0[u] = ent

    def stage_a(u):
        si, it = work[u]
        t0, n = SUBT[si]
        if si not in tabs:
            hs, tH, hd = tabH.next()
            tokH = ph.dma(tH[:, :, :n], C.ropeH.rearrange("c p t -> p c t")[:, :, t0:t0 + n], tabH.ds[hs], deps=hd)
            ms, tM, md = tabM.next()
            tokM = ph.dma(tM[:, :, :n], C.ropeM.rearrange("c p t -> p c t")[:, :, t0:t0 + n], tabM.ds[ms], deps=md)
            tabs[si] = (hs, tH, tokH, ms, tM, tokM)
        nch = len(it["src"])
        s, ps, pdeps = psr.next()
        zs = []
        pet = None
        for j, (src, w) in enumerate(it["src"]):
            zs_, z, lt, _w = st0[u][j]
            ss, sq, sd = sqr.next()
            ta = ph.op("scalar", lambda e, sq=sq, z=z, w=w, n=n: e.activation(out=sq[:w, :n], in_=z[:w, :n], func=AF.Square),
                       deps=[lt] + sd)
            pet = ph.op("tensor", lambda e, ps=ps, sq=sq, w=w, n=n, j=j, nch=nch: e.matmul(
                ps[:, :n], lhsT=C.ones_bf[:w, :], rhs=sq[:w, :n], start=(j == 0), stop=(j == nch - 1)),
                deps=[ta] + (pdeps if j == 0 else []))
            sqr.release(ss, pet)
            zs.append((zs_, z, lt, w))
        st[u] = dict(s=s, ps=ps, pet=pet, zs=zs)

    def stage_b(u):
        si, it = work[u]
        t0, n = SUBT[si]
        d = st[u]
        ps, pet = d["ps"], d["pet"]
        rs_, rs, rd = rsr.next()
        tq = ph.op("scalar", lambda e, rs=rs, ps=ps, n=n, dim=it["dim"]: e.activation(
            out=rs[:, :n], in_=ps[:, :n], func=AF.Ln, bias=C.epsc[:, 0:1], scale=1.0 / dim), deps=[pet] + rd)
        psr.release(d["s"], tq)
        tr = ph.op("scalar", lambda e, rs=rs, n=n: e.activation(out=rs[:, :n], in_=rs[:, :n], func=AF.Exp, scale=-0.5), deps=[tq])
        d["rs"] = (rs_, rs)
        d["cwork"] = []
        lastu = tr
        for j, (zs_, z, lt, w) in enumerate(d["zs"]):
            g = it["g"][j]
            rope = it["rope"][j]
            os_, ost, od = osr.next()
            if rope is None:
                tn = ph.op("vector", lambda e, ost=ost, z=z, g=g, rs=rs, w=w, n=n: e.scalar_tensor_tensor(
                    out=ost[:w, :n], in0=z[:w, :n], scalar=g, in1=rs[:w, :n], op0=ALU.mult, op1=ALU.mult), deps=[tr, lt] + od)
                zr.release(zs_, tn)
                dtk = ph.dma(it["dst"][j][:, t0:t0 + n], ost[:w, :n], osr.ds[os_], deps=[tn])
                osr.release(os_, dtk)
                lastu = tn
            else:
                ns_, zn, nd = znr.next()
                tn = ph.op("vector", lambda e, zn=zn, z=z, g=g, rs=rs, w=w, n=n: e.scalar_tensor_tensor(
                    out=zn[:w, :n], in0=z[:w, :n], scalar=g, in1=rs[:w, :n], op0=ALU.mult, op1=ALU.mult), deps=[tr, lt] + nd)
                zr.release(zs_, tn)
                s2, ps2, pd2 = psr.next()
                Rm = C.r128 if rope == "H" else C.r64
                tp = ph.op("tensor", lambda e, ps2=ps2, Rm=Rm, zn=zn, w=w, n=n: e.matmul(
                    ps2[:w, :n], lhsT=Rm[:w, :w], rhs=zn[:w, :n], start=True, stop=True), deps=[tn] + pd2)
                d["cwork"].append((j, w, rope, os_, ost, od, ns_, zn, tn, s2, ps2, tp))
                lastu = tn
        d["lastu"] = lastu

    def stage_c(u):
        si, it = work[u]
        t0, n = SUBT[si]
        d = st.pop(u)
        hs, tH, tokH, ms, tM, tokM = tabs[si]
        lastu = d["lastu"]
        for (j, w, rope, os_, ost, od, ns_, zn, tn, s2, ps2, tp) in d["cwork"]:
            tab, ttok = (tH, tokH) if rope == "H" else (tM, tokM)
            a_, t1, ad = t1r.next()
            ta1 = ph.op("gpsimd", lambda e, t1=t1, zn=zn, tab=tab, w=w, n=n: e.tensor_tensor(
                out=t1[:w, :n], in0=zn[:w, :n], in1=tab[:w, 0, :n], op=ALU.mult), deps=[tn, ttok] + ad)
            b_, t2, bd = t2r.next()
            ta2 = ph.op("vector", lambda e, t2=t2, ps2=ps2, tab=tab, w=w, n=n: e.tensor_tensor(
                out=t2[:w, :n], in0=ps2[:w, :n], in1=tab[:w, 1, :n], op=ALU.mult), deps=[tp, ttok] + bd)
            psr.release(s2, ta2)
            fin = ph.op("gpsimd", lambda e, ost=ost, t1=t1, t2=t2, w=w, n=n: e.tensor_tensor(
                out=ost[:w, :n], in0=t1[:w, :n], in1=t2[:w, :n], op=ALU.add), deps=[ta1, ta2] + od)
            znr.release(ns_, fin)
            t1r.release(a_, fin)
            t2r.release(b_, fin)
            dtk = ph.dma(it["dst"][j][:, t0:t0 + n], ost[:w, :n], osr.ds[os_], deps=[fin])
            osr.release(os_, dtk)
            lastu = fin
        rs_, rs = d["rs"]
        rsr.release(rs_, lastu)
        if u + 1 == len(work) or work[u + 1][0] != si:
            tabH.release(hs, lastu)
            tabM.release(ms, lastu)

    nw = len(work)
    PRE = 4
    for u in range(min(PRE, nw)):
        stage_a0(u)
    for tau in range(nw + 4):
        if tau + PRE < nw:
            stage_a0(tau + PRE)
        if tau < nw:
            stage_a(tau)
        if 0 <= tau - 3 < nw:
            stage_b(tau - 3)
        if 0 <= tau - 4 < nw:
            stage_c(tau - 4)
    ph.run()


def attn_phase(P, C, vheads):
    ph = Phase(P, "attn")
    NKT = NT // 128
    LOOK, DEFER = ATT_LOOK, ATT_DEFER
    k128 = Ring(ph, "k128", 3, [128, NT], BF16, dma=True)
    k64 = Ring(ph, "k64", 3, [64, NT], BF16, dma=True)
    vr = Ring(ph, "v", 3, [128, NKT, 256], BF16, dma=True)
    q128 = Ring(ph, "q128", 3, [128, 512], BF16, dma=True)
    q64 = Ring(ph, "q64", 3, [64, 512], BF16, dma=True)
    pr = Ring(ph, "p", 6, [128, 512], BF16)
    rvr = Ring(ph, "rv", 3, [128, 512], F32)
    accR = Ring(ph, "acc", 4, bufs=P.psb[0:4])
    sR = Ring(ph, "s", 4, bufs=P.psb[4:8])
    saD = Ring(ph, "saD", 3, [128, 512], F32)
    saP = Ring(ph, "saP", 3, [128, 512], F32)
    ostb = Ring(ph, "ob", 4, [128, 512], BF16, dma=True)
    ostf = Ring(ph, "of", 4, [128, 512], F32, dma=True)

    groups = []
    for v in range(len(vheads)):
        for (t0, n) in SUBT:
            kts = list(range(NKT)) if t0 < SEQ else [NKT - 2, NKT - 1]
            groups.append(dict(v=v, t0=t0, n=n, kts=kts))
    tiles = [(gi, ti) for gi, g in enumerate(groups) for ti in range(len(g["kts"]))]
    kv = {}
    gst = {}
    pinfo = {}

    def load_kv(v):
        if v >= len(vheads) or v in kv:
            return
        vh = vheads[v]
        kparts = []
        for (src, w) in vh["k"]:
            ring = k128 if w == 128 else k64
            ks, kb, kd = ring.next()
            tk = None
            for q in range(0, NT, 1088):
                tk = ph.dma(kb[:w, q:q + 1088], src[:, q:q + 1088], ring.ds[ks], deps=kd)
            kparts.append((ring, ks, kb, tk, w))
        vs, vb, vd = vr.next()
        tv = None
        Vv = vh["V"].rearrange("(kt p) e -> p kt e", p=128)
        for q in range(0, NKT, 17):
            tv = ph.dma(vb[:, q:q + 17, :vh["ew"]], Vv[:, q:q + 17, :], vr.ds[vs], deps=vd)
        kv[v] = dict(kparts=kparts, vs=vs, vb=vb, tv=tv)

    def start_group(gi):
        if gi >= len(groups) or gi in gst:
            return
        g = groups[gi]
        vh = vheads[g["v"]]
        qparts = []
        for (src, w) in vh["q"]:
            ring = q128 if w == 128 else q64
            qs, qb, qd = ring.next()
            tq = ph.dma(qb[:w, :g["n"]], src[:, g["t0"]:g["t0"] + g["n"]], ring.ds[qs], deps=qd)
            qparts.append((ring, qs, qb, tq, w))
        gst[gi] = dict(qparts=qparts, accs=None, sa={}, lastpv=None)

    def emit_s(idx):
        gi, ti = tiles[idx]
        g = groups[gi]
        v = g["v"]
        vh = vheads[v]
        n = g["n"]
        if ti == 0:
            start_group(gi)
            start_group(gi + 1)
            if gi == 0 or groups[gi - 1]["v"] != v:
                load_kv(v)
                load_kv(v + 1)
        kt = g["kts"][ti]
        kparts = kv[v]["kparts"]
        qparts = gst[gi]["qparts"]
        s, ps, pd = sR.next()
        pet = None
        npart = len(kparts)
        for j in range(npart):
            _, _, kb, tk, w = kparts[j]
            _, _, qb, tq, _ = qparts[j]
            pet = ph.op("tensor", lambda e, ps=ps, kb=kb, qb=qb, w=w, kt=kt, n=n, j=j, npart=npart: e.matmul(
                ps[:, :n], lhsT=kb[:w, kt * 128:(kt + 1) * 128], rhs=qb[:w, :n], start=(j == 0), stop=(j == npart - 1)),
                deps=([tk, tq] + (pd if j == 0 else [])), signal=(j == npart - 1))
        p_, pb, ppd = pr.next()
        ta = ph.op("scalar", lambda e, pb=pb, ps=ps, n=n, sc=vh["scale"]: e.activation(out=pb[:, :n], in_=ps[:, :n], func=AF.Exp,
                                                                                      scale=sc), deps=[pet] + ppd)
        sR.release(s, ta)
        pinfo[idx] = (p_, pb, ta)
        if ti == len(g["kts"]) - 1:
            for (ring, qs, qb, tq, w) in qparts:
                ring.release(qs, pet)
            if gi + 1 == len(groups) or groups[gi + 1]["v"] != v:
                for (ring, ks, kb, tk, w) in kparts:
                    ring.release(ks, pet)

    def emit_pv(idx):
        gi, ti = tiles[idx]
        g = groups[gi]
        v = g["v"]
        vh = vheads[v]
        n = g["n"]
        nec = vh["ew"] // 128
        st_ = gst[gi]
        kt = g["kts"][ti]
        first = ti == 0
        last = ti == len(g["kts"]) - 1
        if first:
            st_["accs"] = [accR.next() for _ in range(nec)]
        p_, pb, ta = pinfo.pop(idx)
        vb, tv = kv[v]["vb"], kv[v]["tv"]
        pet = None
        for ec in range(nec):
            a_, acc, ad = st_["accs"][ec]
            lhs = vb[:, kt, ec * 128:(ec + 1) * 128]
            pet = ph.op("tensor", lambda e, acc=acc, lhs=lhs, pb=pb, n=n, first=first, last=last: e.matmul(
                acc[:, :n], lhsT=lhs, rhs=pb[:, :n], start=first, stop=last),
                deps=[ta, tv] + (ad if first else []), signal=(ec == nec - 1))
        eng = "gpsimd" if (ATT_POOL and ti % 3 == 2) else "vector"
        if eng not in st_["sa"]:
            ring = saP if eng == "gpsimd" else saD
            x_, sa, xd = ring.next()
            tacc = ph.op(eng, lambda e, sa=sa, pb=pb, n=n: e.tensor_copy(out=sa[:, :n], in_=pb[:, :n]), deps=[ta] + xd)
            st_["sa"][eng] = [ring, x_, sa, tacc]
        else:
            rec = st_["sa"][eng]
            sa = rec[2]
            rec[3] = ph.op(eng, lambda e, sa=sa, pb=pb, n=n: e.tensor_tensor(out=sa[:, :n], in0=sa[:, :n], in1=pb[:, :n], op=ALU.add),
                           deps=[ta, rec[3]])
            tacc = rec[3]
        pr.release(p_, pet)
        pr.release(p_, tacc)
        st_["lastpv"] = pet
        if last and (gi + 1 == len(groups) or groups[gi + 1]["v"] != v):
            vr.release(kv[v]["vs"], pet)

    def finalize(gi):
        g = groups[gi]
        vh = vheads[g["v"]]
        n, t0 = g["n"], g["t0"]
        nec = vh["ew"] // 128
        st_ = gst.pop(gi)
        s, ps, pd = sR.next()
        recs = list(st_["sa"].values())
        tsum = None
        for k, (ring, x_, sa, tacc) in enumerate(recs):
            tsum = ph.op("tensor", lambda e, ps=ps, sa=sa, n=n, k=k, nr=len(recs): e.matmul(
                ps[:, :n], lhsT=C.ones32[:], rhs=sa[:, :n], start=(k == 0), stop=(k == nr - 1)),
                deps=[tacc] + (pd if k == 0 else []), signal=(k == len(recs) - 1))
        for (ring, x_, sa, tacc) in recs:
            ring.release(x_, tsum)
        r_, rv, rd = rvr.next()
        t1 = ph.op("vector", lambda e, rv=rv, ps=ps, n=n: e.reciprocal(out=rv[:, :n], in_=ps[:, :n]), deps=[tsum] + rd)
        sR.release(s, t1)
        lt = None
        for ec in range(nec):
            a_, acc, _ = st_["accs"][ec]
            oring = ostb if vh["dt"] == BF16 else ostf
            o_, ob, od = oring.next()
            lt = ph.op("vector", lambda e, ob=ob, acc=acc, rv=rv, n=n: e.tensor_tensor(out=ob[:, :n], in0=acc[:, :n], in1=rv[:, :n],
                                                                                     op=ALU.mult), deps=[t1, st_["lastpv"]] + od)
            accR.release(a_, lt)
            dk = ph.dma(vh["dst"](ec)[:, t0:t0 + n], ob[:, :n], oring.ds[o_], deps=[lt])
            oring.release(o_, dk)
        rvr.release(r_, lt)

    ntl = len(tiles)
    finals = []
    for idx in range(min(LOOK, ntl)):
        emit_s(idx)
    for i in range(ntl):
        if i + LOOK < ntl:
            emit_s(i + LOOK)
        while finals and finals[0][0] <= i:
            finalize(finals.pop(0)[1])
        emit_pv(i)
        gi, ti = tiles[i]
        if ti == len(groups[gi]["kts"]) - 1:
            finals.append((i + DEFER, gi))
    while finals:
        finalize(finals.pop(0)[1])
    ph.run()


def swa_phase(P, C):
    ph = Phase(P, "swa")
    NKT = NT // 128
    NB = SEQ // 128
    scale = 128 ** -0.5
    kr = Ring(ph, "k", 2, [128, NT], BF16, dma=True)
    vr = Ring(ph, "v", 2, [128, NKT, 128], BF16, dma=True)
    qr = Ring(ph, "q", 2, [128, 3, NT], BF16, dma=True)
    pr = Ring(ph, "p", 6, [128, 384], BF16)
    dnr = Ring(ph, "dn", 3, [128, 128], F32)
    osr = Ring(ph, "os", 4, [128, 128], BF16, dma=True)
    accR = Ring(ph, "acc", 4, bufs=P.psb[0:4])
    sR = Ring(ph, "s", 4, bufs=P.psb[4:8])
    for g in range(4):
        ks, kb, kd = kr.next()
        tk = None
        for q in range(0, NT, 1088):
            tk = ph.dma(kb[:, q:q + 1088], C.skT[g * 128:(g + 1) * 128, q:q + 1088], kr.ds[ks], deps=kd if q == 0 else [])
        vs, vb, vd = vr.next()
        tv = None
        Vv = C.svV[:, g * 128:(g + 1) * 128].rearrange("(kt p) e -> p kt e", p=128)
        for q in range(0, NKT, 17):
            tv = ph.dma(vb[:, q:q + 17, :], Vv[:, q:q + 17, :], vr.ds[vs], deps=vd if q == 0 else [])
        qs, qb, qd = qr.next()
        tq = None
        for r in range(3):
            h = 3 * g + r
            tq = ph.dma(qb[:, r, :], C.sqT[h * 128:(h + 1) * 128, :], qr.ds[qs], deps=qd if r == 0 else [])
        lastpe = None
        for nb in range(NKT):
            if nb < NB:
                kts = []
                if nb > 0:
                    kts.append((nb - 1, 0))
                kts.append((nb, None))
                if nb < NB - 1:
                    kts.append((nb + 1, 1))
                kts += [(NKT - 2, None), (NKT - 1, None)]
            else:
                kts = [(NKT - 2, None), (NKT - 1, None)]
            a0, acc_o, ad0 = accR.next()
            a1, acc_s, ad1 = accR.next()
            pinfos = []
            for (kt, mk) in kts:
                s, ps, pd = sR.next()
                pet = ph.op("tensor", lambda e, ps=ps, kb=kb, qb=qb, kt=kt, nb=nb: e.matmul(
                    ps[:, 0:384].rearrange("p (r q) -> p r q", r=3), lhsT=kb[:, kt * 128:(kt + 1) * 128],
                    rhs=qb[:, :, nb * 128:(nb + 1) * 128], start=True, stop=True), deps=[tk, tq] + pd)
                p_, pb, ppd = pr.next()
                ta = ph.op("scalar", lambda e, pb=pb, ps=ps: e.activation(out=pb[:, :], in_=ps[:, 0:384], func=AF.Exp, scale=scale),
                           deps=[pet] + ppd)
                sR.release(s, ta)
                if mk is not None:
                    ta = ph.op("gpsimd", lambda e, pb=pb, mk=mk: e.tensor_tensor(out=pb[:, :], in0=pb[:, :], in1=C.masks[:, mk, :],
                                                                                 op=ALU.mult), deps=[ta])
                pinfos.append((p_, pb, ta, kt))
            for i, (p_, pb, ta, kt) in enumerate(pinfos):
                first = i == 0
                last = i == len(pinfos) - 1
                ph.op("tensor", lambda e, acc_o=acc_o, vb=vb, kt=kt, pb=pb, first=first, last=last: e.matmul(
                    acc_o[:, 0:384], lhsT=vb[:, kt, :], rhs=pb[:, :], start=first, stop=last),
                    deps=[ta, tv] + (ad0 if first else []), signal=False)
                lastpe = ph.op("tensor", lambda e, acc_s=acc_s, pb=pb, first=first, last=last: e.matmul(
                    acc_s[:, 0:384], lhsT=C.ones_bf[:], rhs=pb[:, :], start=first, stop=last),
                    deps=(ad1 if first else []))
                pr.release(p_, lastpe)
            lt = None
            for r in range(3):
                h = 3 * g + r
                d_, dn, dd = dnr.next()
                t1 = ph.op("vector", lambda e, dn=dn, acc_s=acc_s, r=r, h=h: e.tensor_scalar(
                    out=dn[:, :], in0=acc_s[:, r * 128:(r + 1) * 128], scalar1=C.esink[:, h:h + 1], scalar2=None, op0=ALU.add),
                    deps=[lastpe] + dd)
                t2 = ph.op("vector", lambda e, dn=dn: e.reciprocal(out=dn[:, :], in_=dn[:, :]), deps=[t1])
                o_, ob, od = osr.next()
                lt = ph.op("vector", lambda e, ob=ob, acc_o=acc_o, dn=dn, r=r: e.tensor_tensor(
                    out=ob[:, :], in0=acc_o[:, r * 128:(r + 1) * 128], in1=dn[:, :], op=ALU.mult), deps=[t2] + od)
                dnr.release(d_, lt)
                dk = ph.dma(C.yT[h * 128:(h + 1) * 128, nb * 128:(nb + 1) * 128], ob[:, :], osr.ds[o_], deps=[lt])
                osr.release(o_, dk)
            accR.release(a0, lt)
            accR.release(a1, lt)
        kr.release(ks, lastpe)
        vr.release(vs, lastpe)
        qr.release(qs, lastpe)
    ph.run()


def dif_merge_phase(P, C):
    ph = Phase(P, "dmrg")
    yr = Ring(ph, "y", 8, [128, 512], F32, dma=True)
    ydr = Ring(ph, "yd", 4, [128, 512], F32)
    sqr = Ring(ph, "sq", 4, [128, 512], BF16)
    rsr = Ring(ph, "rs", 2, [128, 512], F32)
    osr = Ring(ph, "os", 4, [128, 512], BF16, dma=True)
    psr = Ring(ph, "ps", 8, bufs=P.psb)
    for h in range(5):
        for (t0, n) in SUBT:
            s, ps, pd = psr.next()
            yds = []
            pet = None
            for c in range(2):
                a_, ya, ad = yr.next()
                la = ph.dma(ya[:, :n], C.dyT[h, 0, c * 128:(c + 1) * 128, t0:t0 + n], yr.ds[a_], deps=ad)
                b_, yb, bd = yr.next()
                lb = ph.dma(yb[:, :n], C.dyT[h, 1, c * 128:(c + 1) * 128, t0:t0 + n], yr.ds[b_], deps=bd)
                d_, yd, dd = ydr.next()
                t1 = ph.op("vector", lambda e, yd=yd, yb=yb, ya=ya, n=n: e.scalar_tensor_tensor(
                    out=yd[:, :n], in0=yb[:, :n], scalar=C.lam[:, 1:2], in1=ya[:, :n], op0=ALU.mult, op1=ALU.add), deps=[la, lb] + dd)
                yr.release(a_, t1)
                yr.release(b_, t1)
                q_, sq, qd = sqr.next()
                t2 = ph.op("scalar", lambda e, sq=sq, yd=yd, n=n: e.activation(out=sq[:, :n], in_=yd[:, :n], func=AF.Square),
                           deps=[t1] + qd)
                pet = ph.op("tensor", lambda e, ps=ps, sq=sq, n=n, c=c: e.matmul(ps[:, :n], lhsT=C.ones_bf[:], rhs=sq[:, :n],
                                                                               start=(c == 0), stop=(c == 1)),
                            deps=[t2] + (pd if c == 0 else []))
                sqr.release(q_, pet)
                yds.append((d_, yd, t1))
            r_, rs, rd = rsr.next()
            tq = ph.op("scalar", lambda e, rs=rs, ps=ps, n=n: e.activation(out=rs[:, :n], in_=ps[:, :n], func=AF.Sqrt,
                                                                          bias=C.epsc[:, 0:1], scale=1.0 / 256), deps=[pet] + rd)
            psr.release(s, tq)
            tr = ph.op("vector", lambda e, rs=rs, n=n: e.reciprocal(out=rs[:, :n], in_=rs[:, :n]), deps=[tq])
            lt = None
            for c, (d_, yd, t1) in enumerate(yds):
                o_, ob, od = osr.next()
                lt = ph.op("vector", lambda e, ob=ob, yd=yd, rs=rs, n=n, c=c: e.scalar_tensor_tensor(
                    out=ob[:, :n], in0=yd[:, :n], scalar=C.subg[:, c:c + 1], in1=rs[:, :n], op0=ALU.mult, op1=ALU.mult),
                    deps=[tr, t1] + od)
                ydr.release(d_, lt)
                r0 = 2816 + h * 256 + c * 128
                dk = ph.dma(C.yT[r0:r0 + 128, t0:t0 + n], ob[:, :n], osr.ds[o_], deps=[lt])
                osr.release(o_, dk)
            rsr.release(r_, lt)
    ph.run()


def moe_gate_phase(P, C):
    ph = Phase(P, "gate")
    NKT = NT // 128
    lg = ph.sb("lg", [128, NKT, 8], F32)
    ds = ph.dsem()
    tl = ph.dma(lg[:], C.lgT.rearrange("(kt p) e -> p kt e", p=128), ds)
    combT = ph.sb("combT", [8, NT], F32)
    wk = Ring(ph, "wk", 3, [128, 48], F32)
    psr = Ring(ph, "ps", 8, bufs=P.psb)
    last = None
    for kt in range(NKT):
        w_, w, wd = wk.next()
        L = lg[:, kt, :]
        m1, eq1, l2, m2, eq2, dd, g1, cmb = (w[:, 0:1], w[:, 8:16], w[:, 16:24], w[:, 1:2], w[:, 24:32], w[:, 2:3], w[:, 3:4], w[:, 32:40])
        g2 = w[:, 4:5]
        t = ph.op("vector", lambda e, m1=m1, L=L: e.tensor_reduce(out=m1, in_=L, axis=AX.X, op=ALU.max), deps=[tl] + wd)
        t = ph.op("vector", lambda e, eq1=eq1, L=L, m1=m1: e.tensor_scalar(out=eq1, in0=L, scalar1=m1, scalar2=None, op0=ALU.is_equal), deps=[t])
        t = ph.op("vector", lambda e, l2=l2, eq1=eq1, L=L: e.scalar_tensor_tensor(out=l2, in0=eq1, scalar=-1e30, in1=L, op0=ALU.mult,
                                                                                 op1=ALU.add), deps=[t])
        t = ph.op("vector", lambda e, m2=m2, l2=l2: e.tensor_reduce(out=m2, in_=l2, axis=AX.X, op=ALU.max), deps=[t])
        t = ph.op("vector", lambda e, eq2=eq2, l2=l2, m2=m2: e.tensor_scalar(out=eq2, in0=l2, scalar1=m2, scalar2=None, op0=ALU.is_equal),
                  deps=[t])
        t = ph.op("vector", lambda e, dd=dd, m2=m2, m1=m1: e.tensor_tensor(out=dd, in0=m2, in1=m1, op=ALU.subtract), deps=[t])
        t = ph.op("scalar", lambda e, dd=dd: e.activation(out=dd, in_=dd, func=AF.Exp), deps=[t])
        t = ph.op("vector", lambda e, g1=g1, dd=dd: e.tensor_scalar(out=g1, in0=dd, scalar1=1.0, scalar2=None, op0=ALU.add), deps=[t])
        t = ph.op("vector", lambda e, g1=g1: e.reciprocal(out=g1, in_=g1), deps=[t])
        t = ph.op("vector", lambda e, g2=g2, dd=dd, g1=g1: e.tensor_tensor(out=g2, in0=dd, in1=g1, op=ALU.mult), deps=[t])
        t = ph.op("vector", lambda e, cmb=cmb, eq1=eq1, g1=g1: e.tensor_scalar(out=cmb, in0=eq1, scalar1=g1, scalar2=None, op0=ALU.mult),
                  deps=[t])
        t = ph.op("vector", lambda e, cmb=cmb, eq2=eq2, g2=g2: e.scalar_tensor_tensor(out=cmb, in0=eq2, scalar=g2, in1=cmb, op0=ALU.mult,
                                                                                      op1=ALU.add), deps=[t])
        s, ps, pd = psr.next()
        tp = ph.op("tensor", lambda e, ps=ps, cmb=cmb: e.transpose(out=ps[0:8, 0:128], in_=cmb, identity=C.ident[:]), deps=[t] + pd)
        last = ph.op("vector", lambda e, ps=ps, kt=kt: e.tensor_copy(out=combT[0:8, kt * 128:(kt + 1) * 128], in_=ps[0:8, 0:128]),
                     deps=[tp])
        psr.release(s, last)
        wk.release(w_, tp)
    osr = Ring(ph, "os", 3, [128, 512], F32, dma=True)
    for ex in range(NEXP):
        for (t0, n) in SUBT:
            s, ps, pd = psr.next()
            tp = ph.op("tensor", lambda e, ps=ps, ex=ex, t0=t0, n=n: e.matmul(ps[:, :n], lhsT=C.sel[0:8, ex, :], rhs=combT[0:8, t0:t0 + n],
                                                                             start=True, stop=True), deps=[last] + pd)
            o_, ob, od = osr.next()
            tc = ph.op("scalar", lambda e, ob=ob, ps=ps, n=n: e.activation(out=ob[:, :n], in_=ps[:, :n], func=AF.Copy), deps=[tp] + od)
            psr.release(s, tc)
            dk = ph.dma(C.cb[ex, :, t0:t0 + n], ob[:, :n], osr.ds[o_], deps=[tc])
            osr.release(o_, dk)
    ph.run()


def final_phase(P, C):
    ph = Phase(P, "fin")
    ds = ph.dsem()
    for q in range(0, D, 512):
        ph.dma(C.outT[q:q + 512, :], C.hT[q:q + 512, 0:SEQ], ds)
    ph.run()


GROUPS = [(0, 1024), (1024, 1024), (2048, 1024), (3072, 1280)]


def build(n_layers=DEPTH, dbg=(), stop=None, Lw=DEPTH):
    nc = bass.Bass("TRN2", target_bir_lowering=False)
    C = Ctx()
    L = Lw
    L2 = max(1, Lw // 2)

    def din(name, shape, dt=F32):
        return nc.dram_tensor(name, list(shape), dt, kind="ExternalInput").ap()

    def dscr(name, shape, dt):
        kind = "ExternalOutput" if name in dbg else "Internal"
        return nc.dram_tensor(name, list(shape), dt, kind=kind).ap()

    C.xT = din("xT", [D, NT])
    C.cvec = din("cvec", [128, KC, 2])
    C.ada_w = din("ada_w", [L, D, 6 * D])
    C.w_in = din("w_in", [L, D, IN_W])
    C.w_out = din("w_out", [L, D, D])
    C.w_uq = din("mla_w_uq", [L, 768, 1920])
    C.w_ukv = din("mla_w_ukv", [L, 512, 2560])
    C.ffn_w1 = din("ffn_w1", [L2, D, D])
    C.ffn_w3 = din("ffn_w3", [L2, D, D])
    C.ffn_w2 = din("ffn_w2", [L2, D, D])
    C.router = din("moe_router", [L2, D, NEXP])
    C.moe_w1 = din("moe_w1", [L2, NEXP, D, DFE])
    C.moe_w3 = din("moe_w3", [L2, NEXP, D, DFE])
    C.moe_w2 = din("moe_w2", [L2, NEXP, DFE, D])
    C.vecs = din("vecs", [L, 128, NV])
    C.ropeH = din("ropeH", [2, 128, NT])
    C.ropeM = din("ropeM", [2, 64, NT])
    C.c_ones = din("c_ones", [128, 128])
    C.c_r128 = din("c_r128", [128, 128])
    C.c_r64 = din("c_r64", [64, 64])
    C.c_ident = din("c_ident", [128, 128])
    C.c_sel = din("c_sel", [8, NEXP, 128])
    C.c_masks = din("c_masks", [128, 2, 384])
    C.outT = nc.dram_tensor("outT", [D, SEQ], F32, kind="ExternalOutput").ap()

    C.hT = dscr("hT", [D, NT], F32)
    C.xnT = dscr("xnT", [D, NT], BF16)
    C.zT = dscr("zT", [IN_W, NT], BF16)
    C.sqT = dscr("sqT", [1536, NT], BF16)
    C.skT = dscr("skT", [512, NT], BF16)
    C.svV = dscr("svV", [NT, 512], BF16)
    C.dvV = dscr("dvV", [NT, 1280], BF16)
    C.cqnT = dscr("cqnT", [768, NT], BF16)
    C.ckvnT = dscr("ckvnT", [512, NT], BF16)
    C.mqraw = dscr("mqraw", [1920, NT], BF16)
    C.mkraw = dscr("mkraw", [1280, NT], BF16)
    C.mvV = dscr("mvV", [NT, 1280], BF16)
    C.mqT = dscr("mqT", [1920, NT], BF16)
    C.mkT = dscr("mkT", [1920, NT], BF16)
    C.dqT = dscr("dqT", [1280, NT], BF16)
    C.dkT = dscr("dkT", [1280, NT], BF16)
    C.dyT = dscr("dyT", [5, 2, 256, NT], F32)
    C.yT = dscr("yT", [D, NT], BF16)
    C.hidT = dscr("hidT", [2 * D, NT], BF16)
    C.lgT = dscr("lgT", [NT, NEXP], F32)
    C.cb = dscr("cb", [NEXP, 128, NT], F32)

    P = Prog(nc)
    with ExitStack() as st:
        def sbp(name, shape, dt):
            return st.enter_context(nc.sbuf_tensor(name, list(shape), dt))

        P.psb = [st.enter_context(nc.psum_tensor("psb%d" % i, [128, 512], F32)) for i in range(8)]
        C.scsb = sbp("scsb", [128, KC, 2], BF16)
        C.modsb = sbp("modsb", [128, 6 * KC, 2], F32)
        C.vsb = sbp("vsb", [128, NV], F32)
        C.ones_bf = sbp("ones_bf", [128, 128], BF16)
        C.ones32 = sbp("ones32", [128, 128], F32)
        C.r128 = sbp("r128", [128, 128], BF16)
        C.r64 = sbp("r64", [64, 64], BF16)
        C.ident = sbp("ident", [128, 128], F32)
        C.sel = sbp("sel", [8, NEXP, 128], F32)
        C.masks = sbp("masks", [128, 2, 384], F32)
        C.lam = sbp("lam", [128, 2], F32)
        C.esink = sbp("esink", [128, 12], F32)
        C.subg = sbp("subg", [128, 2], F32)
        C.epsc = sbp("epsc", [128, 1], F32)

        count = [0]

        def go():
            count[0] += 1
            return stop is None or count[0] <= stop

        ph = Phase(P, "c0")
        ph.op("vector", lambda e: e.memset(C.epsc[:], EPS))
        ph.op("vector", lambda e: e.memset(C.ones32[:], 1.0))
        ph.run()
        init_phase(P, C)

        def zdst(t0, n, j, u):
            w = u["chunks"][j][1]
            return C.zT[u["f0"]:u["f0"] + w, t0:t0 + n]

        for l in range(n_layers):
            if not go(): break
            layer_vec_phase(P, C, l)
            if not go(): break
            mod_phase(P, C, l)
            if not go(): break
            norm_phase(P, C, l, 1)
            if not go(): break
            W = C.w_in[l]
            epiZ = EpiCopy(zdst)
            epiSV = EpiCopy(lambda t0, n, j, u: C.svV[t0:t0 + 128, u["f0"]:u["f0"] + n])
            epiDV = EpiCopy(lambda t0, n, j, u: C.dvV[t0:t0 + 128, u["f0"]:u["f0"] + n])
            blocks = (fblocks(W, 0, C_SV, epiZ) + tblocks(W, C_SV, C_CQ, epiSV) + fblocks(W, C_CQ, C_DQ, epiZ)
                      + fblocks(W, C_DQ, C_DV, epiZ) + tblocks(W, C_DV, IN_W, epiDV))
            linear(P, "inp", D, GROUPS, blocks, xT=C.xnT)
            if not go(): break
            items = []
            for h in range(12):
                items.append(dict(src=[(C.zT[h * 128:(h + 1) * 128, :], 128)], g=[C.vsb[:, V_SQN:V_SQN + 1]], dim=128, rope=["H"],
                                  dst=[C.sqT[h * 128:(h + 1) * 128, :]]))
            for g in range(4):
                r0 = C_SK + g * 128
                items.append(dict(src=[(C.zT[r0:r0 + 128, :], 128)], g=[C.vsb[:, V_SKN:V_SKN + 1]], dim=128, rope=["H"],
                                  dst=[C.skT[g * 128:(g + 1) * 128, :]]))
            for c in range(10):
                m = c % 2
                items.append(dict(src=[(C.zT[C_DQ + c * 128:C_DQ + (c + 1) * 128, :], 128)], g=[C.vsb[:, V_DQN + m:V_DQN + m + 1]],
                                  dim=128, rope=["H"], dst=[C.dqT[c * 128:(c + 1) * 128, :]]))
                items.append(dict(src=[(C.zT[C_DK + c * 128:C_DK + (c + 1) * 128, :], 128)], g=[C.vsb[:, V_DKN + m:V_DKN + m + 1]],
                                  dim=128, rope=["H"], dst=[C.dkT[c * 128:(c + 1) * 128, :]]))
            items.append(dict(src=[(C.zT[C_CQ + j * 128:C_CQ + (j + 1) * 128, :], 128) for j in range(6)],
                              g=[C.vsb[:, V_CQ + j:V_CQ + j + 1] for j in range(6)], dim=768, rope=[None] * 6,
                              dst=[C.cqnT[j * 128:(j + 1) * 128, :] for j in range(6)]))
            items.append(dict(src=[(C.zT[C_CKV + j * 128:C_CKV + (j + 1) * 128, :], 128) for j in range(4)],
                              g=[C.vsb[:, V_CKV + j:V_CKV + j + 1] for j in range(4)], dim=512, rope=[None] * 4,
                              dst=[C.ckvnT[j * 128:(j + 1) * 128, :] for j in range(4)]))
            rmsrope_phase(P, C, items)
            if not go(): break
            epq = EpiCopy(lambda t0, n, j, u: C.mqraw[u["f0"]:u["f0"] + u["chunks"][j][1], t0:t0 + n])
            linear(P, "upq", 768, GROUPS, fblocks(C.w_uq[l], 0, 1920, epq), xT=C.cqnT)
            if not go(): break
            epk = EpiCopy(lambda t0, n, j, u: C.mkraw[u["f0"]:u["f0"] + 128, t0:t0 + n])
            epv = EpiCopy(lambda t0, n, j, u: C.mvV[t0:t0 + 128, u["f0"]:u["f0"] + n])
            blocks = []
            for hp in range(5):
                units = []
                for r in range(2):
                    h = hp * 2 + r
                    units.append(dict(mode="F", chunks=[(r * 256, 128)], epi=epk, f0=h * 128))
                    units.append(dict(mode="T", chunks=[(r * 256 + 128, 128)], epi=epv, f0=h * 128))
                blocks.append(dict(segs=[(C.w_ukv[l], hp * 512, 512)], units=units))
            linear(P, "upkv", 512, GROUPS, blocks, xT=C.ckvnT)
            if not go(): break
            items = []
            for h in range(10):
                items.append(dict(src=[(C.mqraw[h * 192:h * 192 + 128, :], 128), (C.mqraw[h * 192 + 128:(h + 1) * 192, :], 64)],
                                  g=[C.vsb[:, V_MQN:V_MQN + 1], C.vsb[0:64, V_MQN + 1:V_MQN + 2]], dim=192, rope=[None, "M"],
                                  dst=[C.mqT[h * 192:h * 192 + 128, :], C.mqT[h * 192 + 128:(h + 1) * 192, :]]))
                items.append(dict(src=[(C.mkraw[h * 128:(h + 1) * 128, :], 128), (C.zT[C_KR:C_KR + 64, :], 64)],
                                  g=[C.vsb[:, V_MKN:V_MKN + 1], C.vsb[0:64, V_MKN + 1:V_MKN + 2]], dim=192, rope=[None, "M"],
                                  dst=[C.mkT[h * 192:h * 192 + 128, :], C.mkT[h * 192 + 128:(h + 1) * 192, :]]))
            rmsrope_phase(P, C, items)
            if not go(): break
            vheads = []
            for h in range(10):
                vheads.append(dict(q=[(C.mqT[h * 192:h * 192 + 128, :], 128), (C.mqT[h * 192 + 128:(h + 1) * 192, :], 64)],
                                   k=[(C.mkT[h * 192:h * 192 + 128, :], 128), (C.mkT[h * 192 + 128:(h + 1) * 192, :], 64)],
                                   V=C.mvV[:, h * 128:(h + 1) * 128], ew=128, scale=192 ** -0.5, dt=BF16,
                                   dst=(lambda ec, h=h: C.yT[1536 + h * 128:1536 + (h + 1) * 128, :])))
            for h in range(5):
                for m in range(2):
                    c = h * 2 + m
                    vheads.append(dict(q=[(C.dqT[c * 128:(c + 1) * 128, :], 128)], k=[(C.dkT[c * 128:(c + 1) * 128, :], 128)],
                                       V=C.dvV[:, h * 256:(h + 1) * 256], ew=256, scale=128 ** -0.5, dt=F32,
                                       dst=(lambda ec, h=h, m=m: C.dyT[h, m, ec * 128:(ec + 1) * 128, :])))
            attn_phase(P, C, vheads)
            if not go(): break
            swa_phase(P, C)
            if not go(): break
            dif_merge_phase(P, C)
            if not go(): break
            eo = EpiResid(C.hT, lambda fc, isc: C.modsb[:, 2 * KC + fc, (1 if isc else 0):(2 if isc else 1)])
            linear(P, "outp", D, GROUPS, fblocks(C.w_out[l], 0, D, eo), xT=C.yT)
            if not go(): break
            norm_phase(P, C, l, 2)
            if not go(): break
            e2 = EpiResid(C.hT, lambda fc, isc: C.modsb[:, 5 * KC + fc, (1 if isc else 0):(2 if isc else 1)])
            i = l // 2
            if l % 2 == 0:
                es = EpiSwiGLU(lambda t0, n, j, u: C.hidT[u["f0"]:u["f0"] + 128, t0:t0 + n])
                blocks = []
                for f in range(0, D, 256):
                    units = [dict(mode="F", chunks=[(o, 128), (256 + o, 128)], epi=es, f0=f + o) for o in (0, 128)]
                    blocks.append(dict(segs=[(C.ffn_w1[i], f, 256), (C.ffn_w3[i], f, 256)], units=units))
                linear(P, "ffu", D, GROUPS, blocks, xT=C.xnT)
                if not go(): break
                linear(P, "ffd", D, GROUPS, fblocks(C.ffn_w2[i], 0, D, e2), xT=C.hidT[0:D, :])
            else:
                er = EpiCopy(lambda t0, n, j, u: C.lgT[t0:t0 + 128, 0:n], dt=F32)
                linear(P, "rtr", D, GROUPS, tblocks(C.router[i], 0, NEXP, er), xT=C.xnT)
                if not go(): break
                moe_gate_phase(P, C)
                if not go(): break
                es = EpiSwiGLU(lambda t0, n, j, u: C.hidT[u["f0"]:u["f0"] + 128, t0:t0 + n],
                               cb=lambda t0, n, u: C.cb[u["ex"], :, t0:t0 + n])
                blocks = []
                for ex in range(NEXP):
                    for f in range(0, DFE, 256):
                        units = [dict(mode="F", chunks=[(o, 128), (256 + o, 128)], epi=es, f0=ex * DFE + f + o, ex=ex) for o in (0, 128)]
                        blocks.append(dict(segs=[(C.moe_w1[i, ex], f, 256), (C.moe_w3[i, ex], f, 256)], units=units))
                linear(P, "mou", D, GROUPS, blocks, xT=C.xnT)
                if not go(): break
                W2 = C.moe_w2[i].rearrange("e k d -> (e k) d")
                linear(P, "mod1", D, GROUPS, fblocks(W2[0:D, :], 0, D, e2), xT=C.hidT[0:D, :])
                e3 = EpiResid(C.hT, lambda fc, isc: C.modsb[:, 5 * KC + fc, (1 if isc else 0):(2 if isc else 1)])
                linear(P, "mod2", D, GROUPS, fblocks(W2[D:2 * D, :], 0, D, e3), xT=C.hidT[D:2 * D, :])
        final_phase(P, C)
    return nc


def _rope_tables(dim):
    rows = SEQ // GRID_W
    t = np.arange(SEQ)
    t_row = (t // GRID_W).astype(np.float32)
    t_col = (t % GRID_W).astype(np.float32)
    quarter = dim // 4
    inv = (np.float32(10000.0) ** (-np.arange(quarter, dtype=np.float32) / np.float32(quarter))).astype(np.float32)
    ang = np.concatenate([t_row[:, None] * inv, t_col[:, None] * inv], axis=-1).astype(np.float32)
    cos = np.cos(ang).astype(np.float32)
    sin = np.sin(ang).astype(np.float32)
    tab = np.zeros((2, dim, NT), np.float32)
    tab[0, :, SEQ:] = 1.0
    half = dim // 2
    tab[0, :half, :SEQ] = cos.T
    tab[0, half:, :SEQ] = cos.T
    tab[1, :half, :SEQ] = sin.T
    tab[1, half:, :SEQ] = sin.T
    return tab


def _rot_lhsT(dim):
    half = dim // 2
    m = np.zeros((dim, dim), np.float32)
    for i in range(half):
        m[i + half, i] = -1.0
        m[i, i + half] = 1.0
    return m


def _host_inputs(inp, L=DEPTH, ncores=NCORES):
    f = lambda a: np.ascontiguousarray(np.asarray(a, dtype=np.float32))
    vecs = np.zeros((L, 128, NV), np.float32)
    for l in range(L):
        vecs[l, :, V_N1:V_N1 + KC] = f(inp["norm1_g"])[l].reshape(KC, 128).T
        vecs[l, :, V_N2:V_N2 + KC] = f(inp["norm2_g"])[l].reshape(KC, 128).T
        vecs[l, :, V_ADAB:V_ADAB + 6 * KC] = f(inp["ada_b"])[l].reshape(6 * KC, 128).T
        vecs[l, :, V_CQ:V_CQ + 6] = f(inp["mla_cq_norm"])[l].reshape(6, 128).T
        vecs[l, :, V_CKV:V_CKV + 4] = f(inp["mla_ckv_norm"])[l].reshape(4, 128).T
        vecs[l, :, V_SQN] = f(inp["swa_q_norm"])[l]
        vecs[l, :, V_SKN] = f(inp["swa_k_norm"])[l]
        vecs[l, :, V_MQN] = f(inp["mla_q_norm"])[l][:128]
        vecs[l, :64, V_MQN + 1] = f(inp["mla_q_norm"])[l][128:]
        vecs[l, :, V_MKN] = f(inp["mla_k_norm"])[l][:128]
        vecs[l, :64, V_MKN + 1] = f(inp["mla_k_norm"])[l][128:]
        vecs[l, :, V_DQN:V_DQN + 2] = f(inp["dif_q_norm"])[l].T
        vecs[l, :, V_DKN:V_DKN + 2] = f(inp["dif_k_norm"])[l].T
        vecs[l, :, V_SUB:V_SUB + 2] = f(inp["dif_subln"])[l].reshape(2, 128).T
        vecs[l, :, V_LAM:V_LAM + 4] = f(inp["dif_lambda"])[l].T
        vecs[l, :, V_SINK:V_SINK + 12] = f(inp["swa_sink"])[l][None, :]
    masks = np.zeros((128, 2, 384), np.float32)
    k = np.arange(128)[:, None]
    q = np.arange(128)[None, :]
    masks[:, 0, :] = np.tile((q <= k).astype(np.float32), (1, 3))
    masks[:, 1, :] = np.tile((k <= q).astype(np.float32), (1, 3))
    sel = np.zeros((8, NEXP, 128), np.float32)
    for e in range(NEXP):
        sel[e, e, :] = 1.0
    shared = dict(
        ada_w=f(inp["ada_w"]), w_in=f(inp["w_in"]), w_out=f(inp["w_out"]), mla_w_uq=f(inp["mla_w_uq"]),
        mla_w_ukv=f(inp["mla_w_ukv"]), ffn_w1=f(inp["ffn_w1"]), ffn_w3=f(inp["ffn_w3"]), ffn_w2=f(inp["ffn_w2"]),
        moe_router=f(inp["moe_router"]), moe_w1=f(inp["moe_w1"]), moe_w3=f(inp["moe_w3"]), moe_w2=f(inp["moe_w2"]),
        vecs=vecs, ropeH=_rope_tables(128), ropeM=_rope_tables(64), c_ones=np.ones((128, 128), np.float32),
        c_r128=_rot_lhsT(128), c_r64=_rot_lhsT(64), c_ident=np.eye(128, dtype=np.float32), c_sel=sel, c_masks=masks)
    x = f(inp["x"])
    ctx = f(inp["ctx"])
    c = f(inp["c"])
    cc = f(inp["c_ctx"])
    maps = []
    for b in range(ncores):
        xT = np.ascontiguousarray(np.concatenate([x[b].T, ctx[b].T], axis=1))
        cvec = np.ascontiguousarray(np.stack([c[b].reshape(KC, 128).T, cc.reshape(KC, 128).T], axis=-1))
        m = dict(shared)
        m["xT"] = xT
        m["cvec"] = cvec
        maps.append(m)
    return maps


def kernel(**inputs):
    maps = _host_inputs(inputs)
    nc = build()
    res = run_bass_kernel_spmd(nc, maps, core_ids=list(range(NCORES)))
    out = np.stack([np.ascontiguousarray(res.results[b]["outT"].T) for b in range(NCORES)], axis=0)
    return out.astype(np.float32)
```
